# Optimizing a Trainium2 kernel written in Bass

```python
import jax, jax.numpy as jnp
from jax import lax
import numpy as np

D_MODEL = 1024
BATCH = 8
SEQ = 2048
DEPTH = 4

CHUNK = 64
GLA_HEADS = 4
GLA_DK = 64
GLA_DV = 128
GLA_RANK = 16
GLA_TAU = 16.0
RET_HEADS = 4
RET_DK = 128
RET_DV = 128
ROPE_BASE = 10000.0
D_FF = 2816
N_EXPERTS = 8
TOP_K = 2
D_FF_EXPERT = 1408
EPS = 1e-6
GLA_QK = GLA_HEADS * GLA_DK
GLA_V = GLA_HEADS * GLA_DV
RET_QK = RET_HEADS * RET_DK
RET_V = RET_HEADS * RET_DV
MIX_WIDTH = GLA_V + RET_V
IN_SPLITS = (GLA_QK, GLA_QK, GLA_V, GLA_V, GLA_RANK, RET_QK, RET_QK, RET_V, RET_V)
IN_WIDTH = sum(IN_SPLITS)
N_DENSE = (DEPTH + 1) // 2
N_MOE = DEPTH // 2

kernel_name = 'hybrid_gla_retention_moe_adaln'

F32 = jnp.float32


def rms_norm(x, g):
    x32 = x.astype(F32)
    y = x32 * lax.rsqrt(jnp.mean(x32 * x32, axis=-1, keepdims=True) + EPS)
    return (y * g.astype(F32)).astype(x.dtype)


def to_chunks(t, n_heads):
    return t.reshape(t.shape[0], t.shape[1] // CHUNK, CHUNK, n_heads, -1)


def chunk_scan(decay, update):
    def step(state, inp):
        a, u = inp
        return a[..., None] * state + u, state
    init = jnp.zeros_like(update[:, 0])
    _, prev = lax.scan(step, init, (jnp.moveaxis(decay, 1, 0), jnp.moveaxis(update, 1, 0)))
    return jnp.moveaxis(prev, 0, 1)


def gla_core(q, k, v, log_a):
    b = jnp.cumsum(log_a, axis=2)
    b_last = b[:, :, -1:]
    eb, enb = jnp.exp(b), jnp.exp(-b)
    q_f, k_f = q * eb, k * enb
    q_b, k_b = q * enb, k * eb
    t_idx = jnp.arange(CHUNK)
    causal = t_idx[:, None] >= t_idx[None, :]
    s_fwd = jnp.einsum('bnthk,bnshk->bnhts', q_f, k_f)
    s_bwd = jnp.einsum('bnthk,bnshk->bnhts', q_b, k_b)
    scores = jnp.where(causal, s_fwd, s_bwd)
    o_intra = jnp.einsum('bnhts,bnshv->bnthv', scores, v)
    upd = jnp.einsum('bnshk,bnshv->bnhkv', k * jnp.exp(b_last - b), v)
    s_prev = chunk_scan(jnp.exp(b_last[:, :, 0]), upd)
    o_inter = jnp.einsum('bnthk,bnhkv->bnthv', q_f, s_prev)
    return o_intra + o_inter


def retention_core(q, k, v, log_gamma):
    t = jnp.arange(CHUNK, dtype=F32)
    dist = jnp.abs(t[:, None] - t[None, :])
    d_intra = jnp.exp(log_gamma[:, None, None] * dist)
    scores = jnp.einsum('bnthk,bnshk->bnhts', q, k) * d_intra
    o_intra = jnp.einsum('bnhts,bnshv->bnthv', scores, v)
    k_dec = k * jnp.exp(log_gamma[None, :] * (CHUNK - 1 - t)[:, None])[None, None, :, :, None]
    upd = jnp.einsum('bnshk,bnshv->bnhkv', k_dec, v)
    decay = jnp.broadcast_to(jnp.exp(log_gamma * CHUNK)[None, None, :, None], upd.shape[:-1])
    r_prev = chunk_scan(decay, upd)
    q_dec = q * jnp.exp(log_gamma[None, :] * (t + 1)[:, None])[None, None, :, :, None]
    o_inter = jnp.einsum('bnthk,bnhkv->bnthv', q_dec, r_prev)
    return o_intra + o_inter


def rotary(x, positions):
    half = x.shape[-1] // 2
    inv_freq = ROPE_BASE ** (-jnp.arange(half, dtype=F32) / half)
    ang = positions.astype(F32)[..., None] * inv_freq
    cos, sin = jnp.cos(ang)[:, :, None, :], jnp.sin(ang)[:, :, None, :]
    x1, x2 = x[..., :half], x[..., half:]
    return jnp.concatenate([x1 * cos - x2 * sin, x1 * sin + x2 * cos], axis=-1)


def hybrid_mixer(h, positions, w_in, w_alpha, b_alpha, gla_g, gn_g, gn_b, w_out):
    bsz, seq, _ = h.shape
    proj = (h @ w_in).astype(F32)
    gq, gk, gv, gr, ga, rq, rk, rv, rg = jnp.split(proj, np.cumsum(IN_SPLITS)[:-1], axis=-1)
    log_a = jax.nn.log_sigmoid(ga @ w_alpha.astype(F32) + b_alpha.astype(F32)) / GLA_TAU
    o_g = gla_core(to_chunks(gq * GLA_DK ** -0.5, GLA_HEADS), to_chunks(gk, GLA_HEADS),
                   to_chunks(gv, GLA_HEADS), to_chunks(log_a, GLA_HEADS))
    o_g = o_g.reshape(bsz, seq, GLA_HEADS, GLA_DV)
    o_g = o_g * lax.rsqrt(jnp.mean(o_g * o_g, axis=-1, keepdims=True) + EPS)
    o_g = o_g.reshape(bsz, seq, GLA_V) * gla_g.astype(F32) * jax.nn.silu(gr)
    log_gamma = jnp.log(1.0 - 2.0 ** (-5.0 - jnp.arange(RET_HEADS, dtype=F32)))
    rq = rotary(rq.reshape(bsz, seq, RET_HEADS, RET_DK), positions) * RET_DK ** -0.5
    rk = rotary(rk.reshape(bsz, seq, RET_HEADS, RET_DK), positions)
    o_r = retention_core(to_chunks(rq, RET_HEADS), to_chunks(rk, RET_HEADS),
                         to_chunks(rv, RET_HEADS), log_gamma)
    o_r = o_r.reshape(bsz, seq, RET_HEADS, RET_DV)
    mu = jnp.mean(o_r, axis=-1, keepdims=True)
    var = jnp.mean(jnp.square(o_r - mu), axis=-1, keepdims=True)
    o_r = ((o_r - mu) * lax.rsqrt(var + EPS)).reshape(bsz, seq, RET_V)
    o_r = (o_r * gn_g.astype(F32) + gn_b.astype(F32)) * jax.nn.silu(rg)
    o = jnp.concatenate([o_g, o_r], axis=-1).astype(h.dtype)
    return o @ w_out


def swiglu(h, w1, w3, w2):
    return (jax.nn.silu(h @ w1) * (h @ w3)) @ w2


def moe_swiglu(h, router_w, w1, w3, w2):
    logits = (h @ router_w).astype(F32)
    top_v, top_i = lax.top_k(logits, TOP_K)
    probs = jax.nn.softmax(top_v, axis=-1)
    gates = jnp.sum(jax.nn.one_hot(top_i, N_EXPERTS, dtype=F32) * probs[..., None], axis=-2)
    gates = gates.astype(h.dtype)
    out = jnp.zeros_like(h)
    for e in range(N_EXPERTS):
        out = out + gates[..., e:e + 1] * swiglu(h, w1[e], w3[e], w2[e])
    return out


def setup_inputs(seed: int = 0) -> dict:
    key = jax.random.key(seed)
    ks = jax.random.split(key, 24)
    nrm = lambda k, shape, scale: jax.random.normal(k, shape, F32) * scale
    D = D_MODEL
    offsets = jax.random.randint(ks[2], (BATCH, 1), 0, 4096, dtype=jnp.int32)
    return {
        'x': nrm(ks[0], (BATCH, SEQ, D), 1.0),
        'c': nrm(ks[1], (BATCH, D), 1.0),
        'positions': offsets + jnp.arange(SEQ, dtype=jnp.int32)[None, :],
        'ada_w': nrm(ks[3], (DEPTH, D, 6 * D), 0.5 * D ** -0.5),
        'ada_b': nrm(ks[4], (DEPTH, 6 * D), 0.02),
        'norm_mix_g': 1.0 + nrm(ks[5], (DEPTH, D), 0.02),
        'norm_ffn_g': 1.0 + nrm(ks[6], (DEPTH, D), 0.02),
        'w_in': nrm(ks[7], (DEPTH, D, IN_WIDTH), D ** -0.5),
        'gla_w_alpha': nrm(ks[8], (DEPTH, GLA_RANK, GLA_QK), GLA_RANK ** -0.5),
        'gla_b_alpha': nrm(ks[9], (DEPTH, GLA_QK), 0.1),
        'gla_norm_g': 1.0 + nrm(ks[10], (DEPTH, GLA_V), 0.02),
        'ret_gn_g': 1.0 + nrm(ks[11], (DEPTH, RET_V), 0.02),
        'ret_gn_b': nrm(ks[12], (DEPTH, RET_V), 0.02),
        'w_out': nrm(ks[13], (DEPTH, MIX_WIDTH, D), MIX_WIDTH ** -0.5),
        'ffn_w1': nrm(ks[14], (N_DENSE, D, D_FF), D ** -0.5),
        'ffn_w3': nrm(ks[15], (N_DENSE, D, D_FF), D ** -0.5),
        'ffn_w2': nrm(ks[16], (N_DENSE, D_FF, D), D_FF ** -0.5),
        'router_w': nrm(ks[17], (N_MOE, D, N_EXPERTS), D ** -0.5),
        'moe_w1': nrm(ks[18], (N_MOE, N_EXPERTS, D, D_FF_EXPERT), D ** -0.5),
        'moe_w3': nrm(ks[19], (N_MOE, N_EXPERTS, D, D_FF_EXPERT), D ** -0.5),
        'moe_w2': nrm(ks[20], (N_MOE, N_EXPERTS, D_FF_EXPERT, D), D_FF_EXPERT ** -0.5),
        'final_g': 1.0 + nrm(ks[21], (D,), 0.02),
    }


def reference(x, c, positions, ada_w, ada_b, norm_mix_g, norm_ffn_g, w_in, gla_w_alpha,
              gla_b_alpha, gla_norm_g, ret_gn_g, ret_gn_b, w_out, ffn_w1, ffn_w3, ffn_w2,
              router_w, moe_w1, moe_w3, moe_w2, final_g):
    cond = jax.nn.silu(c)
    for layer in range(DEPTH):
        mod = (cond @ ada_w[layer] + ada_b[layer])[:, None, :]
        sh1, sc1, g1, sh2, sc2, g2 = jnp.split(mod, 6, axis=-1)
        h = rms_norm(x, norm_mix_g[layer]) * (1.0 + sc1) + sh1
        x = x + g1 * hybrid_mixer(h, positions, w_in[layer], gla_w_alpha[layer],
                                  gla_b_alpha[layer], gla_norm_g[layer], ret_gn_g[layer],
                                  ret_gn_b[layer], w_out[layer])
        h = rms_norm(x, norm_ffn_g[layer]) * (1.0 + sc2) + sh2
        i = layer // 2
        if layer % 2 == 0:
            y = swiglu(h, ffn_w1[i], ffn_w3[i], ffn_w2[i])
        else:
            y = moe_swiglu(h, router_w[i], moe_w1[i], moe_w3[i], moe_w2[i])
        x = x + g2 * y
    return rms_norm(x, final_g)
```

```python
import math
from contextlib import ExitStack

import numpy as np
import concourse.bass as bass
import concourse.mybir as mybir
from concourse.bass_utils import run_bass_kernel_spmd

F32 = mybir.dt.float32
BF16 = mybir.dt.bfloat16
I32 = mybir.dt.int32
AF = mybir.ActivationFunctionType
ALU = mybir.AluOpType
AX = mybir.AxisListType

D = 1024
SEQ = 2048
NT = 16
DEPTH = 4
D_FF = 2816
NE = 8
D_FFE = 1408
EPS = 1e-6
ALL = slice(None)
import os as _os
_DBG_STOP = int(_os.environ.get('DBG_STOP', '0'))


class Buf:
    __slots__ = ("name", "w", "r", "excl")

    def __init__(self, name, excl=False):
        self.name = name
        self.w = None
        self.r = {}
        self.excl = excl


class V:
    __slots__ = ("ap", "bufs")

    def __init__(self, ap, bufs):
        self.ap = ap
        self.bufs = bufs


class T:
    def __init__(self, tensor, name, nsub=1, excl=False):
        self.t = tensor
        self.name = name
        self.bufs = [Buf(f"{name}.{i}", excl) for i in range(nsub)]

    def __call__(self, *idx, sub=None):
        ap = self.t[idx] if idx else self.t[:]
        if sub is None:
            bufs = self.bufs
        elif isinstance(sub, int):
            bufs = [self.bufs[sub]]
        else:
            bufs = [self.bufs[s] for s in sub]
        return V(ap, bufs)


class Eng:
    def __init__(self, key, h, sem):
        self.key = key
        self.h = h
        self.sem = sem
        self.seq = 0
        self.cnt = 0
        self.last = None
        self.last_inc = False
        self.miles = []
        self.seen = {}


class Sync:
    def __init__(self, nc, sems, dma_sems):
        self.nc = nc
        keys = [("pe", nc.tensor), ("act", nc.scalar), ("dve", nc.vector),
                ("pool", nc.gpsimd), ("sp", nc.sync)]
        self.E = {}
        for (k, h), sm in zip(keys, sems):
            self.E[k] = Eng(k, h, sm)
        self.dsem = dma_sems
        self.dval = {q: [0] * len(v) for q, v in dma_sems.items()}
        self.drr = {q: 0 for q in dma_sems}
        self.nwaits = 0
        self.nins = 0

    def _resolve(self, tok):
        if tok[0] == 'd':
            return ('d', tok[1], tok[2]), self.dsem[tok[1]][tok[2]], tok[3]
        e = self.E[tok[1]]
        n = tok[2]
        val = None
        for (sq, v) in reversed(e.miles):
            if sq >= n:
                val = v
            else:
                break
        if val is None:
            assert e.seq >= n and e.last is not None
            if not e.last_inc:
                e.cnt += 1
                e.last.then_inc(e.sem, 1)
                e.last_inc = True
                e.miles.append((e.seq, e.cnt))
            val = e.cnt
        return ('e', e.key), e.sem, val

    def _wait(self, eng, toks, raw=()):
        e = self.E[eng]
        need = {}
        for lst, same_ok in ((toks, True), (raw, False)):
            for t in lst:
                if t is None:
                    continue
                if t[0] == 'e' and t[1] == eng and (same_ok or eng == "pe"):
                    continue
                sk, sh, v = self._resolve(t)
                if e.seen.get(sk, 0) >= v:
                    continue
                if sk not in need or need[sk][1] < v:
                    need[sk] = (sh, v)
        for sk, (sh, v) in need.items():
            e.h.wait_ge(sh, v)
            e.seen[sk] = v
            self.nwaits += 1

    @staticmethod
    def _deps(reads, writes):
        toks = []
        raw = []
        for b in reads:
            raw.append(b.w)
            if b.excl:
                toks.extend(b.r.values())
        for b in writes:
            toks.append(b.w)
            toks.extend(b.r.values())
        return toks, raw

    @staticmethod
    def _commit(tok, reads, writes):
        key = tok[:2] if tok[0] == 'e' else tok[:3]
        for b in reads:
            b.r[key] = tok
        for b in writes:
            b.w = tok
            b.r = {}

    def op(self, eng, fn, reads=(), writes=(), signal=None):
        e = self.E[eng]
        if signal is None:
            signal = eng != "pe"
        self._wait(eng, *self._deps(reads, writes))
        ins = fn(e.h)
        e.seq += 1
        self.nins += 1
        e.last = ins
        e.last_inc = False
        if signal:
            e.cnt += 1
            ins.then_inc(e.sem, 1)
            e.last_inc = True
            e.miles.append((e.seq, e.cnt))
            if len(e.miles) > 4096:
                e.miles = e.miles[-2048:]
        tok = ('e', eng, e.seq)
        self._commit(tok, reads, writes)
        return tok

    def dma(self, q, out, in_, reads=(), writes=(), **kw):
        e = self.E[q]
        i = self.drr[q]
        self.drr[q] = (i + 1) % len(self.dsem[q])
        toks, raw = self._deps(reads, writes)
        if self.dval[q][i] > 0:
            toks.append(('d', q, i, self.dval[q][i]))
        self._wait(q, toks, raw)
        ins = e.h.dma_start(out=out, in_=in_, **kw)
        ins.then_inc(self.dsem[q][i], 16)
        self.dval[q][i] += 16
        self.nins += 1
        tok = ('d', q, i, self.dval[q][i])
        self._commit(tok, reads, writes)
        return tok

    def wait_bufs(self, eng, bufs):
        toks = []
        for b in bufs:
            toks.append(b.w)
            toks.extend(b.r.values())
        self._wait(eng, toks)

    @staticmethod
    def _rb(*vs):
        out = []
        for v in vs:
            if isinstance(v, V):
                out.extend(v.bufs)
        return out

    @staticmethod
    def _a(v):
        return v.ap if isinstance(v, V) else v

    def mm(self, out, lhsT, rhs, start=True, stop=True, signal=None, **kw):
        if signal is None:
            signal = bool(stop)
        return self.op("pe", lambda h: h.matmul(out.ap, lhsT.ap, rhs.ap, start=start, stop=stop, **kw),
                       reads=self._rb(lhsT, rhs), writes=out.bufs, signal=signal)

    def tr(self, out, in_, ident, signal=True):
        return self.op("pe", lambda h: h.transpose(out.ap, in_.ap, ident.ap),
                       reads=self._rb(in_, ident), writes=out.bufs, signal=signal)

    def act(self, out, in_, func, bias=None, scale=None):
        kw = {}
        if bias is not None:
            kw["bias"] = self._a(bias)
        if scale is not None:
            kw["scale"] = self._a(scale)
        return self.op("act", lambda h: h.activation(out=out.ap, in_=in_.ap, func=func, **kw),
                       reads=self._rb(in_, bias, scale), writes=out.bufs)

    def tt(self, eng, out, in0, in1, op):
        return self.op(eng, lambda h: h.tensor_tensor(out=out.ap, in0=in0.ap, in1=in1.ap, op=op),
                       reads=self._rb(in0, in1), writes=out.bufs)

    def ts(self, eng, out, in0, s1, s2=None, op0=ALU.mult, op1=None):
        kw = {}
        if op1 is not None:
            kw["op1"] = op1
        return self.op(eng, lambda h: h.tensor_scalar(out=out.ap, in0=in0.ap, scalar1=self._a(s1),
                                                      scalar2=self._a(s2), op0=op0, **kw),
                       reads=self._rb(in0, s1, s2), writes=out.bufs)

    def stt(self, out, in0, scalar, in1, op0, op1):
        return self.op("dve", lambda h: h.scalar_tensor_tensor(out=out.ap, in0=in0.ap, scalar=self._a(scalar),
                                                               in1=in1.ap, op0=op0, op1=op1),
                       reads=self._rb(in0, scalar, in1), writes=out.bufs)

    def copy(self, eng, out, in_):
        if eng == "act":
            return self.op(eng, lambda h: h.copy(out=out.ap, in_=in_.ap), reads=in_.bufs, writes=out.bufs)
        return self.op(eng, lambda h: h.tensor_copy(out=out.ap, in_=in_.ap), reads=in_.bufs, writes=out.bufs)

    def reduce(self, out, in_, op, axis=AX.X):
        return self.op("dve", lambda h: h.tensor_reduce(out=out.ap, in_=in_.ap, axis=axis, op=op),
                       reads=in_.bufs, writes=out.bufs)

    def memset(self, eng, out, val):
        return self.op(eng, lambda h: h.memset(out.ap, val), writes=out.bufs)


class CL:
    off = {}
    n = 0

    @classmethod
    def add(cls, name, w):
        cls.off[name] = (cls.n, cls.n + w)
        cls.n += w


for _n, _w in [("ident", 128), ("ones", 128), ("tri", 128), ("dm", 128), ("cind", 2), ("m1", 128), ("m2", 128),
               ("dret", 512), ("qd0", 512), ("qd1", 512), ("kdec", 8), ("invf", 64), ("c0", 128), ("c1", 128), ("hm", 2)]:
    CL.add(_n, _w)

GAMMA = [1.0 - 2.0 ** (-5.0 - h) for h in range(4)]


def make_consts():
    c = np.zeros((128, CL.n), np.float64)

    def put(name, arr):
        a, b = CL.off[name]
        c[:, a:b] = np.asarray(arr, np.float64).reshape(128, b - a)

    s = np.arange(128)[:, None]
    t = np.arange(128)[None, :]
    same = (s // 64) == (t // 64)
    put("ident", np.eye(128))
    put("ones", np.ones((128, 128)))
    put("tri", np.where(same & (s <= t), -1.0 / 16, 0.0))
    put("dm", np.where(same & (s > t), -1.0 / 16, 0.0))
    put("cind", np.where((s // 64) == np.arange(2)[None, :], -1.0 / 16, 0.0))
    put("m1", np.where(same & (t >= s), 1.0, 0.0))
    put("m2", np.where(same & (t < s), 1.0, 0.0))
    dret = np.zeros((128, 4, 128))
    qd0 = np.zeros((128, 4, 128))
    qd1 = np.zeros((128, 4, 128))
    kdec = np.zeros((128, 8))
    tt = np.arange(128)
    for h in range(4):
        lg = math.log(GAMMA[h])
        dret[:, h, :] = np.where(same, np.exp(lg * np.abs(t - s)), 0.0) * 128 ** -0.5
        qd = np.exp(lg * ((tt % 64) + 1)) * 128 ** -0.5
        qd0[:, h, :] = np.where(tt < 64, qd, 0.0)[None, :]
        qd1[:, h, :] = np.where(tt >= 64, qd, 0.0)[None, :]
        kdec[:, h] = np.where(tt < 64, np.exp(lg * (63 - (tt % 64))), 0.0)
        kdec[:, 4 + h] = np.where(tt >= 64, np.exp(lg * (63 - (tt % 64))), 0.0)
    put("dret", dret)
    put("qd0", qd0)
    put("qd1", qd1)
    put("kdec", kdec)
    invf = 10000.0 ** (-np.arange(64) / 64.0)
    put("invf", np.broadcast_to(invf[None, :], (128, 64)))
    put("c0", np.broadcast_to((tt < 64)[None, :], (128, 128)))
    put("c1", np.broadcast_to((tt >= 64)[None, :], (128, 128)))
    put("hm", np.stack([tt < 64, tt >= 64], axis=1))
    return c.astype(np.float32)


def build_program(layers=(0, 1, 2, 3), do_final=True, parts=("gla", "ret", "ffn"), nslots=6, debug=False):
    nc = bass.Bass("TRN2", target_bir_lowering=False)

    def din(name, shape, dt=F32):
        return nc.dram_tensor(name, list(shape), dt, kind="ExternalInput").ap()

    x_d = din("x", [SEQ, D])
    c_d = din("cvec", [128, 8])
    pos_d = din("pos", [128, NT], I32)
    cst_d = din("cst", [128, CL.n])
    adaw_d = din("ada_w", [DEPTH, D, 6 * D])
    adab_d = din("ada_b_fm", [DEPTH, 128, 48])
    ng_d = din("norm_g_fm", [128, 2 * DEPTH * 8 + 8])
    win_d = din("w_in", [DEPTH, D, 3600])
    wal_d = din("w_alpha_aug", [DEPTH, 17, 256])
    bc_d = din("bc_params", [DEPTH, 128, 1536])
    wout_d = din("w_out", [DEPTH, D, D])
    w1_d = din("ffn_w1", [2, D, D_FF])
    w3_d = din("ffn_w3", [2, D, D_FF])
    w2_d = din("ffn_w2", [2, D_FF, D])
    rw_d = din("router_w", [2, D, NE])
    m1_d = din("moe_w1", [2, NE, D, D_FFE])
    m3_d = din("moe_w3", [2, NE, D, D_FFE])
    m2_d = din("moe_w2", [2, NE, D_FFE, D])
    y_d = nc.dram_tensor("y", [SEQ, D], F32, kind="ExternalOutput").ap()
    dbg_d = nc.dram_tensor("dbg", [128, 4096], F32, kind="ExternalOutput").ap() if debug else None

    with ExitStack() as st:
        def sb(name, shape, dt, nsub=1):
            return T(st.enter_context(nc.sbuf_tensor(name, list(shape), dt)), name, nsub)

        X = sb("X", [128, 8, SEQ], F32, NT)
        HT = sb("HT", [128, 8, SEQ], BF16, NT)
        CST = sb("CST", [128, CL.n], F32)
        SLOT = [sb(f"slot{i}", [128, 4096], BF16) for i in range(nslots)]
        Fp = [sb(f"F{i}", [128, 512], F32) for i in range(8)]
        Hp = [sb(f"H{i}", [128, 512], BF16) for i in range(14)]
        NG = sb("NG", [128, 2 * DEPTH * 8 + 8], F32)
        MOD = sb("MOD", [128, 48], F32)
        MODB = sb("MODB", [128, 48], F32)
        AB = sb("AB", [128, 16], F32)
        CV = sb("CV", [128, 8], F32)
        CVB = sb("CVB", [128, 8], BF16)
        IDB = sb("IDB", [128, 128], BF16)
        WGA = sb("WGA", [128, 8, 16], BF16)
        WAL = sb("WAL", [32, 256], BF16)
        GAT = sb("GAT", [32, 128], BF16)
        BCP = sb("BCP", [128, 1536], F32)
        ROT = sb("ROT", [128, NT, 128], F32)
        SM = sb("SM", [128, 64], F32)
        POSI = sb("POSI", [128, NT], I32)
        RW = sb("RW", [128, 8, 8], BF16)
        GATES = sb("GATES", [128, NT, 8], F32)
        GBL = [sb(f"GBL{i}", [128, 128], F32) for i in range(2)]
        P = [T(st.enter_context(nc.psum_tensor(f"P{i}", [128, 512], F32)), f"P{i}", excl=True) for i in range(7)]
        PB = T(st.enter_context(nc.psum_tensor("PB", [128, 1024], BF16)), "PB", excl=True)
        sems = [st.enter_context(nc.semaphore(f"s_{k}")) for k in ["pe", "act", "dve", "pool", "sp"]]
        dsems = {q: [st.enter_context(nc.semaphore(f"d_{q}{i}")) for i in range(12)] for q in ["sp", "act", "pool"]}
        S = Sync(nc, sems, dsems)

        def C(name, lo=None, hi=None, rows=ALL):
            a, b = CL.off[name]
            if lo is not None:
                a, b = a + lo, a + hi
            return CST(rows, slice(a, b))

        ident = C("ident")
        ones = C("ones")

        def pbf(p, w=1024):
            return V(PB.t[:, 0:w], PB.bufs)

        jobs = []
        job_slot = {}
        state = {"next": 0}
        free = list(range(nslots))

        def issue_pending():
            while free and state["next"] < len(jobs):
                name, fn = jobs[state["next"]]
                state["next"] += 1
                si = free.pop(0)
                sl = SLOT[si]
                for (o, i) in fn(sl):
                    S.dma("pool", o, i, writes=sl.bufs)
                job_slot[name] = si

        def wget(name):
            assert name in job_slot, f"weight job {name} not issued (ring too small)"
            return SLOT[job_slot[name]]

        def wrel(name):
            si = job_slot.pop(name)
            free.append(si)
            issue_pending()

        def job_cols(name, src2d, c0, w):
            def fn(sl):
                dst = sl.t[:, 0:8 * w].rearrange("p (k c) -> p k c", k=8)
                return [(dst, src2d.rearrange("(k p) c -> p k c", p=128)[:, :, c0:c0 + w])]
            jobs.append((name, fn))

        def job_rows(name, src2d, r0, nch):
            def fn(sl):
                dst = sl.t[:, 0:nch * 1024].rearrange("p (j c) -> p j c", j=nch)
                return [(dst, src2d[r0:r0 + nch * 128, :].rearrange("(j p) c -> p j c", p=128))]
            jobs.append((name, fn))

        def wcols(sl, w):
            return lambda k, a, b: V(sl.t[:, k * w + a:k * w + b], sl.bufs)

        def wrows(sl):
            return lambda j, a, b: V(sl.t[:, j * 1024 + a:j * 1024 + b], sl.bufs)

        def ffn_groups(nchunks):
            g = []
            c = 0
            while c < nchunks:
                n = min(4, nchunks - c)
                g.append((c, n))
                c += n
            return g

        for l in layers:
            for g in range(12):
                job_cols(f"ada{l}_{g}", adaw_d[l], g * 512, 512)
            if "gla" in parts:
                job_cols(f"gqk{l}", win_d[l], 0, 512)
                job_cols(f"gv{l}", win_d[l], 512, 512)
                job_cols(f"gr{l}", win_d[l], 1024, 512)
                job_rows(f"wog{l}", wout_d[l], 0, 4)
            if "ret" in parts:
                job_cols(f"rq{l}", win_d[l], 1552, 512)
                job_cols(f"rk{l}", win_d[l], 2064, 512)
                job_cols(f"rv{l}", win_d[l], 2576, 512)
                job_cols(f"rg{l}", win_d[l], 3088, 512)
                job_rows(f"wor{l}", wout_d[l], 512, 4)
            if "ffn" in parts:
                i = l // 2
                if l % 2 == 0:
                    for gi, (c0, n) in enumerate(ffn_groups(22)):
                        job_cols(f"w1_{l}_{gi}", w1_d[i], c0 * 128, n * 128)
                        job_cols(f"w3_{l}_{gi}", w3_d[i], c0 * 128, n * 128)
                        job_rows(f"w2_{l}_{gi}", w2_d[i], c0 * 128, n)
                else:
                    for e in range(NE):
                        for gi, (c0, n) in enumerate(ffn_groups(11)):
                            job_cols(f"w1_{l}_{e}_{gi}", m1_d[i, e], c0 * 128, n * 128)
                            job_cols(f"w3_{l}_{e}_{gi}", m3_d[i, e], c0 * 128, n * 128)
                            job_rows(f"w2_{l}_{e}_{gi}", m2_d[i, e], c0 * 128, n)

        S.dma("sp", CST.t[:], cst_d, writes=CST.bufs)
        S.dma("sp", NG.t[:], ng_d, writes=NG.bufs)
        S.dma("sp", CV.t[:], c_d, writes=CV.bufs)
        S.dma("sp", POSI.t[:], pos_d, writes=POSI.bufs)
        issue_pending()
        S.copy("dve", IDB(), ident)
        S.act(CVB(), CV(), AF.Silu)
        S.memset("dve", GAT(), 1.0)

        for i in range(NT):
            xb = Fp[(i % 2) * 2], Fp[(i % 2) * 2 + 1]
            for hh in range(2):
                S.dma("sp" if hh == 0 else "act", xb[hh].t[:], x_d[i * 128:(i + 1) * 128, hh * 512:(hh + 1) * 512],
                      writes=xb[hh].bufs)
                pt = P[hh]
                for j in range(4):
                    S.tr(V(pt.t[:, j * 128:(j + 1) * 128], pt.bufs), xb[hh](ALL, slice(j * 128, (j + 1) * 128)), ident,
                         signal=(j == 3))
                dst = V(X.t[:, hh * 4:(hh + 1) * 4, i * 128:(i + 1) * 128], [X.bufs[i]])
                src = V(pt.t[:].rearrange("p (j t) -> p j t", j=4), pt.bufs)
                S.copy("act" if hh == 0 else "dve", dst, src)

        if "ret" in parts:
            posf = V(SM.t[:, 0:NT], SM.bufs)
            S.copy("dve", posf, POSI())
            ang = Fp[4]
            angv = lambda i: V(ang.t[:, 0:1024].rearrange("p (i j) -> p i j", i=NT)[:, i, :], ang.bufs)
            two_pi = 2.0 * math.pi
            for which, shift in (("sin", 0.0), ("cos", math.pi / 2)):
                for half in range(2):
                    a = Fp[4 + half]
                    for ii in range(8):
                        i = half * 8 + ii
                        S.ts("dve", a(ALL, slice(ii * 64, (ii + 1) * 64)), C("invf"), V(SM.t[:, i:i + 1], SM.bufs),
                             shift, op0=ALU.mult, op1=ALU.add)
                    u = Fp[6]
                    ki = V(Hp[0].t[:].bitcast(I32)[:, 0:256], Hp[0].bufs)
                    ki2 = V(Hp[1].t[:].bitcast(I32)[:, 0:256], Hp[1].bufs)
                    S.ts("dve", u(), a(), 1.0 / two_pi, None, op0=ALU.mult)
                    for q4, kk in ((0, ki), (1, ki2)):
                        S.copy("dve", kk, u(ALL, slice(q4 * 256, (q4 + 1) * 256)))
                        S.copy("dve", u(ALL, slice(q4 * 256, (q4 + 1) * 256)), kk)
                    C1 = 6.28125
                    C2 = two_pi - C1
                    S.stt(a(), u(), -C1, a(), ALU.mult, ALU.add)
                    S.stt(a(), u(), -C2, a(), ALU.mult, ALU.add)
                    m = Fp[7]
                    S.ts("dve", m(), a(), math.pi, -two_pi, op0=ALU.is_gt, op1=ALU.mult)
                    S.tt("dve", a(), a(), m(), ALU.add)
                    S.ts("dve", m(), a(), -math.pi, two_pi, op0=ALU.is_lt, op1=ALU.mult)
                    S.tt("dve", a(), a(), m(), ALU.add)
                    S.ts("dve", a(), a(), math.pi, -math.pi, op0=ALU.min, op1=ALU.max)
                    col = 64 if which == "sin" else 0
                    dst = V(ROT.t[:, half * 8:(half + 1) * 8, col:col + 64], ROT.bufs)
                    S.act(dst, V(a.t[:].rearrange("p (i j) -> p i j", i=8), a.bufs), AF.Sin)

        def ada_phase(l):
            S.dma("sp", MODB.t[:], adab_d[l], writes=MODB.bufs)
            prow = P[5]
            pmod = P[6]
            for g in range(12):
                sl = wget(f"ada{l}_{g}")
                wc = wcols(sl, 512)
                for k in range(8):
                    S.mm(V(prow.t[0:1, :], prow.bufs), CVB(ALL, slice(k, k + 1)), wc(k, 0, 512),
                         start=(k == 0), stop=(k == 7))
                wrel(f"ada{l}_{g}")
                rb = Fp[g % 2]
                S.copy("act", rb(slice(0, 1), ALL), V(prow.t[0:1, :], prow.bufs))
                for j in range(4):
                    cc = g * 4 + j
                    S.mm(V(pmod.t[:, cc:cc + 1], pmod.bufs), rb(slice(0, 1), slice(j * 128, (j + 1) * 128)),
                         C("ones", 0, 1, rows=slice(0, 1)), start=True, stop=True, signal=(j == 3))
            S.tt("dve", MOD(), V(pmod.t[:, 0:48], pmod.bufs), MODB(), ALU.add)
            S.stt(AB(ALL, slice(0, 8)), MOD(ALL, slice(8, 16)), 1.0, NG(ALL, slice(l * 8, l * 8 + 8)), ALU.add, ALU.mult)
            S.stt(AB(ALL, slice(8, 16)), MOD(ALL, slice(32, 40)), 1.0,
                  NG(ALL, slice(DEPTH * 8 + l * 8, DEPTH * 8 + l * 8 + 8)), ALU.add, ALU.mult)

        def norm_stats(s, pss, sqa, sqb, lnv, rstd):
            sl = slice(s * 512, (s + 1) * 512)
            subs = list(range(4 * s, 4 * s + 4))
            for k in range(8):
                q = sqa if k % 2 == 0 else sqb
                S.act(q(), X(ALL, k, sl, sub=subs), AF.Square)
                S.mm(pss(), ones, q(), start=(k == 0), stop=(k == 7))
            S.act(lnv(), pss(), AF.Ln, bias=EPS, scale=1.0 / D)
            S.act(rstd(), lnv(), AF.Exp, scale=-0.5)

        def norm_phase(a_off, b_off):
            for s in range(4):
                sl = slice(s * 512, (s + 1) * 512)
                subs = list(range(4 * s, 4 * s + 4))
                pss = P[s % 2]
                rstd = Fp[3]
                norm_stats(s, pss, Fp[0], Fp[1], Fp[2], rstd)
                for k in range(8):
                    t = Fp[4 + (k % 2)]
                    S.stt(t(), X(ALL, k, sl, sub=subs), AB(ALL, slice(a_off + k, a_off + k + 1)), rstd(), ALU.mult, ALU.mult)
                    S.act(HT(ALL, k, sl, sub=subs), t(), AF.Identity, bias=MOD(ALL, slice(b_off + k, b_off + k + 1)), scale=1.0)

        def out_proj_update(i, wo, oT, g_off, pys):
            wr = wrows(wo)
            tsl = slice(i * 128, (i + 1) * 128)
            for m in range(8):
                py = pys[m // 4]
                o = V(py.t[:, (m % 4) * 128:(m % 4 + 1) * 128], py.bufs)
                for j in range(4):
                    S.mm(o, wr(j, m * 128, (m + 1) * 128), oT(ALL, slice(j * 128, (j + 1) * 128)),
                         start=(j == 0), stop=(j == 3))
                S.stt(X(ALL, m, tsl, sub=i), o, MOD(ALL, slice(g_off + m, g_off + m + 1)), X(ALL, m, tsl, sub=i),
                      ALU.mult, ALU.add)

        def gla_pass(l):
            S.dma("pool", WGA.t[:], win_d[l].rearrange("(k p) c -> p k c", p=128)[:, :, 1536:1552], writes=WGA.bufs)
            S.dma("pool", WAL.t[0:17, :], wal_d[l], writes=WAL.bufs)
            S.dma("sp", BCP.t[:], bc_d[l], writes=BCP.bufs)
            wqk = wcols(wget(f"gqk{l}"), 512)
            wv = wcols(wget(f"gv{l}"), 512)
            wr_ = wcols(wget(f"gr{l}"), 512)
            wo = wget(f"wog{l}")
            Sst = Fp[7]
            SbA, SbB = Hp[11], Hp[12]
            S.memset("dve", Sst(ALL, slice(0, 256)), 0.0)
            S.memset("dve", SbA(ALL, slice(0, 256)), 0.0)
            for i in range(NT):
                tsl = slice(i * 128, (i + 1) * 128)
                hT = lambda k: HT(ALL, k, tsl, sub=i)
                pga = V(P[0].t[0:16, 0:128], P[0].bufs)
                for k in range(8):
                    S.mm(pga, V(WGA.t[:, k, :], WGA.bufs), hT(k), start=(k == 0), stop=(k == 7))
                S.copy("act", GAT(slice(0, 16), ALL), pga)
                pqk, pv, pr = P[3], P[4], P[5]
                for (pp, w) in ((pv, wv), (pr, wr_), (pqk, wqk)):
                    for k in range(8):
                        S.mm(pp(), hT(k), w(k, 0, 512), start=(k == 0), stop=(k == 7))
                if _DBG_STOP == 1:
                    continue
                pz = V(P[1].t[:, 0:256], P[1].bufs)
                S.mm(pz, GAT(slice(0, 17), ALL), WAL(slice(0, 17), ALL))
                if _DBG_STOP == 2:
                    continue
                e_, sp_ = V(Fp[0].t[:, 0:256], Fp[0].bufs), V(Fp[0].t[:, 256:512], Fp[0].bufs)
                S.act(e_, pz, AF.Exp, scale=-1.0)
                S.act(sp_, e_, AF.Ln, bias=1.0, scale=1.0)
                if _DBG_STOP == 3:
                    continue
                pb = V(P[2].t[:, 0:256], P[2].bufs)
                pd = V(P[2].t[:, 256:512], P[2].bufs)
                S.mm(pb, C("tri"), sp_)
                S.mm(pd, C("dm"), sp_)
                pbl = V(P[0].t[:, 128:132], P[0].bufs)
                for p_ in range(2):
                    S.mm(V(P[0].t[:, 128 + 2 * p_:130 + 2 * p_], P[0].bufs),
                         V(Fp[0].t[:, 256 + p_ * 128:256 + (p_ + 1) * 128], Fp[0].bufs), C("cind"))
                eb, enb = V(Fp[1].t[:, 0:256], Fp[1].bufs), V(Fp[1].t[:, 256:512], Fp[1].bufs)
                eD = V(Fp[2].t[:, 0:256], Fp[2].bufs)
                dec = V(SM.t[:, 32:36], SM.bufs)
                S.act(eb, pb, AF.Exp)
                S.act(enb, pb, AF.Exp, scale=-1.0)
                S.act(eD, pd, AF.Exp)
                S.act(dec, pbl, AF.Exp)
                if _DBG_STOP == 4:
                    continue
                vbf = Hp[0]
                S.copy("act", vbf(), pv())
                sg = Fp[3]
                S.act(sg(), pr(), AF.Silu)
                S.tt("dve", sg(), sg(), BCP(ALL, slice(0, 512)), ALU.mult)
                if _DBG_STOP == 5:
                    continue
                QKa, QKb, ku = Hp[1], Hp[2], Hp[3]
                pq_ = V(pqk.t[:, 0:256], pqk.bufs)
                pk_ = V(pqk.t[:, 256:512], pqk.bufs)
                S.stt(QKa(ALL, slice(0, 256)), pq_, 0.125, eb, ALU.mult, ALU.mult)
                S.tt("dve", QKa(ALL, slice(256, 512)), pk_, enb, ALU.mult)
                S.stt(QKb(ALL, slice(0, 256)), pq_, 0.125, enb, ALU.mult, ALU.mult)
                S.tt("dve", QKb(ALL, slice(256, 512)), pk_, eb, ALU.mult)
                hm = lambda j: C("hm", j, j + 1)
                for c in range(2):
                    S.stt(ku(ALL, slice(c * 256, (c + 1) * 256)), pk_, hm(c), eD, ALU.mult, ALU.mult)
                if _DBG_STOP == 6:
                    continue
                ptb = pbf(P[6])
                for src_i, src in enumerate((QKa, QKb)):
                    for j in range(4):
                        jj = src_i * 4 + j
                        S.tr(V(ptb.ap[:, jj * 128:(jj + 1) * 128], ptb.bufs), src(ALL, slice(j * 128, (j + 1) * 128)), IDB(),
                             signal=(j == 3))
                KT, QF, QB, QM0, QM1 = Hp[4], Hp[5], Hp[6], Hp[10], Hp[13]
                S.copy("act", KT(ALL, slice(0, 256)), V(ptb.ap[:, 256:512], ptb.bufs))
                S.copy("act", KT(ALL, slice(256, 512)), V(ptb.ap[:, 768:1024], ptb.bufs))
                c0b = V(C("c0").ap.unsqueeze(1).broadcast_to([128, 2, 128]), CST.bufs)
                c1b = V(C("c1").ap.unsqueeze(1).broadcast_to([128, 2, 128]), CST.bufs)
                r2 = lambda v: V(v.ap.rearrange("p (a t) -> p a t", a=2), v.bufs)
                qfT = V(ptb.ap[:, 0:256], ptb.bufs)
                qbT = V(ptb.ap[:, 512:768], ptb.bufs)
                for hl in range(2):
                    hsl = slice(hl * 256, (hl + 1) * 256)
                    S.ts("dve", QF(ALL, hsl), qfT, hm(hl), None, op0=ALU.mult)
                    S.ts("dve", QB(ALL, hsl), qbT, hm(hl), None, op0=ALU.mult)
                    S.stt(r2(QM0(ALL, hsl)), r2(qfT), hm(hl), c0b, ALU.mult, ALU.mult)
                    S.stt(r2(QM1(ALL, hsl)), r2(qfT), hm(hl), c1b, ALU.mult, ALU.mult)
                if _DBG_STOP == 8:
                    continue
                psf, psb = P[6], P[1]
                for h in range(4):
                    pr2, hl = h // 2, h % 2
                    S.mm(V(psf.t[:, h * 128:(h + 1) * 128], psf.bufs), KT(ALL, slice(pr2 * 128, (pr2 + 1) * 128)),
                         QF(ALL, slice(hl * 256 + pr2 * 128, hl * 256 + (pr2 + 1) * 128)), signal=(h == 3))
                    S.mm(V(psb.t[:, h * 128:(h + 1) * 128], psb.bufs), KT(ALL, slice(256 + pr2 * 128, 256 + (pr2 + 1) * 128)),
                         QB(ALL, slice(hl * 256 + pr2 * 128, hl * 256 + (pr2 + 1) * 128)), signal=(h == 3))
                m1b = V(C("m1").ap.unsqueeze(1).broadcast_to([128, 4, 128]), CST.bufs)
                m2b = V(C("m2").ap.unsqueeze(1).broadcast_to([128, 4, 128]), CST.bufs)
                r3 = lambda v: V(v.ap.rearrange("p (h t) -> p h t", h=4), v.bufs)
                mm1, mm2, scT = Fp[4], Fp[5], Hp[7]
                S.tt("dve", r3(mm1()), r3(psf()), m1b, ALU.mult)
                S.tt("dve", r3(mm2()), r3(psb()), m2b, ALU.mult)
                S.tt("dve", scT(), mm1(), mm2(), ALU.add)
                if _DBG_STOP == 9:
                    continue
                pu = P[2]
                po = P[4]
                for h in range(4):
                    S.mm(V(po.t[:, h * 128:(h + 1) * 128], po.bufs), scT(ALL, slice(h * 128, (h + 1) * 128)),
                         vbf(ALL, slice(h * 128, (h + 1) * 128)), start=(h == 0), stop=False, signal=False)
                for c in range(2):
                    Sb = SbA if c == 0 else SbB
                    QM = QM0 if c == 0 else QM1
                    for h in range(4):
                        pr2, hl = h // 2, h % 2
                        S.mm(V(po.t[:, h * 128:(h + 1) * 128], po.bufs),
                             QM(ALL, slice(hl * 256 + pr2 * 128, hl * 256 + (pr2 + 1) * 128)),
                             Sb(ALL, slice(pr2 * 128, (pr2 + 1) * 128)),
                             start=False, stop=(c == 1 and h == 3), signal=(c == 1 and h == 3))
                    for pr2 in range(2):
                        S.mm(V(pu.t[:, pr2 * 256:(pr2 + 1) * 256], pu.bufs),
                             ku(ALL, slice(c * 256 + pr2 * 128, c * 256 + (pr2 + 1) * 128)),
                             vbf(ALL, slice(pr2 * 256, (pr2 + 1) * 256)), signal=(pr2 == 1))
                    for pr2 in range(2):
                        for hl in range(2):
                            rows = slice(hl * 64, hl * 64 + 64)
                            sv = Sst(rows, slice(pr2 * 128, (pr2 + 1) * 128))
                            S.stt(sv, sv, V(SM.t[rows, 32 + 2 * pr2 + c:33 + 2 * pr2 + c], SM.bufs),
                                  V(pu.t[rows, pr2 * 256 + hl * 128:pr2 * 256 + (hl + 1) * 128], pu.bufs),
                                  ALU.mult, ALU.add)
                    Sn = SbB if c == 0 else SbA
                    S.copy("act", Sn(ALL, slice(0, 256)), Sst(ALL, slice(0, 256)))
                if _DBG_STOP == 10:
                    continue
                sq = Fp[4]
                S.act(sq(), po(), AF.Square)
                ss = V(SM.t[:, 36:40], SM.bufs)
                S.reduce(ss, r3(sq()), ALU.add)
                lv = V(SM.t[:, 40:44], SM.bufs)
                rs = V(SM.t[:, 44:48], SM.bufs)
                S.act(lv, ss, AF.Ln, bias=EPS, scale=1.0 / 128)
                S.act(rs, lv, AF.Exp, scale=-0.5)
                og = Hp[8]
                for h in range(4):
                    hs = slice(h * 128, (h + 1) * 128)
                    S.stt(og(ALL, hs), V(po.t[:, hs], po.bufs), V(SM.t[:, 44 + h:45 + h], SM.bufs), sg(ALL, hs),
                          ALU.mult, ALU.mult)
                if _DBG_STOP == 11:
                    continue
                pt2 = pbf(P[6], 512)
                for h in range(4):
                    S.tr(V(pt2.ap[:, h * 128:(h + 1) * 128], pt2.bufs), og(ALL, slice(h * 128, (h + 1) * 128)), IDB(),
                         signal=(h == 3))
                ogT = Hp[9]
                S.copy("act", ogT(), pt2)
                out_proj_update(i, wo, ogT, 16, (P[3], P[5]))
            for n_ in (f"gqk{l}", f"gv{l}", f"gr{l}", f"wog{l}"):
                wrel(n_)

        def ret_pass(l):
            if "gla" not in parts:
                S.dma("sp", BCP.t[:], bc_d[l], writes=BCP.bufs)
            wq = wcols(wget(f"rq{l}"), 512)
            wk = wcols(wget(f"rk{l}"), 512)
            wv = wcols(wget(f"rv{l}"), 512)
            wg = wcols(wget(f"rg{l}"), 512)
            wo = wget(f"wor{l}")
            R = Fp[7]
            RbA, RbB = Hp[11], Hp[12]
            S.memset("dve", R(), 0.0)
            S.memset("dve", RbA(), 0.0)
            r3 = lambda v: V(v.ap.rearrange("p (h t) -> p h t", h=4), v.bufs)
            r4 = lambda v: V(v.ap.rearrange("p (h a j) -> p h a j", h=4, a=2), v.bufs)
            for i in range(NT):
                tsl = slice(i * 128, (i + 1) * 128)
                hT = lambda k: HT(ALL, k, tsl, sub=i)
                pq, pk, pv, pg = P[0], P[1], P[2], P[3]
                for (pp, w) in ((pq, wq), (pk, wk), (pv, wv), (pg, wg)):
                    for k in range(8):
                        S.mm(pp(), hT(k), w(k, 0, 512), start=(k == 0), stop=(k == 7))
                vbf = Hp[0]
                S.copy("act", vbf(), pv())
                sg = Fp[3]
                S.act(sg(), pg(), AF.Silu)
                cosb = V(ROT.t[:, i, 0:64].unsqueeze(1).unsqueeze(1).broadcast_to([128, 4, 2, 64]), ROT.bufs)
                sinb = V(ROT.t[:, i, 64:128].unsqueeze(1).broadcast_to([128, 4, 64]), ROT.bufs)
                qr, kr, kd = Hp[1], Hp[2], Hp[3]
                for (pp, dst) in ((pq, qr), (pk, kr)):
                    a_, b_ = Fp[0], Fp[1]
                    S.tt("dve", r4(a_()), r4(pp()), cosb, ALU.mult)
                    p4 = r4(pp())
                    b4 = r4(b_())
                    S.tt("dve", V(b4.ap[:, :, 0, :], b_.bufs), V(p4.ap[:, :, 1, :], pp.bufs), sinb, ALU.mult)
                    S.tt("dve", V(b4.ap[:, :, 1, :], b_.bufs), V(p4.ap[:, :, 0, :], pp.bufs), sinb, ALU.mult)
                    a4 = r4(a_())
                    d4 = r4(dst())
                    S.tt("dve", V(d4.ap[:, :, 0, :], dst.bufs), V(a4.ap[:, :, 0, :], a_.bufs), V(b4.ap[:, :, 0, :], b_.bufs),
                         ALU.subtract)
                    S.tt("dve", V(d4.ap[:, :, 1, :], dst.bufs), V(a4.ap[:, :, 1, :], a_.bufs), V(b4.ap[:, :, 1, :], b_.bufs),
                         ALU.add)
                kdc = (Hp[3], Hp[13])
                for c in range(2):
                    for h in range(4):
                        hs = slice(h * 128, (h + 1) * 128)
                        S.ts("dve", kdc[c](ALL, hs), kr(ALL, hs), C("kdec", 4 * c + h, 4 * c + h + 1), None, op0=ALU.mult)
                ptq = pbf(P[4], 512)
                ptk = V(PB.t[:, 512:1024], PB.bufs)
                for h in range(4):
                    hs = slice(h * 128, (h + 1) * 128)
                    S.tr(V(ptq.ap[:, hs], ptq.bufs), qr(ALL, hs), IDB(), signal=(h == 3))
                for h in range(4):
                    hs = slice(h * 128, (h + 1) * 128)
                    S.tr(V(ptk.ap[:, hs], ptk.bufs), kr(ALL, hs), IDB(), signal=(h == 3))
                qT, kT, qd0, qd1 = Hp[4], Hp[5], Hp[6], Hp[7]
                S.copy("act", qT(), ptq)
                S.copy("act", kT(), ptk)
                S.tt("dve", qd0(), ptq, C("qd0"), ALU.mult)
                S.tt("dve", qd1(), ptq, C("qd1"), ALU.mult)
                ps = P[5]
                for h in range(4):
                    hs = slice(h * 128, (h + 1) * 128)
                    S.mm(V(ps.t[:, hs], ps.bufs), kT(ALL, hs), qT(ALL, hs), signal=(h == 3))
                scT = Hp[8]
                S.tt("dve", scT(), ps(), C("dret"), ALU.mult)
                po = P[6]
                for h in range(4):
                    hs = slice(h * 128, (h + 1) * 128)
                    S.mm(V(po.t[:, hs], po.bufs), scT(ALL, hs), vbf(ALL, hs), start=(h == 0), stop=False, signal=False)
                for c in range(2):
                    Rb = RbA if c == 0 else RbB
                    qd = qd0 if c == 0 else qd1
                    for h in range(4):
                        hs = slice(h * 128, (h + 1) * 128)
                        S.mm(V(po.t[:, hs], po.bufs), qd(ALL, hs), Rb(ALL, hs), start=False, stop=(c == 1 and h == 3),
                             signal=(c == 1 and h == 3))
                    pu = P[4]
                    for h in range(4):
                        hs = slice(h * 128, (h + 1) * 128)
                        S.mm(V(pu.t[:, hs], pu.bufs), kdc[c](ALL, hs), vbf(ALL, hs), signal=(h == 3))
                    for h in range(4):
                        hs = slice(h * 128, (h + 1) * 128)
                        S.stt(R(ALL, hs), R(ALL, hs), float(GAMMA[h] ** 64), V(pu.t[:, hs], pu.bufs), ALU.mult, ALU.add)
                    Rn = RbB if c == 0 else RbA
                    S.copy("act", Rn(), R())
                s1 = V(SM.t[:, 36:40], SM.bufs)
                s2 = V(SM.t[:, 40:44], SM.bufs)
                S.reduce(s1, r3(po()), ALU.add)
                sq = Fp[0]
                S.act(sq(), po(), AF.Square)
                S.reduce(s2, r3(sq()), ALU.add)
                mean = V(SM.t[:, 44:48], SM.bufs)
                var = V(SM.t[:, 48:52], SM.bufs)
                rs = V(SM.t[:, 52:56], SM.bufs)
                nmr = V(SM.t[:, 56:60], SM.bufs)
                S.ts("dve", mean, s1, 1.0 / 128, None, op0=ALU.mult)
                S.tt("dve", var, mean, mean, ALU.mult)
                S.stt(var, s2, 1.0 / 128, var, ALU.mult, ALU.subtract)
                S.ts("dve", var, var, 0.0, None, op0=ALU.max)
                S.act(var, var, AF.Ln, bias=EPS, scale=1.0)
                S.act(rs, var, AF.Exp, scale=-0.5)
                S.stt(nmr, mean, -1.0, rs, ALU.mult, ALU.mult)
                on = Fp[1]
                for h in range(4):
                    hs = slice(h * 128, (h + 1) * 128)
                    S.act(on(ALL, hs), V(po.t[:, hs], po.bufs), AF.Identity, bias=V(SM.t[:, 56 + h:57 + h], SM.bufs),
                          scale=V(SM.t[:, 52 + h:53 + h], SM.bufs))
                S.tt("dve", on(), on(), BCP(ALL, slice(512, 1024)), ALU.mult)
                S.tt("dve", on(), on(), BCP(ALL, slice(1024, 1536)), ALU.add)
                orr = Hp[9]
                S.tt("dve", orr(), on(), sg(), ALU.mult)
                pt2 = pbf(P[4], 512)
                for h in range(4):
                    hs = slice(h * 128, (h + 1) * 128)
                    S.tr(V(pt2.ap[:, hs], pt2.bufs), orr(ALL, hs), IDB(), signal=(h == 3))
                orT = Hp[10]
                S.copy("act", orT(), pt2)
                out_proj_update(i, wo, orT, 16, (P[1], P[2]))
            for n_ in (f"rq{l}", f"rk{l}", f"rv{l}", f"rg{l}", f"wor{l}"):
                wrel(n_)

        def ffn_group(names, nch, gate):
            n1, n3, n2 = names
            w1 = wcols(wget(n1), nch * 128)
            w3 = wcols(wget(n3), nch * 128)
            w2 = wrows(wget(n2))
            for u in range(8):
                usl = slice(u * 256, (u + 1) * 256)
                subs = [2 * u, 2 * u + 1]
                py = P[0:4]

                def up(j):
                    ph = P[4 + (j % 3)]
                    for (off, w) in ((0, w1), (256, w3)):
                        for k in range(8):
                            S.mm(V(ph.t[:, off:off + 256], ph.bufs), w(k, j * 128, (j + 1) * 128), HT(ALL, k, usl, sub=subs),
                                 start=(k == 0), stop=(k == 7))
                    s_ = Fp[j % 4]
                    a_ = Hp[j % 4]
                    S.act(s_(ALL, slice(0, 256)), V(ph.t[:, 0:256], ph.bufs), AF.Silu)
                    if gate is not None:
                        S.tt("dve", s_(ALL, slice(0, 256)), s_(ALL, slice(0, 256)), gate(usl), ALU.mult)
                    S.tt("dve", a_(ALL, slice(0, 256)), V(ph.t[:, 256:512], ph.bufs), s_(ALL, slice(0, 256)), ALU.mult)

                def down(j):
                    a_ = Hp[j % 4]
                    for m in range(8):
                        o = V(py[m // 2].t[:, (m % 2) * 256:(m % 2 + 1) * 256], py[m // 2].bufs)
                        S.mm(o, w2(j, m * 128, (m + 1) * 128), a_(ALL, slice(0, 256)), start=(j == 0 and m % 2 == 0),
                             stop=(j == nch - 1 and m % 2 == 1), signal=(j == nch - 1))

                up(0)
                for j in range(nch):
                    if j + 1 < nch:
                        up(j + 1)
                    down(j)
                for m in range(8):
                    o = V(py[m // 2].t[:, (m % 2) * 256:(m % 2 + 1) * 256], py[m // 2].bufs)
                    S.stt(X(ALL, m, usl, sub=subs), o, MOD(ALL, slice(40 + m, 41 + m)), X(ALL, m, usl, sub=subs),
                          ALU.mult, ALU.add)
            for n_ in names:
                wrel(n_)

        def dense_ffn(l):
            for gi, (c0, n) in enumerate(ffn_groups(22)):
                ffn_group((f"w1_{l}_{gi}", f"w3_{l}_{gi}", f"w2_{l}_{gi}"), n, None)

        def moe_ffn(l):
            i_ = l // 2
            S.dma("pool", RW.t[:], rw_d[i_].rearrange("(k p) e -> p k e", p=128), writes=RW.bufs)
            for i in range(NT):
                tsl = slice(i * 128, (i + 1) * 128)
                pl = V(P[4 + (i % 2)].t[:, 0:8], P[4 + (i % 2)].bufs)
                for k in range(8):
                    S.mm(pl, HT(ALL, k, tsl, sub=i), V(RW.t[:, k, :], RW.bufs), start=(k == 0), stop=(k == 7))
                sm = lambda a, b: V(SM.t[:, a:b], SM.bufs)
                lg, m1_, eq, l2, m2_, selm, ex, nm1, ssum = (sm(0, 8), sm(8, 9), sm(16, 24), sm(24, 32), sm(9, 10),
                                                              sm(32, 40), sm(40, 48), sm(10, 11), sm(11, 12))
                S.copy("dve", lg, pl)
                S.reduce(m1_, lg, ALU.max)
                S.ts("dve", eq, lg, m1_, None, op0=ALU.is_equal)
                S.stt(l2, eq, -1e30, lg, ALU.mult, ALU.add)
                S.reduce(m2_, l2, ALU.max)
                S.ts("dve", selm, lg, m2_, None, op0=ALU.is_ge)
                S.ts("dve", nm1, m1_, -1.0, None, op0=ALU.mult)
                S.act(ex, lg, AF.Exp, bias=nm1, scale=1.0)
                S.tt("dve", ex, ex, selm, ALU.mult)
                S.reduce(ssum, ex, ALU.add)
                S.op("dve", lambda h: h.reciprocal(out=ssum.ap, in_=ssum.ap), reads=ssum.bufs, writes=ssum.bufs)
                S.ts("dve", V(GATES.t[:, i, :], GATES.bufs), ex, ssum, None, op0=ALU.mult)
            GB = [Fp[4], Fp[5], Fp[6], Fp[7]]
            if debug:
                S.dma("sp", dbg_d[:, 0:128], GATES.t[:].rearrange("p i e -> p (i e)"), reads=GATES.bufs)
            for e in range(NE):
                for s in range(4):
                    pg_ = P[4 + (s % 2)]
                    for j in range(4):
                        i = s * 4 + j
                        gb = GBL[i % 2]
                        S.ts("dve", gb(), ones, V(GATES.t[:, i, e:e + 1], GATES.bufs), None, op0=ALU.mult)
                        S.mm(V(pg_.t[:, j * 128:(j + 1) * 128], pg_.bufs), gb(), ident, signal=(j == 3))
                    S.copy("act", GB[s](), pg_())
                if debug and e == 3:
                    for s in range(4):
                        S.dma("sp", dbg_d[:, 128 + s * 512:128 + (s + 1) * 512], GB[s].t[:], reads=GB[s].bufs)
                gate = lambda usl: V(GB[usl.start // 512].t[:, usl.start % 512:usl.start % 512 + 256], GB[usl.start // 512].bufs)
                for gi, (c0, n) in enumerate(ffn_groups(11)):
                    ffn_group((f"w1_{l}_{e}_{gi}", f"w3_{l}_{e}_{gi}", f"w2_{l}_{e}_{gi}"), n, gate)

        for l in layers:
            ada_phase(l)
            if "gla" in parts or "ret" in parts:
                norm_phase(0, 0)
            if "gla" in parts:
                gla_pass(l)
            if "ret" in parts:
                ret_pass(l)
            if "ffn" in parts:
                norm_phase(8, 24)
                if l % 2 == 0:
                    dense_ffn(l)
                else:
                    moe_ffn(l)

        fg0 = 2 * DEPTH * 8
        for s in range(4):
            subs = list(range(4 * s, 4 * s + 4))
            rstd = Fp[3]
            if do_final:
                norm_stats(s, P[s % 2], Fp[0], Fp[1], Fp[2], rstd)
            for j in range(4):
                ti = s * 4 + j
                tsl = slice(ti * 128, (ti + 1) * 128)
                ob = (Fp[4], Fp[5]) if ti % 2 == 0 else (Fp[6], Fp[7])
                for k in range(8):
                    t = Hp[k % 2]
                    tv = V(t.t[:].bitcast(F32)[:, 0:128], t.bufs)
                    if do_final:
                        S.stt(tv, X(ALL, k, tsl, sub=ti), NG(ALL, slice(fg0 + k, fg0 + k + 1)),
                              rstd(ALL, slice(j * 128, (j + 1) * 128)), ALU.mult, ALU.mult)
                    else:
                        S.copy("dve", tv, X(ALL, k, tsl, sub=ti))
                    pt = P[2 + k // 4]
                    S.tr(V(pt.t[:, (k % 4) * 128:(k % 4 + 1) * 128], pt.bufs), tv, ident, signal=(k % 4 == 3))
                    if k % 4 == 3:
                        S.copy("act", ob[k // 4](), pt())
                for hh in range(2):
                    S.dma("sp" if hh == 0 else "act", y_d[ti * 128:(ti + 1) * 128, hh * 512:(hh + 1) * 512], ob[hh].t[:],
                          reads=ob[hh].bufs)
        S.wait_bufs("sp", Fp[4].bufs + Fp[5].bufs + Fp[6].bufs + Fp[7].bufs)
        assert state["next"] == len(jobs), (state["next"], len(jobs))
        build_program.stats = (S.nins, S.nwaits)
    return nc


def _prep_shared(inp):
    f = lambda a: np.ascontiguousarray(np.asarray(a, dtype=np.float32))
    sh = {}
    sh["cst"] = make_consts()
    sh["ada_w"] = f(inp["ada_w"])
    sh["ada_b_fm"] = f(np.asarray(inp["ada_b"], np.float32).reshape(DEPTH, 48, 128).transpose(0, 2, 1))
    ng = np.concatenate([
        np.asarray(inp["norm_mix_g"], np.float32).reshape(DEPTH, 8, 128).transpose(2, 0, 1).reshape(128, DEPTH * 8),
        np.asarray(inp["norm_ffn_g"], np.float32).reshape(DEPTH, 8, 128).transpose(2, 0, 1).reshape(128, DEPTH * 8),
        np.asarray(inp["final_g"], np.float32).reshape(8, 128).T], axis=1)
    sh["norm_g_fm"] = f(ng)
    sh["w_in"] = f(inp["w_in"])
    sh["w_alpha_aug"] = f(np.concatenate([np.asarray(inp["gla_w_alpha"], np.float32),
                                          np.asarray(inp["gla_b_alpha"], np.float32)[:, None, :]], axis=1))
    bc = np.concatenate([np.asarray(inp["gla_norm_g"], np.float32), np.asarray(inp["ret_gn_g"], np.float32),
                         np.asarray(inp["ret_gn_b"], np.float32)], axis=1)
    sh["bc_params"] = f(np.broadcast_to(bc[:, None, :], (DEPTH, 128, 1536)))
    sh["w_out"] = f(inp["w_out"])
    for k_ in ("ffn_w1", "ffn_w3", "ffn_w2", "router_w", "moe_w1", "moe_w3", "moe_w2"):
        sh[k_] = f(inp[k_])
    return sh


def _prep_core(inp, b):
    d = {}
    d["x"] = np.ascontiguousarray(np.asarray(inp["x"][b], np.float32))
    d["cvec"] = np.ascontiguousarray(np.asarray(inp["c"][b], np.float32).reshape(8, 128).T)
    d["pos"] = np.ascontiguousarray(np.asarray(inp["positions"][b]).astype(np.int32).reshape(NT, 128).T)
    return d


_CACHE = {}


def kernel(**inputs):
    key = "full"
    if key not in _CACHE:
        _CACHE[key] = build_program()
    nc = _CACHE[key]
    sh = _prep_shared(inputs)
    in_maps = []
    for b in range(8):
        d = dict(sh)
        d.update(_prep_core(inputs, b))
        in_maps.append(d)
    res = run_bass_kernel_spmd(nc, in_maps, core_ids=list(range(8)))
    out = np.stack([np.asarray(res.results[b]["y"], np.float32) for b in range(8)], axis=0)
    return out
```

```python
import math
from contextlib import ExitStack

import numpy as np
import concourse.bass as bass
import concourse.mybir as mybir
from concourse.bass_utils import run_bass_kernel_spmd

F32 = mybir.dt.float32
BF16 = mybir.dt.bfloat16
I32 = mybir.dt.int32
AF = mybir.ActivationFunctionType
ALU = mybir.AluOpType
AX = mybir.AxisListType

D = 1024
SEQ = 2048
NT = 16
DEPTH = 4
D_FF = 2816
NE = 8
D_FFE = 1408
EPS = 1e-6
ALL = slice(None)
import os as _os
_DBG_STOP = int(_os.environ.get('DBG_STOP', '0'))


class Buf:
    __slots__ = ("name", "w", "r", "excl")

    def __init__(self, name, excl=False):
        self.name = name
        self.w = None
        self.r = {}
        self.excl = excl


class V:
    __slots__ = ("ap", "bufs")

    def __init__(self, ap, bufs):
        self.ap = ap
        self.bufs = bufs


class T:
    def __init__(self, tensor, name, nsub=1, excl=False):
        self.t = tensor
        self.name = name
        self.bufs = [Buf(f"{name}.{i}", excl) for i in range(nsub)]

    def __call__(self, *idx, sub=None):
        ap = self.t[idx] if idx else self.t[:]
        if sub is None:
            bufs = self.bufs
        elif isinstance(sub, int):
            bufs = [self.bufs[sub]]
        else:
            bufs = [self.bufs[s] for s in sub]
        return V(ap, bufs)


class Eng:
    def __init__(self, key, h, sem):
        self.key = key
        self.h = h
        self.sem = sem
        self.seq = 0
        self.cnt = 0
        self.last = None
        self.last_inc = False
        self.miles = []
        self.seen = {}


class Sync:
    def __init__(self, nc, sems, dma_sems):
        self.nc = nc
        keys = [("pe", nc.tensor), ("act", nc.scalar), ("dve", nc.vector),
                ("pool", nc.gpsimd), ("sp", nc.sync)]
        self.E = {}
        for (k, h), sm in zip(keys, sems):
            self.E[k] = Eng(k, h, sm)
        self.dsem = dma_sems
        self.dval = {q: [0] * len(v) for q, v in dma_sems.items()}
        self.drr = {q: 0 for q in dma_sems}
        self.nwaits = 0
        self.nins = 0

    def _resolve(self, tok):
        if tok[0] == 'd':
            return ('d', tok[1], tok[2]), self.dsem[tok[1]][tok[2]], tok[3]
        e = self.E[tok[1]]
        n = tok[2]
        val = None
        for (sq, v) in reversed(e.miles):
            if sq >= n:
                val = v
            else:
                break
        if val is None:
            assert e.seq >= n and e.last is not None
            if not e.last_inc:
                e.cnt += 1
                e.last.then_inc(e.sem, 1)
                e.last_inc = True
                e.miles.append((e.seq, e.cnt))
            val = e.cnt
        return ('e', e.key), e.sem, val

    def _wait(self, eng, toks, raw=()):
        e = self.E[eng]
        need = {}
        for lst, same_ok in ((toks, True), (raw, False)):
            for t in lst:
                if t is None:
                    continue
                if t[0] == 'e' and t[1] == eng and eng == "pe":
                    continue
                sk, sh, v = self._resolve(t)
                if e.seen.get(sk, 0) >= v:
                    continue
                if sk not in need or need[sk][1] < v:
                    need[sk] = (sh, v)
        for sk, (sh, v) in need.items():
            e.h.wait_ge(sh, v)
            e.seen[sk] = v
            self.nwaits += 1

    @staticmethod
    def _deps(reads, writes):
        toks = []
        raw = []
        for b in reads:
            raw.append(b.w)
            if b.excl:
                toks.extend(b.r.values())
        for b in writes:
            toks.append(b.w)
            toks.extend(b.r.values())
        return toks, raw

    @staticmethod
    def _commit(tok, reads, writes):
        key = tok[:2] if tok[0] == 'e' else tok[:3]
        for b in reads:
            b.r[key] = tok
        for b in writes:
            b.w = tok
            b.r = {}

    def op(self, eng, fn, reads=(), writes=(), signal=None):
        e = self.E[eng]
        if signal is None:
            signal = eng != "pe"
        self._wait(eng, *self._deps(reads, writes))
        ins = fn(e.h)
        e.seq += 1
        self.nins += 1
        e.last = ins
        e.last_inc = False
        if signal:
            e.cnt += 1
            ins.then_inc(e.sem, 1)
            e.last_inc = True
            e.miles.append((e.seq, e.cnt))
            if len(e.miles) > 4096:
                e.miles = e.miles[-2048:]
        tok = ('e', eng, e.seq)
        self._commit(tok, reads, writes)
        return tok

    def dma(self, q, out, in_, reads=(), writes=(), **kw):
        e = self.E[q]
        i = self.drr[q]
        self.drr[q] = (i + 1) % len(self.dsem[q])
        toks, raw = self._deps(reads, writes)
        if self.dval[q][i] > 0:
            toks.append(('d', q, i, self.dval[q][i]))
        self._wait(q, toks, raw)
        ins = e.h.dma_start(out=out, in_=in_, **kw)
        ins.then_inc(self.dsem[q][i], 16)
        self.dval[q][i] += 16
        self.nins += 1
        tok = ('d', q, i, self.dval[q][i])
        self._commit(tok, reads, writes)
        return tok

    def wait_bufs(self, eng, bufs):
        toks = []
        for b in bufs:
            toks.append(b.w)
            toks.extend(b.r.values())
        self._wait(eng, toks)

    @staticmethod
    def _rb(*vs):
        out = []
        for v in vs:
            if isinstance(v, V):
                out.extend(v.bufs)
        return out

    @staticmethod
    def _a(v):
        return v.ap if isinstance(v, V) else v

    def mm(self, out, lhsT, rhs, start=True, stop=True, signal=None, **kw):
        if signal is None:
            signal = bool(stop)
        return self.op("pe", lambda h: h.matmul(out.ap, lhsT.ap, rhs.ap, start=start, stop=stop, **kw),
                       reads=self._rb(lhsT, rhs), writes=out.bufs, signal=signal)

    def tr(self, out, in_, ident, signal=True):
        return self.op("pe", lambda h: h.transpose(out.ap, in_.ap, ident.ap),
                       reads=self._rb(in_, ident), writes=out.bufs, signal=signal)

    def act(self, out, in_, func, bias=None, scale=None):
        kw = {}
        if bias is not None:
            kw["bias"] = self._a(bias)
        if scale is not None:
            kw["scale"] = self._a(scale)
        return self.op("act", lambda h: h.activation(out=out.ap, in_=in_.ap, func=func, **kw),
                       reads=self._rb(in_, bias, scale), writes=out.bufs)

    def tt(self, eng, out, in0, in1, op):
        return self.op(eng, lambda h: h.tensor_tensor(out=out.ap, in0=in0.ap, in1=in1.ap, op=op),
                       reads=self._rb(in0, in1), writes=out.bufs)

    def ts(self, eng, out, in0, s1, s2=None, op0=ALU.mult, op1=None):
        kw = {}
        if op1 is not None:
            kw["op1"] = op1
        return self.op(eng, lambda h: h.tensor_scalar(out=out.ap, in0=in0.ap, scalar1=self._a(s1),
                                                      scalar2=self._a(s2), op0=op0, **kw),
                       reads=self._rb(in0, s1, s2), writes=out.bufs)

    def stt(self, out, in0, scalar, in1, op0, op1):
        return self.op("dve", lambda h: h.scalar_tensor_tensor(out=out.ap, in0=in0.ap, scalar=self._a(scalar),
                                                               in1=in1.ap, op0=op0, op1=op1),
                       reads=self._rb(in0, scalar, in1), writes=out.bufs)

    def copy(self, eng, out, in_):
        if eng == "act":
            return self.op(eng, lambda h: h.copy(out=out.ap, in_=in_.ap), reads=in_.bufs, writes=out.bufs)
        return self.op(eng, lambda h: h.tensor_copy(out=out.ap, in_=in_.ap), reads=in_.bufs, writes=out.bufs)

    def reduce(self, out, in_, op, axis=AX.X):
        return self.op("dve", lambda h: h.tensor_reduce(out=out.ap, in_=in_.ap, axis=axis, op=op),
                       reads=in_.bufs, writes=out.bufs)

    def memset(self, eng, out, val):
        return self.op(eng, lambda h: h.memset(out.ap, val), writes=out.bufs)


class CL:
    off = {}
    n = 0

    @classmethod
    def add(cls, name, w):
        cls.off[name] = (cls.n, cls.n + w)
        cls.n += w


for _n, _w in [("ident", 128), ("ones", 128), ("tri", 128), ("dm", 128), ("cind", 2), ("m1", 128), ("m2", 128),
               ("dret", 512), ("qd0", 512), ("qd1", 512), ("kdec", 8), ("invf", 64), ("c0", 128), ("c1", 128), ("hm", 2)]:
    CL.add(_n, _w)

GAMMA = [1.0 - 2.0 ** (-5.0 - h) for h in range(4)]


def make_consts():
    c = np.zeros((128, CL.n), np.float64)

    def put(name, arr):
        a, b = CL.off[name]
        c[:, a:b] = np.asarray(arr, np.float64).reshape(128, b - a)

    s = np.arange(128)[:, None]
    t = np.arange(128)[None, :]
    same = (s // 64) == (t // 64)
    put("ident", np.eye(128))
    put("ones", np.ones((128, 128)))
    put("tri", np.where(same & (s <= t), -1.0 / 16, 0.0))
    put("dm", np.where(same & (s > t), -1.0 / 16, 0.0))
    put("cind", np.where((s // 64) == np.arange(2)[None, :], -1.0 / 16, 0.0))
    put("m1", np.where(same & (t >= s), 1.0, 0.0))
    put("m2", np.where(same & (t < s), 1.0, 0.0))
    dret = np.zeros((128, 4, 128))
    qd0 = np.zeros((128, 4, 128))
    qd1 = np.zeros((128, 4, 128))
    kdec = np.zeros((128, 8))
    tt = np.arange(128)
    for h in range(4):
        lg = math.log(GAMMA[h])
        dret[:, h, :] = np.where(same, np.exp(lg * np.abs(t - s)), 0.0) * 128 ** -0.5
        qd = np.exp(lg * ((tt % 64) + 1)) * 128 ** -0.5
        qd0[:, h, :] = np.where(tt < 64, qd, 0.0)[None, :]
        qd1[:, h, :] = np.where(tt >= 64, qd, 0.0)[None, :]
        kdec[:, h] = np.where(tt < 64, np.exp(lg * (63 - (tt % 64))), 0.0)
        kdec[:, 4 + h] = np.where(tt >= 64, np.exp(lg * (63 - (tt % 64))), 0.0)
    put("dret", dret)
    put("qd0", qd0)
    put("qd1", qd1)
    put("kdec", kdec)
    invf = 10000.0 ** (-np.arange(64) / 64.0)
    put("invf", np.broadcast_to(invf[None, :], (128, 64)))
    put("c0", np.broadcast_to((tt < 64)[None, :], (128, 128)))
    put("c1", np.broadcast_to((tt >= 64)[None, :], (128, 128)))
    put("hm", np.stack([tt < 64, tt >= 64], axis=1))
    return c.astype(np.float32)


def build_program(layers=(0, 1, 2, 3), do_final=True, parts=("gla", "ret", "ffn"), nslots=6, debug=False):
    nc = bass.Bass("TRN2", target_bir_lowering=False)

    def din(name, shape, dt=F32):
        return nc.dram_tensor(name, list(shape), dt, kind="ExternalInput").ap()

    x_d = din("x", [SEQ, D])
    c_d = din("cvec", [128, 8])
    pos_d = din("pos", [128, NT], I32)
    cst_d = din("cst", [128, CL.n])
    adaw_d = din("ada_w", [DEPTH, D, 6 * D])
    adab_d = din("ada_b_fm", [DEPTH, 128, 48])
    ng_d = din("norm_g_fm", [128, 2 * DEPTH * 8 + 8])
    win_d = din("w_in", [DEPTH, D, 3600])
    wal_d = din("w_alpha_aug", [DEPTH, 17, 256])
    bc_d = din("bc_params", [DEPTH, 128, 1536])
    wout_d = din("w_out", [DEPTH, D, D])
    w1_d = din("ffn_w1", [2, D, D_FF])
    w3_d = din("ffn_w3", [2, D, D_FF])
    w2_d = din("ffn_w2", [2, D_FF, D])
    rw_d = din("router_w", [2, D, NE])
    m1_d = din("moe_w1", [2, NE, D, D_FFE])
    m3_d = din("moe_w3", [2, NE, D, D_FFE])
    m2_d = din("moe_w2", [2, NE, D_FFE, D])
    y_d = nc.dram_tensor("y", [SEQ, D], F32, kind="ExternalOutput").ap()
    dbg_d = nc.dram_tensor("dbg", [128, 4096], F32, kind="ExternalOutput").ap() if debug else None

    with ExitStack() as st:
        def sb(name, shape, dt, nsub=1):
            return T(st.enter_context(nc.sbuf_tensor(name, list(shape), dt)), name, nsub)

        X = sb("X", [128, 8, SEQ], F32, NT)
        HT = sb("HT", [128, 8, SEQ], BF16, NT)
        CST = sb("CST", [128, CL.n], F32)
        SLOT = [sb(f"slot{i}", [128, 4096], BF16) for i in range(nslots)]
        Fp = [sb(f"F{i}", [128, 512], F32) for i in range(8)]
        Hp = [sb(f"H{i}", [128, 512], BF16) for i in range(20)]
        NG = sb("NG", [128, 2 * DEPTH * 8 + 8], F32)
        MOD = sb("MOD", [128, 48], F32)
        MODB = sb("MODB", [128, 48], F32)
        AB = sb("AB", [128, 16], F32)
        CV = sb("CV", [128, 8], F32)
        CVB = sb("CVB", [128, 8], BF16)
        IDB = sb("IDB", [128, 128], BF16)
        WGA = sb("WGA", [128, 8, 16], BF16)
        WAL = sb("WAL", [32, 256], BF16)
        GAT = sb("GAT", [32, 128], BF16)
        BCP = sb("BCP", [128, 1536], F32)
        ROT = sb("ROT", [128, NT, 128], F32)
        SM = sb("SM", [128, 64], F32)
        POSI = sb("POSI", [128, NT], I32)
        RW = sb("RW", [128, 8, 8], BF16)
        GATES = sb("GATES", [128, NT, 8], F32)
        P = [T(st.enter_context(nc.psum_tensor(f"P{i}", [128, 512], F32)), f"P{i}", excl=True) for i in range(7)]
        PB = T(st.enter_context(nc.psum_tensor("PB", [128, 1024], BF16)), "PB", excl=True)
        sems = [st.enter_context(nc.semaphore(f"s_{k}")) for k in ["pe", "act", "dve", "pool", "sp"]]
        dsems = {q: [st.enter_context(nc.semaphore(f"d_{q}{i}")) for i in range(12)] for q in ["sp", "act", "pool"]}
        S = Sync(nc, sems, dsems)

        def C(name, lo=None, hi=None, rows=ALL):
            a, b = CL.off[name]
            if lo is not None:
                a, b = a + lo, a + hi
            return CST(rows, slice(a, b))

        ident = C("ident")
        ones = C("ones")

        def pbf(p, w=1024):
            return V(PB.t[:, 0:w], PB.bufs)

        jobs = []
        job_slot = {}
        state = {"next": 0}
        free = list(range(nslots))

        def issue_pending():
            while free and state["next"] < len(jobs):
                name, fn = jobs[state["next"]]
                state["next"] += 1
                si = free.pop(0)
                sl = SLOT[si]
                for (o, i) in fn(sl):
                    S.dma("pool", o, i, writes=sl.bufs)
                job_slot[name] = si

        def wget(name):
            assert name in job_slot, f"weight job {name} not issued (ring too small)"
            return SLOT[job_slot[name]]

        def wrel(name):
            si = job_slot.pop(name)
            free.append(si)
            issue_pending()

        def job_cols(name, src2d, c0, w):
            def fn(sl):
                dst = sl.t[:, 0:8 * w].rearrange("p (k c) -> p k c", k=8)
                return [(dst, src2d.rearrange("(k p) c -> p k c", p=128)[:, :, c0:c0 + w])]
            jobs.append((name, fn))

        def job_rows(name, src2d, r0, nch):
            def fn(sl):
                dst = sl.t[:, 0:nch * 1024].rearrange("p (j c) -> p j c", j=nch)
                return [(dst, src2d[r0:r0 + nch * 128, :].rearrange("(j p) c -> p j c", p=128))]
            jobs.append((name, fn))

        def wcols(sl, w):
            return lambda k, a, b: V(sl.t[:, k * w + a:k * w + b], sl.bufs)

        def wrows(sl):
            return lambda j, a, b: V(sl.t[:, j * 1024 + a:j * 1024 + b], sl.bufs)

        def ffn_groups(nchunks):
            g = []
            c = 0
            while c < nchunks:
                n = min(4, nchunks - c)
                g.append((c, n))
                c += n
            return g

        for l in layers:
            for g in range(12):
                job_cols(f"ada{l}_{g}", adaw_d[l], g * 512, 512)
            if "gla" in parts:
                job_cols(f"gqk{l}", win_d[l], 0, 512)
                job_cols(f"gv{l}", win_d[l], 512, 512)
                job_cols(f"gr{l}", win_d[l], 1024, 512)
                job_rows(f"wog{l}", wout_d[l], 0, 4)
            if "ret" in parts:
                job_cols(f"rq{l}", win_d[l], 1552, 512)
                job_cols(f"rk{l}", win_d[l], 2064, 512)
                job_cols(f"rv{l}", win_d[l], 2576, 512)
                job_cols(f"rg{l}", win_d[l], 3088, 512)
                job_rows(f"wor{l}", wout_d[l], 512, 4)
            if "ffn" in parts:
                i = l // 2
                if l % 2 == 0:
                    for gi, (c0, n) in enumerate(ffn_groups(22)):
                        job_cols(f"w1_{l}_{gi}", w1_d[i], c0 * 128, n * 128)
                        job_cols(f"w3_{l}_{gi}", w3_d[i], c0 * 128, n * 128)
                        job_rows(f"w2_{l}_{gi}", w2_d[i], c0 * 128, n)
                else:
                    for e in range(NE):
                        for gi, (c0, n) in enumerate(ffn_groups(11)):
                            job_cols(f"w1_{l}_{e}_{gi}", m1_d[i, e], c0 * 128, n * 128)
                            job_cols(f"w3_{l}_{e}_{gi}", m3_d[i, e], c0 * 128, n * 128)
                            job_rows(f"w2_{l}_{e}_{gi}", m2_d[i, e], c0 * 128, n)

        S.dma("sp", CST.t[:], cst_d, writes=CST.bufs)
        S.dma("sp", NG.t[:], ng_d, writes=NG.bufs)
        S.dma("sp", CV.t[:], c_d, writes=CV.bufs)
        S.dma("sp", POSI.t[:], pos_d, writes=POSI.bufs)
        issue_pending()
        S.copy("dve", IDB(), ident)
        S.act(CVB(), CV(), AF.Silu)
        S.memset("dve", GAT(), 1.0)

        for i in range(NT):
            xb = Fp[(i % 2) * 2], Fp[(i % 2) * 2 + 1]
            for hh in range(2):
                S.dma("sp" if hh == 0 else "act", xb[hh].t[:], x_d[i * 128:(i + 1) * 128, hh * 512:(hh + 1) * 512],
                      writes=xb[hh].bufs)
                pt = P[hh]
                for j in range(4):
                    S.tr(V(pt.t[:, j * 128:(j + 1) * 128], pt.bufs), xb[hh](ALL, slice(j * 128, (j + 1) * 128)), ident,
                         signal=(j == 3))
                dst = V(X.t[:, hh * 4:(hh + 1) * 4, i * 128:(i + 1) * 128], [X.bufs[i]])
                src = V(pt.t[:].rearrange("p (j t) -> p j t", j=4), pt.bufs)
                S.copy("act" if hh == 0 else "dve", dst, src)

        if "ret" in parts:
            posf = V(SM.t[:, 0:NT], SM.bufs)
            S.copy("dve", posf, POSI())
            ang = Fp[4]
            angv = lambda i: V(ang.t[:, 0:1024].rearrange("p (i j) -> p i j", i=NT)[:, i, :], ang.bufs)
            two_pi = 2.0 * math.pi
            for which, shift in (("sin", 0.0), ("cos", math.pi / 2)):
                for half in range(2):
                    a = Fp[4 + half]
                    for ii in range(8):
                        i = half * 8 + ii
                        S.ts("dve", a(ALL, slice(ii * 64, (ii + 1) * 64)), C("invf"), V(SM.t[:, i:i + 1], SM.bufs),
                             shift, op0=ALU.mult, op1=ALU.add)
                    u = Fp[6]
                    ki = V(Hp[0].t[:].bitcast(I32)[:, 0:256], Hp[0].bufs)
                    ki2 = V(Hp[1].t[:].bitcast(I32)[:, 0:256], Hp[1].bufs)
                    S.ts("dve", u(), a(), 1.0 / two_pi, None, op0=ALU.mult)
                    for q4, kk in ((0, ki), (1, ki2)):
                        S.copy("dve", kk, u(ALL, slice(q4 * 256, (q4 + 1) * 256)))
                        S.copy("dve", u(ALL, slice(q4 * 256, (q4 + 1) * 256)), kk)
                    C1 = 6.28125
                    C2 = two_pi - C1
                    S.stt(a(), u(), -C1, a(), ALU.mult, ALU.add)
                    S.stt(a(), u(), -C2, a(), ALU.mult, ALU.add)
                    m = Fp[7]
                    S.ts("dve", m(), a(), math.pi, -two_pi, op0=ALU.is_gt, op1=ALU.mult)
                    S.tt("dve", a(), a(), m(), ALU.add)
                    S.ts("dve", m(), a(), -math.pi, two_pi, op0=ALU.is_lt, op1=ALU.mult)
                    S.tt("dve", a(), a(), m(), ALU.add)
                    S.ts("dve", a(), a(), math.pi, -math.pi, op0=ALU.min, op1=ALU.max)
                    col = 64 if which == "sin" else 0
                    dst = V(ROT.t[:, half * 8:(half + 1) * 8, col:col + 64], ROT.bufs)
                    S.act(dst, V(a.t[:].rearrange("p (i j) -> p i j", i=8), a.bufs), AF.Sin)

        def ada_phase(l):
            S.dma("sp", MODB.t[:], adab_d[l], writes=MODB.bufs)
            prow = P[5]
            pmod = P[6]
            for g in range(12):
                sl = wget(f"ada{l}_{g}")
                wc = wcols(sl, 512)
                for k in range(8):
                    S.mm(V(prow.t[0:1, :], prow.bufs), CVB(ALL, slice(k, k + 1)), wc(k, 0, 512),
                         start=(k == 0), stop=(k == 7))
                wrel(f"ada{l}_{g}")
                rb = Fp[g % 2]
                S.copy("act", rb(slice(0, 1), ALL), V(prow.t[0:1, :], prow.bufs))
                for j in range(4):
                    cc = g * 4 + j
                    S.mm(V(pmod.t[:, cc:cc + 1], pmod.bufs), rb(slice(0, 1), slice(j * 128, (j + 1) * 128)),
                         C("ones", 0, 1, rows=slice(0, 1)), start=True, stop=True, signal=(j == 3))
            S.tt("dve", MOD(), V(pmod.t[:, 0:48], pmod.bufs), MODB(), ALU.add)
            S.stt(AB(ALL, slice(0, 8)), MOD(ALL, slice(8, 16)), 1.0, NG(ALL, slice(l * 8, l * 8 + 8)), ALU.add, ALU.mult)
            S.stt(AB(ALL, slice(8, 16)), MOD(ALL, slice(32, 40)), 1.0,
                  NG(ALL, slice(DEPTH * 8 + l * 8, DEPTH * 8 + l * 8 + 8)), ALU.add, ALU.mult)

        def norm_stats(s, pss, sqa, sqb, lnv, rstd):
            sl = slice(s * 512, (s + 1) * 512)
            subs = list(range(4 * s, 4 * s + 4))
            for k in range(8):
                q = sqa if k % 2 == 0 else sqb
                S.act(q(), X(ALL, k, sl, sub=subs), AF.Square)
                S.mm(pss(), ones, q(), start=(k == 0), stop=(k == 7))
            S.act(lnv(), pss(), AF.Ln, bias=EPS, scale=1.0 / D)
            S.act(rstd(), lnv(), AF.Exp, scale=-0.5)

        def norm_phase(a_off, b_off):
            for s in range(4):
                sl = slice(s * 512, (s + 1) * 512)
                subs = list(range(4 * s, 4 * s + 4))
                pss = P[s % 2]
                rstd = Fp[3]
                norm_stats(s, pss, Fp[0], Fp[1], Fp[2], rstd)
                for k in range(8):
                    t = Fp[4 + (k % 2)]
                    S.stt(t(), X(ALL, k, sl, sub=subs), AB(ALL, slice(a_off + k, a_off + k + 1)), rstd(), ALU.mult, ALU.mult)
                    S.act(HT(ALL, k, sl, sub=subs), t(), AF.Identity, bias=MOD(ALL, slice(b_off + k, b_off + k + 1)), scale=1.0)

        def out_proj_update(i, wo, oT, g_off, pys):
            wr = wrows(wo)
            tsl = slice(i * 128, (i + 1) * 128)
            for half in range(2):
                py = pys[half]
                for mm_ in range(4):
                    m = half * 4 + mm_
                    o = V(py.t[:, mm_ * 128:(mm_ + 1) * 128], py.bufs)
                    for j in range(4):
                        S.mm(o, wr(j, m * 128, (m + 1) * 128), oT(ALL, slice(j * 128, (j + 1) * 128)),
                             start=(j == 0), stop=(j == 3), signal=(j == 3 and mm_ == 3))
                yield
                for mm_ in range(4):
                    m = half * 4 + mm_
                    o = V(py.t[:, mm_ * 128:(mm_ + 1) * 128], py.bufs)
                    S.stt(X(ALL, m, tsl, sub=i), o, MOD(ALL, slice(g_off + m, g_off + m + 1)), X(ALL, m, tsl, sub=i),
                          ALU.mult, ALU.add)
                yield

        def run_pipelined(tile_gen, depth=2):
            active = []
            nxt = 0
            while nxt < NT or active:
                if nxt < NT and len(active) < depth and all(a_[1] for a_ in active):
                    active.append([tile_gen(nxt), False])
                    nxt += 1
                for idx, a_ in enumerate(list(active)):
                    if a_[1] and idx > 0:
                        continue
                    try:
                        r = next(a_[0])
                        if r == 'B':
                            a_[1] = True
                    except StopIteration:
                        active.remove(a_)

        r3 = lambda v: V(v.ap.rearrange("p (h t) -> p h t", h=4), v.bufs)
        r2 = lambda v: V(v.ap.rearrange("p (a t) -> p a t", a=2), v.bufs)
        r4 = lambda v: V(v.ap.rearrange("p (h a j) -> p h a j", h=4, a=2), v.bufs)

        def gla_pass(l):
            S.dma("pool", WGA.t[:], win_d[l].rearrange("(k p) c -> p k c", p=128)[:, :, 1536:1552], writes=WGA.bufs)
            S.dma("pool", WAL.t[0:17, :], wal_d[l], writes=WAL.bufs)
            S.dma("sp", BCP.t[:], bc_d[l], writes=BCP.bufs)
            wqk = wcols(wget(f"gqk{l}"), 512)
            wv = wcols(wget(f"gv{l}"), 512)
            wr_ = wcols(wget(f"gr{l}"), 512)
            wo = wget(f"wog{l}")
            Sst = Fp[7]
            SbA, SbB = Hp[11], Hp[12]
            S.memset("dve", Sst(ALL, slice(0, 256)), 0.0)
            S.memset("dve", SbA(ALL, slice(0, 256)), 0.0)
            hm = lambda j: C("hm", j, j + 1)
            c0b = V(C("c0").ap.unsqueeze(1).broadcast_to([128, 2, 128]), CST.bufs)
            c1b = V(C("c1").ap.unsqueeze(1).broadcast_to([128, 2, 128]), CST.bufs)
            m1b = V(C("m1").ap.unsqueeze(1).broadcast_to([128, 4, 128]), CST.bufs)
            m2b = V(C("m2").ap.unsqueeze(1).broadcast_to([128, 4, 128]), CST.bufs)

            def tile(i):
                par = i % 2
                tsl = slice(i * 128, (i + 1) * 128)
                hT = lambda k: HT(ALL, k, tsl, sub=i)
                vbf = Hp[0] if par == 0 else Hp[14]
                ku = Hp[3] if par == 0 else Hp[15]
                QM0 = Hp[10] if par == 0 else Hp[16]
                QM1 = Hp[13] if par == 0 else Hp[17]
                scT = Hp[7] if par == 0 else Hp[18]
                sg = Fp[3] if par == 0 else Fp[6]
                pga = V(P[0].t[0:16, 0:128], P[0].bufs)
                for k in range(8):
                    S.mm(pga, V(WGA.t[:, k, :], WGA.bufs), hT(k), start=(k == 0), stop=(k == 7))
                S.copy("act", GAT(slice(0, 16), ALL), pga)
                pqk, pv = P[2], P[3]
                for k in range(8):
                    S.mm(pv(), hT(k), wv(k, 0, 512), start=(k == 0), stop=(k == 7))
                yield
                pz = V(P[0].t[:, 256:512], P[0].bufs)
                S.mm(pz, GAT(slice(0, 17), ALL), WAL(slice(0, 17), ALL))
                for k in range(8):
                    S.mm(pqk(), hT(k), wqk(k, 0, 512), start=(k == 0), stop=(k == 7))
                e_, sp_ = V(Fp[0].t[:, 0:256], Fp[0].bufs), V(Fp[0].t[:, 256:512], Fp[0].bufs)
                S.act(e_, pz, AF.Exp, scale=-1.0)
                S.act(sp_, e_, AF.Ln, bias=1.0, scale=1.0)
                S.copy("act", vbf(), pv())
                yield
                pb = V(P[1].t[:, 0:256], P[1].bufs)
                pd = V(P[1].t[:, 256:512], P[1].bufs)
                S.mm(pb, C("tri"), sp_)
                S.mm(pd, C("dm"), sp_)
                for p_ in range(2):
                    S.mm(V(P[0].t[:, 128 + 2 * p_:130 + 2 * p_], P[0].bufs),
                         V(Fp[0].t[:, 256 + p_ * 128:256 + (p_ + 1) * 128], Fp[0].bufs), C("cind"))
                pbl = V(P[0].t[:, 128:132], P[0].bufs)
                pr = P[3]
                for k in range(8):
                    S.mm(pr(), hT(k), wr_(k, 0, 512), start=(k == 0), stop=(k == 7))
                eb, enb = V(Fp[1].t[:, 0:256], Fp[1].bufs), V(Fp[1].t[:, 256:512], Fp[1].bufs)
                eD = V(Fp[2].t[:, 0:256], Fp[2].bufs)
                dec = V(SM.t[:, 32 + 4 * par:36 + 4 * par], SM.bufs)
                S.act(eb, pb, AF.Exp)
                S.act(enb, pb, AF.Exp, scale=-1.0)
                S.act(eD, pd, AF.Exp)
                S.act(dec, pbl, AF.Exp)
                yield
                QKa, QKb = Hp[1], Hp[2]
                pq_ = V(pqk.t[:, 0:256], pqk.bufs)
                pk_ = V(pqk.t[:, 256:512], pqk.bufs)
                S.stt(QKa(ALL, slice(0, 256)), pq_, 0.125, eb, ALU.mult, ALU.mult)
                S.tt("dve", QKa(ALL, slice(256, 512)), pk_, enb, ALU.mult)
                S.stt(QKb(ALL, slice(0, 256)), pq_, 0.125, enb, ALU.mult, ALU.mult)
                S.tt("dve", QKb(ALL, slice(256, 512)), pk_, eb, ALU.mult)
                for c in range(2):
                    S.stt(ku(ALL, slice(c * 256, (c + 1) * 256)), pk_, hm(c), eD, ALU.mult, ALU.mult)
                S.act(sg(), pr(), AF.Silu)
                yield
                S.tt("dve", sg(), sg(), BCP(ALL, slice(0, 512)), ALU.mult)
                ptb = pbf(None)
                for src_i, src in enumerate((QKa, QKb)):
                    for j in range(4):
                        jj = src_i * 4 + j
                        S.tr(V(ptb.ap[:, jj * 128:(jj + 1) * 128], ptb.bufs), src(ALL, slice(j * 128, (j + 1) * 128)), IDB(),
                             signal=(j == 3))
                yield
                KT, QF, QB = Hp[4], Hp[5], Hp[6]
                S.copy("act", KT(ALL, slice(0, 256)), V(ptb.ap[:, 256:512], ptb.bufs))
                S.copy("act", KT(ALL, slice(256, 512)), V(ptb.ap[:, 768:1024], ptb.bufs))
                qfT = V(ptb.ap[:, 0:256], ptb.bufs)
                qbT = V(ptb.ap[:, 512:768], ptb.bufs)
                for hl in range(2):
                    hsl = slice(hl * 256, (hl + 1) * 256)
                    S.ts("dve", QF(ALL, hsl), qfT, hm(hl), None, op0=ALU.mult)
                    S.ts("dve", QB(ALL, hsl), qbT, hm(hl), None, op0=ALU.mult)
                    S.stt(r2(QM0(ALL, hsl)), r2(qfT), hm(hl), c0b, ALU.mult, ALU.mult)
                    S.stt(r2(QM1(ALL, hsl)), r2(qfT), hm(hl), c1b, ALU.mult, ALU.mult)
                yield
                psf, psb = P[2], P[3]
                for h in range(4):
                    pr2, hl = h // 2, h % 2
                    S.mm(V(psf.t[:, h * 128:(h + 1) * 128], psf.bufs), KT(ALL, slice(pr2 * 128, (pr2 + 1) * 128)),
                         QF(ALL, slice(hl * 256 + pr2 * 128, hl * 256 + (pr2 + 1) * 128)), signal=(h == 3))
                for h in range(4):
                    pr2, hl = h // 2, h % 2
                    S.mm(V(psb.t[:, h * 128:(h + 1) * 128], psb.bufs), KT(ALL, slice(256 + pr2 * 128, 256 + (pr2 + 1) * 128)),
                         QB(ALL, slice(hl * 256 + pr2 * 128, hl * 256 + (pr2 + 1) * 128)), signal=(h == 3))
                yield
                mm1, mm2 = Fp[4], Fp[5]
                S.tt("dve", r3(mm1()), r3(psf()), m1b, ALU.mult)
                S.tt("dve", r3(mm2()), r3(psb()), m2b, ALU.mult)
                S.tt("dve", scT(), mm1(), mm2(), ALU.add)
                yield 'B'
                po, pu = P[4], P[5]
                for h in range(4):
                    S.mm(V(po.t[:, h * 128:(h + 1) * 128], po.bufs), scT(ALL, slice(h * 128, (h + 1) * 128)),
                         vbf(ALL, slice(h * 128, (h + 1) * 128)), start=(h == 0), stop=False, signal=False)
                for c in range(2):
                    Sb = SbA if c == 0 else SbB
                    QM = QM0 if c == 0 else QM1
                    for h in range(4):
                        pr2, hl = h // 2, h % 2
                        S.mm(V(po.t[:, h * 128:(h + 1) * 128], po.bufs),
                             QM(ALL, slice(hl * 256 + pr2 * 128, hl * 256 + (pr2 + 1) * 128)),
                             Sb(ALL, slice(pr2 * 128, (pr2 + 1) * 128)),
                             start=False, stop=(c == 1 and h == 3), signal=(c == 1 and h == 3))
                    for pr2 in range(2):
                        S.mm(V(pu.t[:, pr2 * 256:(pr2 + 1) * 256], pu.bufs),
                             ku(ALL, slice(c * 256 + pr2 * 128, c * 256 + (pr2 + 1) * 128)),
                             vbf(ALL, slice(pr2 * 256, (pr2 + 1) * 256)), signal=(pr2 == 1))
                    yield
                    for pr2 in range(2):
                        for hl in range(2):
                            rows = slice(hl * 64, hl * 64 + 64)
                            sv = Sst(rows, slice(pr2 * 128, (pr2 + 1) * 128))
                            S.stt(sv, sv, V(SM.t[rows, 32 + 4 * par + 2 * pr2 + c:33 + 4 * par + 2 * pr2 + c], SM.bufs),
                                  V(pu.t[rows, pr2 * 256 + hl * 128:pr2 * 256 + (hl + 1) * 128], pu.bufs),
                                  ALU.mult, ALU.add)
                    Sn = SbB if c == 0 else SbA
                    S.copy("act", Sn(ALL, slice(0, 256)), Sst(ALL, slice(0, 256)))
                    yield
                sq = Fp[4]
                S.act(sq(), po(), AF.Square)
                ss = V(SM.t[:, 40:44], SM.bufs)
                S.reduce(ss, r3(sq()), ALU.add)
                yield
                lv = V(SM.t[:, 44:48], SM.bufs)
                rs = V(SM.t[:, 48:52], SM.bufs)
                S.act(lv, ss, AF.Ln, bias=EPS, scale=1.0 / 128)
                S.act(rs, lv, AF.Exp, scale=-0.5)
                yield
                og = Hp[8]
                for h in range(4):
                    hs = slice(h * 128, (h + 1) * 128)
                    S.stt(og(ALL, hs), V(po.t[:, hs], po.bufs), V(SM.t[:, 48 + h:49 + h], SM.bufs), sg(ALL, hs),
                          ALU.mult, ALU.mult)
                yield
                pt2 = pbf(None, 512)
                for h in range(4):
                    S.tr(V(pt2.ap[:, h * 128:(h + 1) * 128], pt2.bufs), og(ALL, slice(h * 128, (h + 1) * 128)), IDB(),
                         signal=(h == 3))
                ogT = Hp[9]
                S.copy("act", ogT(), pt2)
                yield
                yield from out_proj_update(i, wo, ogT, 16, (P[5], P[6]))

            run_pipelined(tile)
            for n_ in (f"gqk{l}", f"gv{l}", f"gr{l}", f"wog{l}"):
                wrel(n_)

        def ret_pass(l):
            if "gla" not in parts:
                S.dma("sp", BCP.t[:], bc_d[l], writes=BCP.bufs)
            wq = wcols(wget(f"rq{l}"), 512)
            wk = wcols(wget(f"rk{l}"), 512)
            wv = wcols(wget(f"rv{l}"), 512)
            wg = wcols(wget(f"rg{l}"), 512)
            wo = wget(f"wor{l}")
            R = Fp[7]
            RbA, RbB = Hp[11], Hp[12]
            S.memset("dve", R(), 0.0)
            S.memset("dve", RbA(), 0.0)

            def tile(i):
                par = i % 2
                tsl = slice(i * 128, (i + 1) * 128)
                hT = lambda k: HT(ALL, k, tsl, sub=i)
                vbf = Hp[0] if par == 0 else Hp[14]
                kdc = (Hp[3], Hp[13]) if par == 0 else (Hp[15], Hp[16])
                qd0 = Hp[6] if par == 0 else Hp[17]
                qd1 = Hp[7] if par == 0 else Hp[18]
                scT = Hp[8] if par == 0 else Hp[19]
                sg = Fp[3] if par == 0 else Fp[6]
                so = 0 if par == 0 else 24
                pq, pk, pv, pg = P[0], P[1], P[2], P[3]
                for (pp, w) in ((pq, wq), (pk, wk)):
                    for k in range(8):
                        S.mm(pp(), hT(k), w(k, 0, 512), start=(k == 0), stop=(k == 7))
                    yield
                cosb = V(ROT.t[:, i, 0:64].unsqueeze(1).unsqueeze(1).broadcast_to([128, 4, 2, 64]), ROT.bufs)
                sinb = V(ROT.t[:, i, 64:128].unsqueeze(1).broadcast_to([128, 4, 64]), ROT.bufs)
                qr, kr = Hp[1], Hp[2]
                for n_, (pp, dst) in enumerate(((pq, qr), (pk, kr))):
                    a_, b_ = Fp[0], Fp[1]
                    S.tt("dve", r4(a_()), r4(pp()), cosb, ALU.mult)
                    p4 = r4(pp())
                    b4 = r4(b_())
                    S.tt("dve", V(b4.ap[:, :, 0, :], b_.bufs), V(p4.ap[:, :, 1, :], pp.bufs), sinb, ALU.mult)
                    S.tt("dve", V(b4.ap[:, :, 1, :], b_.bufs), V(p4.ap[:, :, 0, :], pp.bufs), sinb, ALU.mult)
                    if n_ == 0:
                        for k in range(8):
                            S.mm(pv(), hT(k), wv(k, 0, 512), start=(k == 0), stop=(k == 7))
                    else:
                        for k in range(8):
                            S.mm(pg(), hT(k), wg(k, 0, 512), start=(k == 0), stop=(k == 7))
                    yield
                    a4 = r4(a_())
                    d4 = r4(dst())
                    S.tt("dve", V(d4.ap[:, :, 0, :], dst.bufs), V(a4.ap[:, :, 0, :], a_.bufs), V(b4.ap[:, :, 0, :], b_.bufs),
                         ALU.subtract)
                    S.tt("dve", V(d4.ap[:, :, 1, :], dst.bufs), V(a4.ap[:, :, 1, :], a_.bufs), V(b4.ap[:, :, 1, :], b_.bufs),
                         ALU.add)
                    if n_ == 0:
                        S.copy("act", vbf(), pv())
                    else:
                        S.act(sg(), pg(), AF.Silu)
                    yield
                ptq = pbf(None, 512)
                ptk = V(PB.t[:, 512:1024], PB.bufs)
                for h in range(4):
                    hs = slice(h * 128, (h + 1) * 128)
                    S.tr(V(ptq.ap[:, hs], ptq.bufs), qr(ALL, hs), IDB(), signal=(h == 3))
                for h in range(4):
                    hs = slice(h * 128, (h + 1) * 128)
                    S.tr(V(ptk.ap[:, hs], ptk.bufs), kr(ALL, hs), IDB(), signal=(h == 3))
                for c in range(2):
                    for h in range(4):
                        hs = slice(h * 128, (h + 1) * 128)
                        S.ts("dve", kdc[c](ALL, hs), kr(ALL, hs), C("kdec", 4 * c + h, 4 * c + h + 1), None, op0=ALU.mult)
                yield
                qT, kT = Hp[4], Hp[5]
                S.copy("act", qT(), ptq)
                S.copy("act", kT(), ptk)
                S.tt("dve", qd0(), ptq, C("qd0"), ALU.mult)
                S.tt("dve", qd1(), ptq, C("qd1"), ALU.mult)
                yield
                ps = P[0]
                for h in range(4):
                    hs = slice(h * 128, (h + 1) * 128)
                    S.mm(V(ps.t[:, hs], ps.bufs), kT(ALL, hs), qT(ALL, hs), signal=(h == 3))
                yield
                S.tt("dve", scT(), ps(), C("dret"), ALU.mult)
                yield 'B'
                po, pu = P[4], P[5]
                for h in range(4):
                    hs = slice(h * 128, (h + 1) * 128)
                    S.mm(V(po.t[:, hs], po.bufs), scT(ALL, hs), vbf(ALL, hs), start=(h == 0), stop=False, signal=False)
                for c in range(2):
                    Rb = RbA if c == 0 else RbB
                    qd = qd0 if c == 0 else qd1
                    for h in range(4):
                        hs = slice(h * 128, (h + 1) * 128)
                        S.mm(V(po.t[:, hs], po.bufs), qd(ALL, hs), Rb(ALL, hs), start=False, stop=(c == 1 and h == 3),
                             signal=(c == 1 and h == 3))
                    for h in range(4):
                        hs = slice(h * 128, (h + 1) * 128)
                        S.mm(V(pu.t[:, hs], pu.bufs), kdc[c](ALL, hs), vbf(ALL, hs), signal=(h == 3))
                    yield
                    for h in range(4):
                        hs = slice(h * 128, (h + 1) * 128)
                        S.stt(R(ALL, hs), R(ALL, hs), float(GAMMA[h] ** 64), V(pu.t[:, hs], pu.bufs), ALU.mult, ALU.add)
                    Rn = RbB if c == 0 else RbA
                    S.copy("act", Rn(), R())
                    yield
                sm = lambda a, b: V(SM.t[:, so + a:so + b], SM.bufs)
                s1, s2, mean, var, rs, nmr = sm(0, 4), sm(4, 8), sm(8, 12), sm(12, 16), sm(16, 20), sm(20, 24)
                S.reduce(s1, r3(po()), ALU.add)
                sq = Fp[4]
                S.act(sq(), po(), AF.Square)
                yield
                S.reduce(s2, r3(sq()), ALU.add)
                S.ts("dve", mean, s1, 1.0 / 128, None, op0=ALU.mult)
                S.tt("dve", var, mean, mean, ALU.mult)
                S.stt(var, s2, 1.0 / 128, var, ALU.mult, ALU.subtract)
                S.ts("dve", var, var, 0.0, None, op0=ALU.max)
                yield
                S.act(var, var, AF.Ln, bias=EPS, scale=1.0)
                S.act(rs, var, AF.Exp, scale=-0.5)
                yield
                S.stt(nmr, mean, -1.0, rs, ALU.mult, ALU.mult)
                on = Fp[5]
                for h in range(4):
                    hs = slice(h * 128, (h + 1) * 128)
                    S.act(on(ALL, hs), V(po.t[:, hs], po.bufs), AF.Identity, bias=V(SM.t[:, so + 20 + h:so + 21 + h], SM.bufs),
                          scale=V(SM.t[:, so + 16 + h:so + 17 + h], SM.bufs))
                yield
                S.tt("dve", on(), on(), BCP(ALL, slice(512, 1024)), ALU.mult)
                S.tt("dve", on(), on(), BCP(ALL, slice(1024, 1536)), ALU.add)
                orr = Hp[9]
                S.tt("dve", orr(), on(), sg(), ALU.mult)
                yield
                pt2 = pbf(None, 512)
                for h in range(4):
                    hs = slice(h * 128, (h + 1) * 128)
                    S.tr(V(pt2.ap[:, hs], pt2.bufs), orr(ALL, hs), IDB(), signal=(h == 3))
                orT = Hp[10]
                S.copy("act", orT(), pt2)
                yield
                yield from out_proj_update(i, wo, orT, 16, (P[5], P[6]))

            run_pipelined(tile)
            for n_ in (f"rq{l}", f"rk{l}", f"rv{l}", f"rg{l}", f"wor{l}"):
                wrel(n_)

        def ffn_group(names, nch, gate):
            n1, n3, n2 = names
            w1 = wcols(wget(n1), nch * 128)
            w3 = wcols(wget(n3), nch * 128)
            w2 = wrows(wget(n2))
            for u in range(8):
                usl = slice(u * 256, (u + 1) * 256)
                subs = [2 * u, 2 * u + 1]
                py = P[0:4]

                def up(j):
                    ph = P[4 + (j % 3)]
                    for (off, w) in ((0, w1), (256, w3)):
                        for k in range(8):
                            S.mm(V(ph.t[:, off:off + 256], ph.bufs), w(k, j * 128, (j + 1) * 128), HT(ALL, k, usl, sub=subs),
                                 start=(k == 0), stop=(k == 7))
                    s_ = Fp[j % 4]
                    a_ = Hp[j % 4]
                    S.act(s_(ALL, slice(0, 256)), V(ph.t[:, 0:256], ph.bufs), AF.Silu)
                    if gate is not None:
                        S.tt("dve", s_(ALL, slice(0, 256)), s_(ALL, slice(0, 256)), gate(usl), ALU.mult)
                    S.tt("dve", a_(ALL, slice(0, 256)), V(ph.t[:, 256:512], ph.bufs), s_(ALL, slice(0, 256)), ALU.mult)

                def down(j):
                    a_ = Hp[j % 4]
                    for m in range(8):
                        o = V(py[m // 2].t[:, (m % 2) * 256:(m % 2 + 1) * 256], py[m // 2].bufs)
                        S.mm(o, w2(j, m * 128, (m + 1) * 128), a_(ALL, slice(0, 256)), start=(j == 0 and m % 2 == 0),
                             stop=(j == nch - 1 and m % 2 == 1), signal=(j == nch - 1))

                up(0)
                for j in range(nch):
                    if j + 1 < nch:
                        up(j + 1)
                    down(j)
                for m in range(8):
                    o = V(py[m // 2].t[:, (m % 2) * 256:(m % 2 + 1) * 256], py[m // 2].bufs)
                    S.stt(X(ALL, m, usl, sub=subs), o, MOD(ALL, slice(40 + m, 41 + m)), X(ALL, m, usl, sub=subs),
                          ALU.mult, ALU.add)
            for n_ in names:
                wrel(n_)

        def dense_ffn(l):
            for gi, (c0, n) in enumerate(ffn_groups(22)):
                ffn_group((f"w1_{l}_{gi}", f"w3_{l}_{gi}", f"w2_{l}_{gi}"), n, None)

        def moe_ffn(l):
            i_ = l // 2
            S.dma("pool", RW.t[:], rw_d[i_].rearrange("(k p) e -> p k e", p=128), writes=RW.bufs)
            for i in range(NT):
                tsl = slice(i * 128, (i + 1) * 128)
                pl = V(P[4 + (i % 2)].t[:, 0:8], P[4 + (i % 2)].bufs)
                for k in range(8):
                    S.mm(pl, HT(ALL, k, tsl, sub=i), V(RW.t[:, k, :], RW.bufs), start=(k == 0), stop=(k == 7))
                sm = lambda a, b: V(SM.t[:, a:b], SM.bufs)
                lg, m1_, eq, l2, m2_, selm, ex, nm1, ssum = (sm(0, 8), sm(8, 9), sm(16, 24), sm(24, 32), sm(9, 10),
                                                              sm(32, 40), sm(40, 48), sm(10, 11), sm(11, 12))
                S.copy("dve", lg, pl)
                S.reduce(m1_, lg, ALU.max)
                S.ts("dve", eq, lg, m1_, None, op0=ALU.is_equal)
                S.stt(l2, eq, -1e30, lg, ALU.mult, ALU.add)
                S.reduce(m2_, l2, ALU.max)
                S.ts("dve", selm, lg, m2_, None, op0=ALU.is_ge)
                S.ts("dve", nm1, m1_, -1.0, None, op0=ALU.mult)
                S.act(ex, lg, AF.Exp, bias=nm1, scale=1.0)
                S.tt("dve", ex, ex, selm, ALU.mult)
                S.reduce(ssum, ex, ALU.add)
                S.op("dve", lambda h: h.reciprocal(out=ssum.ap, in_=ssum.ap), reads=ssum.bufs, writes=ssum.bufs)
                S.ts("dve", V(GATES.t[:, i, :], GATES.bufs), ex, ssum, None, op0=ALU.mult)
            GB = [Fp[4], Fp[5], Fp[6], Fp[7]]
            if debug:
                S.dma("sp", dbg_d[:, 0:128], GATES.t[:].rearrange("p i e -> p (i e)"), reads=GATES.bufs)
            for e in range(NE):
                for s in range(4):
                    pg_ = P[4 + (s % 2)]
                    for j in range(4):
                        i = s * 4 + j
                        gbt = Hp[4 + (i % 2)]
                        gb = lambda gbt=gbt: V(gbt.t[:].bitcast(F32)[:, 0:128], gbt.bufs)
                        S.ts("dve", gb(), ones, V(GATES.t[:, i, e:e + 1], GATES.bufs), None, op0=ALU.mult)
                        S.mm(V(pg_.t[:, j * 128:(j + 1) * 128], pg_.bufs), gb(), ident, signal=(j == 3))
                    S.copy("act", GB[s](), pg_())
                if debug and e == 3:
                    for s in range(4):
                        S.dma("sp", dbg_d[:, 128 + s * 512:128 + (s + 1) * 512], GB[s].t[:], reads=GB[s].bufs)
                gate = lambda usl: V(GB[usl.start // 512].t[:, usl.start % 512:usl.start % 512 + 256], GB[usl.start // 512].bufs)
                for gi, (c0, n) in enumerate(ffn_groups(11)):
                    ffn_group((f"w1_{l}_{e}_{gi}", f"w3_{l}_{e}_{gi}", f"w2_{l}_{e}_{gi}"), n, gate)

        for l in layers:
            ada_phase(l)
            if "gla" in parts or "ret" in parts:
                norm_phase(0, 0)
            if "gla" in parts:
                gla_pass(l)
            if "ret" in parts:
                ret_pass(l)
            if "ffn" in parts:
                norm_phase(8, 24)
                if l % 2 == 0:
                    dense_ffn(l)
                else:
                    moe_ffn(l)

        fg0 = 2 * DEPTH * 8
        for s in range(4):
            subs = list(range(4 * s, 4 * s + 4))
            rstd = Fp[3]
            if do_final:
                norm_stats(s, P[s % 2], Fp[0], Fp[1], Fp[2], rstd)
            for j in range(4):
                ti = s * 4 + j
                tsl = slice(ti * 128, (ti + 1) * 128)
                ob = (Fp[4], Fp[5]) if ti % 2 == 0 else (Fp[6], Fp[7])
                for k in range(8):
                    t = Hp[k % 2]
                    tv = V(t.t[:].bitcast(F32)[:, 0:128], t.bufs)
                    if do_final:
                        S.stt(tv, X(ALL, k, tsl, sub=ti), NG(ALL, slice(fg0 + k, fg0 + k + 1)),
                              rstd(ALL, slice(j * 128, (j + 1) * 128)), ALU.mult, ALU.mult)
                    else:
                        S.copy("dve", tv, X(ALL, k, tsl, sub=ti))
                    pt = P[2 + k // 4]
                    S.tr(V(pt.t[:, (k % 4) * 128:(k % 4 + 1) * 128], pt.bufs), tv, ident, signal=(k % 4 == 3))
                    if k % 4 == 3:
                        S.copy("act", ob[k // 4](), pt())
                for hh in range(2):
                    S.dma("sp" if hh == 0 else "act", y_d[ti * 128:(ti + 1) * 128, hh * 512:(hh + 1) * 512], ob[hh].t[:],
                          reads=ob[hh].bufs)
        S.wait_bufs("sp", Fp[4].bufs + Fp[5].bufs + Fp[6].bufs + Fp[7].bufs)
        assert state["next"] == len(jobs), (state["next"], len(jobs))
        build_program.stats = (S.nins, S.nwaits)
    return nc


def _prep_shared(inp):
    f = lambda a: np.ascontiguousarray(np.asarray(a, dtype=np.float32))
    sh = {}
    sh["cst"] = make_consts()
    sh["ada_w"] = f(inp["ada_w"])
    sh["ada_b_fm"] = f(np.asarray(inp["ada_b"], np.float32).reshape(DEPTH, 48, 128).transpose(0, 2, 1))
    ng = np.concatenate([
        np.asarray(inp["norm_mix_g"], np.float32).reshape(DEPTH, 8, 128).transpose(2, 0, 1).reshape(128, DEPTH * 8),
        np.asarray(inp["norm_ffn_g"], np.float32).reshape(DEPTH, 8, 128).transpose(2, 0, 1).reshape(128, DEPTH * 8),
        np.asarray(inp["final_g"], np.float32).reshape(8, 128).T], axis=1)
    sh["norm_g_fm"] = f(ng)
    sh["w_in"] = f(inp["w_in"])
    sh["w_alpha_aug"] = f(np.concatenate([np.asarray(inp["gla_w_alpha"], np.float32),
                                          np.asarray(inp["gla_b_alpha"], np.float32)[:, None, :]], axis=1))
    bc = np.concatenate([np.asarray(inp["gla_norm_g"], np.float32), np.asarray(inp["ret_gn_g"], np.float32),
                         np.asarray(inp["ret_gn_b"], np.float32)], axis=1)
    sh["bc_params"] = f(np.broadcast_to(bc[:, None, :], (DEPTH, 128, 1536)))
    sh["w_out"] = f(inp["w_out"])
    for k_ in ("ffn_w1", "ffn_w3", "ffn_w2", "router_w", "moe_w1", "moe_w3", "moe_w2"):
        sh[k_] = f(inp[k_])
    return sh


def _prep_core(inp, b):
    d = {}
    d["x"] = np.ascontiguousarray(np.asarray(inp["x"][b], np.float32))
    d["cvec"] = np.ascontiguousarray(np.asarray(inp["c"][b], np.float32).reshape(8, 128).T)
    d["pos"] = np.ascontiguousarray(np.asarray(inp["positions"][b]).astype(np.int32).reshape(NT, 128).T)
    return d


_CACHE = {}


def kernel(**inputs):
    key = "full"
    if key not in _CACHE:
        _CACHE[key] = build_program()
    nc = _CACHE[key]
    sh = _prep_shared(inputs)
    in_maps = []
    for b in range(8):
        d = dict(sh)
        d.update(_prep_core(inputs, b))
        in_maps.append(d)
    res = run_bass_kernel_spmd(nc, in_maps, core_ids=list(range(8)))
    out = np.stack([np.asarray(res.results[b]["y"], np.float32) for b in range(8)], axis=0)
    return out
```

```python
import math
from contextlib import ExitStack

import numpy as np
import concourse.bass as bass
import concourse.mybir as mybir
from concourse.bass_utils import run_bass_kernel_spmd

F32 = mybir.dt.float32
BF16 = mybir.dt.bfloat16
I32 = mybir.dt.int32
AF = mybir.ActivationFunctionType
ALU = mybir.AluOpType
AX = mybir.AxisListType

D = 1024
SEQ = 2048
NT = 16
DEPTH = 4
D_FF = 2816
NE = 8
D_FFE = 1408
EPS = 1e-6
ALL = slice(None)
import os as _os
_DBG_STOP = int(_os.environ.get('DBG_STOP', '0'))


class Buf:
    __slots__ = ("name", "w", "r", "excl")

    def __init__(self, name, excl=False):
        self.name = name
        self.w = None
        self.r = {}
        self.excl = excl


class V:
    __slots__ = ("ap", "bufs")

    def __init__(self, ap, bufs):
        self.ap = ap
        self.bufs = bufs


class T:
    def __init__(self, tensor, name, nsub=1, excl=False):
        self.t = tensor
        self.name = name
        self.bufs = [Buf(f"{name}.{i}", excl) for i in range(nsub)]

    def __call__(self, *idx, sub=None):
        ap = self.t[idx] if idx else self.t[:]
        if sub is None:
            bufs = self.bufs
        elif isinstance(sub, int):
            bufs = [self.bufs[sub]]
        else:
            bufs = [self.bufs[s] for s in sub]
        return V(ap, bufs)


class Eng:
    def __init__(self, key, h, sem):
        self.key = key
        self.h = h
        self.sem = sem
        self.seq = 0
        self.cnt = 0
        self.last = None
        self.last_inc = False
        self.miles = []
        self.seen = {}


class Sync:
    def __init__(self, nc, sems, dma_sems):
        self.nc = nc
        keys = [("pe", nc.tensor), ("act", nc.scalar), ("dve", nc.vector),
                ("pool", nc.gpsimd), ("sp", nc.sync)]
        self.E = {}
        for (k, h), sm in zip(keys, sems):
            self.E[k] = Eng(k, h, sm)
        self.dsem = dma_sems
        self.dval = {q: [0] * len(v) for q, v in dma_sems.items()}
        self.drr = {q: 0 for q in dma_sems}
        self.nwaits = 0
        self.nins = 0

    def _resolve(self, tok):
        if tok[0] == 'd':
            return ('d', tok[1], tok[2]), self.dsem[tok[1]][tok[2]], tok[3]
        e = self.E[tok[1]]
        n = tok[2]
        val = None
        for (sq, v) in reversed(e.miles):
            if sq >= n:
                val = v
            else:
                break
        if val is None:
            assert e.seq >= n and e.last is not None
            if not e.last_inc:
                e.cnt += 1
                e.last.then_inc(e.sem, 1)
                e.last_inc = True
                e.miles.append((e.seq, e.cnt))
            val = e.cnt
        return ('e', e.key), e.sem, val

    def _need(self, eng, toks, raw=()):
        e = self.E[eng]
        need = {}
        for lst in (toks, raw):
            for t in lst:
                if t is None:
                    continue
                if t[0] == 'e' and t[1] == eng and eng == "pe":
                    continue
                sk, sh, v = self._resolve(t)
                if e.seen.get(sk, 0) >= v:
                    continue
                if sk not in need or need[sk][1] < v:
                    need[sk] = (sh, v)
        return [(sk, sh, v) for sk, (sh, v) in need.items()]

    def _wait(self, eng, toks, raw=(), keep_one=False):
        e = self.E[eng]
        need = self._need(eng, toks, raw)
        kept = None
        if keep_one and need:
            kept = need.pop()
        for sk, sh, v in need:
            e.h.wait_ge(sh, v)
            e.seen[sk] = v
            self.nwaits += 1
        if kept is not None:
            e.seen[kept[0]] = kept[2]
        return kept

    @staticmethod
    def _deps(reads, writes):
        toks = []
        raw = []
        for b in reads:
            raw.append(b.w)
            if b.excl:
                toks.extend(b.r.values())
        for b in writes:
            toks.append(b.w)
            toks.extend(b.r.values())
        return toks, raw

    @staticmethod
    def _commit(tok, reads, writes):
        key = tok[:2] if tok[0] == 'e' else tok[:3]
        for b in reads:
            b.r[key] = tok
        for b in writes:
            b.w = tok
            b.r = {}

    def op(self, eng, fn, reads=(), writes=(), signal=None):
        e = self.E[eng]
        if signal is None:
            signal = eng != "pe"
        kept = self._wait(eng, *self._deps(reads, writes), keep_one=(eng != "pe"))
        ins = fn(e.h)
        if kept is not None:
            ins._wait_ge(kept[1], kept[2])
        e.seq += 1
        self.nins += 1
        e.last = ins
        e.last_inc = False
        if signal:
            e.cnt += 1
            ins.then_inc(e.sem, 1)
            e.last_inc = True
            e.miles.append((e.seq, e.cnt))
            if len(e.miles) > 4096:
                e.miles = e.miles[-2048:]
        tok = ('e', eng, e.seq)
        self._commit(tok, reads, writes)
        return tok

    def dma(self, q, out, in_, reads=(), writes=(), **kw):
        e = self.E[q]
        i = self.drr[q]
        self.drr[q] = (i + 1) % len(self.dsem[q])
        toks, raw = self._deps(reads, writes)
        if self.dval[q][i] > 0:
            toks.append(('d', q, i, self.dval[q][i]))
        self._wait(q, toks, raw)
        ins = e.h.dma_start(out=out, in_=in_, **kw)
        ins.then_inc(self.dsem[q][i], 16)
        self.dval[q][i] += 16
        self.nins += 1
        tok = ('d', q, i, self.dval[q][i])
        self._commit(tok, reads, writes)
        return tok

    def wait_bufs(self, eng, bufs):
        toks = []
        for b in bufs:
            toks.append(b.w)
            toks.extend(b.r.values())
        self._wait(eng, toks)

    @staticmethod
    def _rb(*vs):
        out = []
        for v in vs:
            if isinstance(v, V):
                out.extend(v.bufs)
        return out

    @staticmethod
    def _a(v):
        return v.ap if isinstance(v, V) else v

    def mm(self, out, lhsT, rhs, start=True, stop=True, signal=None, **kw):
        if signal is None:
            signal = bool(stop)
        return self.op("pe", lambda h: h.matmul(out.ap, lhsT.ap, rhs.ap, start=start, stop=stop, **kw),
                       reads=self._rb(lhsT, rhs), writes=out.bufs, signal=signal)

    def tr(self, out, in_, ident, signal=True):
        return self.op("pe", lambda h: h.transpose(out.ap, in_.ap, ident.ap),
                       reads=self._rb(in_, ident), writes=out.bufs, signal=signal)

    def act(self, out, in_, func, bias=None, scale=None):
        kw = {}
        if bias is not None:
            kw["bias"] = self._a(bias)
        if scale is not None:
            kw["scale"] = self._a(scale)
        return self.op("act", lambda h: h.activation(out=out.ap, in_=in_.ap, func=func, **kw),
                       reads=self._rb(in_, bias, scale), writes=out.bufs)

    def tt(self, eng, out, in0, in1, op):
        return self.op(eng, lambda h: h.tensor_tensor(out=out.ap, in0=in0.ap, in1=in1.ap, op=op),
                       reads=self._rb(in0, in1), writes=out.bufs)

    def ts(self, eng, out, in0, s1, s2=None, op0=ALU.mult, op1=None):
        kw = {}
        if op1 is not None:
            kw["op1"] = op1
        return self.op(eng, lambda h: h.tensor_scalar(out=out.ap, in0=in0.ap, scalar1=self._a(s1),
                                                      scalar2=self._a(s2), op0=op0, **kw),
                       reads=self._rb(in0, s1, s2), writes=out.bufs)

    def stt(self, out, in0, scalar, in1, op0, op1):
        return self.op("dve", lambda h: h.scalar_tensor_tensor(out=out.ap, in0=in0.ap, scalar=self._a(scalar),
                                                               in1=in1.ap, op0=op0, op1=op1),
                       reads=self._rb(in0, scalar, in1), writes=out.bufs)

    def copy(self, eng, out, in_):
        if eng == "act":
            return self.op(eng, lambda h: h.copy(out=out.ap, in_=in_.ap), reads=in_.bufs, writes=out.bufs)
        return self.op(eng, lambda h: h.tensor_copy(out=out.ap, in_=in_.ap), reads=in_.bufs, writes=out.bufs)

    def reduce(self, out, in_, op, axis=AX.X):
        return self.op("dve", lambda h: h.tensor_reduce(out=out.ap, in_=in_.ap, axis=axis, op=op),
                       reads=in_.bufs, writes=out.bufs)

    def memset(self, eng, out, val):
        return self.op(eng, lambda h: h.memset(out.ap, val), writes=out.bufs)


class CL:
    off = {}
    n = 0

    @classmethod
    def add(cls, name, w):
        cls.off[name] = (cls.n, cls.n + w)
        cls.n += w


for _n, _w in [("ident", 128), ("ones", 128), ("tri", 128), ("dm", 128), ("cind", 2), ("m1", 128), ("m2", 128),
               ("dret", 512), ("qd0", 512), ("qd1", 512), ("kdec", 8), ("invf", 64), ("c0", 128), ("c1", 128), ("hm", 2)]:
    CL.add(_n, _w)

GAMMA = [1.0 - 2.0 ** (-5.0 - h) for h in range(4)]


def make_consts():
    c = np.zeros((128, CL.n), np.float64)

    def put(name, arr):
        a, b = CL.off[name]
        c[:, a:b] = np.asarray(arr, np.float64).reshape(128, b - a)

    s = np.arange(128)[:, None]
    t = np.arange(128)[None, :]
    same = (s // 64) == (t // 64)
    put("ident", np.eye(128))
    put("ones", np.ones((128, 128)))
    put("tri", np.where(same & (s <= t), -1.0 / 16, 0.0))
    put("dm", np.where(same & (s > t), -1.0 / 16, 0.0))
    put("cind", np.where((s // 64) == np.arange(2)[None, :], -1.0 / 16, 0.0))
    put("m1", np.where(same & (t >= s), 1.0, 0.0))
    put("m2", np.where(same & (t < s), 1.0, 0.0))
    dret = np.zeros((128, 4, 128))
    qd0 = np.zeros((128, 4, 128))
    qd1 = np.zeros((128, 4, 128))
    kdec = np.zeros((128, 8))
    tt = np.arange(128)
    for h in range(4):
        lg = math.log(GAMMA[h])
        dret[:, h, :] = np.where(same, np.exp(lg * np.abs(t - s)), 0.0) * 128 ** -0.5
        qd = np.exp(lg * ((tt % 64) + 1)) * 128 ** -0.5
        qd0[:, h, :] = np.where(tt < 64, qd, 0.0)[None, :]
        qd1[:, h, :] = np.where(tt >= 64, qd, 0.0)[None, :]
        kdec[:, h] = np.where(tt < 64, np.exp(lg * (63 - (tt % 64))), 0.0)
        kdec[:, 4 + h] = np.where(tt >= 64, np.exp(lg * (63 - (tt % 64))), 0.0)
    put("dret", dret)
    put("qd0", qd0)
    put("qd1", qd1)
    put("kdec", kdec)
    invf = 10000.0 ** (-np.arange(64) / 64.0)
    put("invf", np.broadcast_to(invf[None, :], (128, 64)))
    put("c0", np.broadcast_to((tt < 64)[None, :], (128, 128)))
    put("c1", np.broadcast_to((tt >= 64)[None, :], (128, 128)))
    put("hm", np.stack([tt < 64, tt >= 64], axis=1))
    return c.astype(np.float32)


def build_program(layers=(0, 1, 2, 3), do_final=True, parts=("gla", "ret", "ffn"), nslots=6, debug=False):
    nc = bass.Bass("TRN2", target_bir_lowering=False)

    def din(name, shape, dt=F32):
        return nc.dram_tensor(name, list(shape), dt, kind="ExternalInput").ap()

    x_d = din("x", [SEQ, D])
    c_d = din("cvec", [128, 8])
    pos_d = din("pos", [128, NT], I32)
    cst_d = din("cst", [128, CL.n])
    adaw_d = din("ada_w", [DEPTH, D, 6 * D])
    adab_d = din("ada_b_fm", [DEPTH, 128, 48])
    ng_d = din("norm_g_fm", [128, 2 * DEPTH * 8 + 8])
    win_d = din("w_in", [DEPTH, D, 3600])
    wal_d = din("w_alpha_aug", [DEPTH, 17, 256])
    bc_d = din("bc_params", [DEPTH, 128, 1536])
    wout_d = din("w_out", [DEPTH, D, D])
    w1_d = din("ffn_w1", [2, D, D_FF])
    w3_d = din("ffn_w3", [2, D, D_FF])
    w2_d = din("ffn_w2", [2, D_FF, D])
    rw_d = din("router_w", [2, D, NE])
    m1_d = din("moe_w1", [2, NE, D, D_FFE])
    m3_d = din("moe_w3", [2, NE, D, D_FFE])
    m2_d = din("moe_w2", [2, NE, D_FFE, D])
    y_d = nc.dram_tensor("y", [SEQ, D], F32, kind="ExternalOutput").ap()
    dbg_d = nc.dram_tensor("dbg", [128, 4096], F32, kind="ExternalOutput").ap() if debug else None

    with ExitStack() as st:
        def sb(name, shape, dt, nsub=1):
            return T(st.enter_context(nc.sbuf_tensor(name, list(shape), dt)), name, nsub)

        X = sb("X", [128, 8, SEQ], F32, NT)
        HT = sb("HT", [128, 8, SEQ], BF16, NT)
        CST = sb("CST", [128, CL.n], F32)
        SLOT = [sb(f"slot{i}", [128, 4096], BF16) for i in range(nslots)]
        Fp = [sb(f"F{i}", [128, 512], F32) for i in range(8)]
        Hp = [sb(f"H{i}", [128, 512], BF16) for i in range(20)]
        NG = sb("NG", [128, 2 * DEPTH * 8 + 8], F32)
        MOD = sb("MOD", [128, 48], F32)
        MODB = sb("MODB", [128, 48], F32)
        AB = sb("AB", [128, 16], F32)
        CV = sb("CV", [128, 8], F32)
        CVB = sb("CVB", [128, 8], BF16)
        IDB = sb("IDB", [128, 128], BF16)
        WGA = sb("WGA", [128, 8, 16], BF16)
        WAL = sb("WAL", [32, 256], BF16)
        GAT = sb("GAT", [32, 128], BF16)
        BCP = sb("BCP", [128, 1536], F32)
        ROT = sb("ROT", [128, NT, 128], F32)
        SM = sb("SM", [128, 64], F32)
        POSI = sb("POSI", [128, NT], I32)
        RW = sb("RW", [128, 8, 8], BF16)
        GATES = sb("GATES", [128, NT, 8], F32)
        P = [T(st.enter_context(nc.psum_tensor(f"P{i}", [128, 512], F32)), f"P{i}", excl=True) for i in range(7)]
        PB = T(st.enter_context(nc.psum_tensor("PB", [128, 1024], BF16)), "PB", excl=True)
        sems = [st.enter_context(nc.semaphore(f"s_{k}")) for k in ["pe", "act", "dve", "pool", "sp"]]
        dsems = {q: [st.enter_context(nc.semaphore(f"d_{q}{i}")) for i in range(12)] for q in ["sp", "act", "pool"]}
        S = Sync(nc, sems, dsems)

        def C(name, lo=None, hi=None, rows=ALL):
            a, b = CL.off[name]
            if lo is not None:
                a, b = a + lo, a + hi
            return CST(rows, slice(a, b))

        ident = C("ident")
        ones = C("ones")

        def pbf(p, w=1024):
            return V(PB.t[:, 0:w], PB.bufs)

        jobs = []
        job_slot = {}
        state = {"next": 0}
        free = list(range(nslots))

        def issue_pending():
            while free and state["next"] < len(jobs):
                name, fn = jobs[state["next"]]
                state["next"] += 1
                si = free.pop(0)
                sl = SLOT[si]
                for (o, i) in fn(sl):
                    S.dma("pool", o, i, writes=sl.bufs)
                job_slot[name] = si

        def wget(name):
            assert name in job_slot, f"weight job {name} not issued (ring too small)"
            return SLOT[job_slot[name]]

        def wrel(name):
            si = job_slot.pop(name)
            free.append(si)
            issue_pending()

        def job_cols(name, src2d, c0, w):
            def fn(sl):
                dst = sl.t[:, 0:8 * w].rearrange("p (k c) -> p k c", k=8)
                return [(dst, src2d.rearrange("(k p) c -> p k c", p=128)[:, :, c0:c0 + w])]
            jobs.append((name, fn))

        def job_rows(name, src2d, r0, nch):
            def fn(sl):
                dst = sl.t[:, 0:nch * 1024].rearrange("p (j c) -> p j c", j=nch)
                return [(dst, src2d[r0:r0 + nch * 128, :].rearrange("(j p) c -> p j c", p=128))]
            jobs.append((name, fn))

        def wcols(sl, w):
            return lambda k, a, b: V(sl.t[:, k * w + a:k * w + b], sl.bufs)

        def wrows(sl):
            return lambda j, a, b: V(sl.t[:, j * 1024 + a:j * 1024 + b], sl.bufs)

        def ffn_groups(nchunks):
            g = []
            c = 0
            while c < nchunks:
                n = min(4, nchunks - c)
                g.append((c, n))
                c += n
            return g

        for l in layers:
            for g in range(12):
                job_cols(f"ada{l}_{g}", adaw_d[l], g * 512, 512)
            if "gla" in parts:
                job_cols(f"gqk{l}", win_d[l], 0, 512)
                job_cols(f"gv{l}", win_d[l], 512, 512)
                job_cols(f"gr{l}", win_d[l], 1024, 512)
                job_rows(f"wog{l}", wout_d[l], 0, 4)
            if "ret" in parts:
                job_cols(f"rq{l}", win_d[l], 1552, 512)
                job_cols(f"rk{l}", win_d[l], 2064, 512)
                job_cols(f"rv{l}", win_d[l], 2576, 512)
                job_cols(f"rg{l}", win_d[l], 3088, 512)
                job_rows(f"wor{l}", wout_d[l], 512, 4)
            if "ffn" in parts:
                i = l // 2
                if l % 2 == 0:
                    for gi, (c0, n) in enumerate(ffn_groups(22)):
                        job_cols(f"w1_{l}_{gi}", w1_d[i], c0 * 128, n * 128)
                        job_cols(f"w3_{l}_{gi}", w3_d[i], c0 * 128, n * 128)
                        job_rows(f"w2_{l}_{gi}", w2_d[i], c0 * 128, n)
                else:
                    for e in range(NE):
                        for gi, (c0, n) in enumerate(ffn_groups(11)):
                            job_cols(f"w1_{l}_{e}_{gi}", m1_d[i, e], c0 * 128, n * 128)
                            job_cols(f"w3_{l}_{e}_{gi}", m3_d[i, e], c0 * 128, n * 128)
                            job_rows(f"w2_{l}_{e}_{gi}", m2_d[i, e], c0 * 128, n)

        S.dma("sp", CST.t[:], cst_d, writes=CST.bufs)
        S.dma("sp", NG.t[:], ng_d, writes=NG.bufs)
        S.dma("sp", CV.t[:], c_d, writes=CV.bufs)
        S.dma("sp", POSI.t[:], pos_d, writes=POSI.bufs)
        issue_pending()
        S.copy("dve", IDB(), ident)
        S.act(CVB(), CV(), AF.Silu)
        S.memset("dve", GAT(), 1.0)

        for i in range(NT):
            xb = Fp[(i % 2) * 2], Fp[(i % 2) * 2 + 1]
            for hh in range(2):
                S.dma("sp" if hh == 0 else "act", xb[hh].t[:], x_d[i * 128:(i + 1) * 128, hh * 512:(hh + 1) * 512],
                      writes=xb[hh].bufs)
                pt = P[hh]
                for j in range(4):
                    S.tr(V(pt.t[:, j * 128:(j + 1) * 128], pt.bufs), xb[hh](ALL, slice(j * 128, (j + 1) * 128)), ident,
                         signal=(j == 3))
                dst = V(X.t[:, hh * 4:(hh + 1) * 4, i * 128:(i + 1) * 128], [X.bufs[i]])
                src = V(pt.t[:].rearrange("p (j t) -> p j t", j=4), pt.bufs)
                S.copy("act" if hh == 0 else "dve", dst, src)

        if "ret" in parts:
            posf = V(SM.t[:, 0:NT], SM.bufs)
            S.copy("dve", posf, POSI())
            ang = Fp[4]
            angv = lambda i: V(ang.t[:, 0:1024].rearrange("p (i j) -> p i j", i=NT)[:, i, :], ang.bufs)
            two_pi = 2.0 * math.pi
            for which, shift in (("sin", 0.0), ("cos", math.pi / 2)):
                for half in range(2):
                    a = Fp[4 + half]
                    for ii in range(8):
                        i = half * 8 + ii
                        S.ts("dve", a(ALL, slice(ii * 64, (ii + 1) * 64)), C("invf"), V(SM.t[:, i:i + 1], SM.bufs),
                             shift, op0=ALU.mult, op1=ALU.add)
                    u = Fp[6]
                    ki = V(Hp[0].t[:].bitcast(I32)[:, 0:256], Hp[0].bufs)
                    ki2 = V(Hp[1].t[:].bitcast(I32)[:, 0:256], Hp[1].bufs)
                    S.ts("dve", u(), a(), 1.0 / two_pi, None, op0=ALU.mult)
                    for q4, kk in ((0, ki), (1, ki2)):
                        S.copy("dve", kk, u(ALL, slice(q4 * 256, (q4 + 1) * 256)))
                        S.copy("dve", u(ALL, slice(q4 * 256, (q4 + 1) * 256)), kk)
                    C1 = 6.28125
                    C2 = two_pi - C1
                    S.stt(a(), u(), -C1, a(), ALU.mult, ALU.add)
                    S.stt(a(), u(), -C2, a(), ALU.mult, ALU.add)
                    m = Fp[7]
                    S.ts("dve", m(), a(), math.pi, -two_pi, op0=ALU.is_gt, op1=ALU.mult)
                    S.tt("dve", a(), a(), m(), ALU.add)
                    S.ts("dve", m(), a(), -math.pi, two_pi, op0=ALU.is_lt, op1=ALU.mult)
                    S.tt("dve", a(), a(), m(), ALU.add)
                    S.ts("dve", a(), a(), math.pi, -math.pi, op0=ALU.min, op1=ALU.max)
                    col = 64 if which == "sin" else 0
                    dst = V(ROT.t[:, half * 8:(half + 1) * 8, col:col + 64], ROT.bufs)
                    S.act(dst, V(a.t[:].rearrange("p (i j) -> p i j", i=8), a.bufs), AF.Sin)

        def ada_phase(l):
            S.dma("sp", MODB.t[:], adab_d[l], writes=MODB.bufs)
            prow = P[5]
            pmod = P[6]
            for g in range(12):
                sl = wget(f"ada{l}_{g}")
                wc = wcols(sl, 512)
                for k in range(8):
                    S.mm(V(prow.t[0:1, :], prow.bufs), CVB(ALL, slice(k, k + 1)), wc(k, 0, 512),
                         start=(k == 0), stop=(k == 7))
                wrel(f"ada{l}_{g}")
                rb = Fp[g % 2]
                S.copy("act", rb(slice(0, 1), ALL), V(prow.t[0:1, :], prow.bufs))
                for j in range(4):
                    cc = g * 4 + j
                    S.mm(V(pmod.t[:, cc:cc + 1], pmod.bufs), rb(slice(0, 1), slice(j * 128, (j + 1) * 128)),
                         C("ones", 0, 1, rows=slice(0, 1)), start=True, stop=True, signal=(j == 3))
            S.tt("dve", MOD(), V(pmod.t[:, 0:48], pmod.bufs), MODB(), ALU.add)
            S.stt(AB(ALL, slice(0, 8)), MOD(ALL, slice(8, 16)), 1.0, NG(ALL, slice(l * 8, l * 8 + 8)), ALU.add, ALU.mult)
            S.stt(AB(ALL, slice(8, 16)), MOD(ALL, slice(32, 40)), 1.0,
                  NG(ALL, slice(DEPTH * 8 + l * 8, DEPTH * 8 + l * 8 + 8)), ALU.add, ALU.mult)

        def norm_stats(s, pss, sqa, sqb, lnv, rstd):
            sl = slice(s * 512, (s + 1) * 512)
            subs = list(range(4 * s, 4 * s + 4))
            for k in range(8):
                q = sqa if k % 2 == 0 else sqb
                S.act(q(), X(ALL, k, sl, sub=subs), AF.Square)
                S.mm(pss(), ones, q(), start=(k == 0), stop=(k == 7))
            S.act(lnv(), pss(), AF.Ln, bias=EPS, scale=1.0 / D)
            S.act(rstd(), lnv(), AF.Exp, scale=-0.5)

        def norm_phase(a_off, b_off):
            for s in range(4):
                sl = slice(s * 512, (s + 1) * 512)
                subs = list(range(4 * s, 4 * s + 4))
                pss = P[s % 2]
                rstd = Fp[3]
                norm_stats(s, pss, Fp[0], Fp[1], Fp[2], rstd)
                for k in range(8):
                    t = Fp[4 + (k % 2)]
                    S.stt(t(), X(ALL, k, sl, sub=subs), AB(ALL, slice(a_off + k, a_off + k + 1)), rstd(), ALU.mult, ALU.mult)
                    S.act(HT(ALL, k, sl, sub=subs), t(), AF.Identity, bias=MOD(ALL, slice(b_off + k, b_off + k + 1)), scale=1.0)

        def out_proj_update(i, wo, oT, g_off, pys):
            wr = wrows(wo)
            tsl = slice(i * 128, (i + 1) * 128)
            for half in range(2):
                py = pys[half]
                for mm_ in range(4):
                    m = half * 4 + mm_
                    o = V(py.t[:, mm_ * 128:(mm_ + 1) * 128], py.bufs)
                    for j in range(4):
                        S.mm(o, wr(j, m * 128, (m + 1) * 128), oT(ALL, slice(j * 128, (j + 1) * 128)),
                             start=(j == 0), stop=(j == 3), signal=(j == 3 and mm_ == 3))
                yield
                for mm_ in range(4):
                    m = half * 4 + mm_
                    o = V(py.t[:, mm_ * 128:(mm_ + 1) * 128], py.bufs)
                    S.stt(X(ALL, m, tsl, sub=i), o, MOD(ALL, slice(g_off + m, g_off + m + 1)), X(ALL, m, tsl, sub=i),
                          ALU.mult, ALU.add)
                yield

        def run_pipelined(tile_gen, depth=2):
            active = []
            nxt = 0
            while nxt < NT or active:
                if nxt < NT and len(active) < depth and all(a_[1] for a_ in active):
                    active.append([tile_gen(nxt), False])
                    nxt += 1
                for idx, a_ in enumerate(list(active)):
                    if a_[1] and idx > 0:
                        continue
                    try:
                        r = next(a_[0])
                        if r == 'B':
                            a_[1] = True
                    except StopIteration:
                        active.remove(a_)

        r3 = lambda v: V(v.ap.rearrange("p (h t) -> p h t", h=4), v.bufs)
        r2 = lambda v: V(v.ap.rearrange("p (a t) -> p a t", a=2), v.bufs)
        r4 = lambda v: V(v.ap.rearrange("p (h a j) -> p h a j", h=4, a=2), v.bufs)

        def gla_pass(l):
            S.dma("pool", WGA.t[:], win_d[l].rearrange("(k p) c -> p k c", p=128)[:, :, 1536:1552], writes=WGA.bufs)
            S.dma("pool", WAL.t[0:17, :], wal_d[l], writes=WAL.bufs)
            S.dma("sp", BCP.t[:], bc_d[l], writes=BCP.bufs)
            wqk = wcols(wget(f"gqk{l}"), 512)
            wv = wcols(wget(f"gv{l}"), 512)
            wr_ = wcols(wget(f"gr{l}"), 512)
            wo = wget(f"wog{l}")
            Sst = Fp[7]
            SbA, SbB = Hp[11], Hp[12]
            S.memset("dve", Sst(ALL, slice(0, 256)), 0.0)
            S.memset("dve", SbA(ALL, slice(0, 256)), 0.0)
            hm = lambda j: C("hm", j, j + 1)
            c0b = V(C("c0").ap.unsqueeze(1).broadcast_to([128, 2, 128]), CST.bufs)
            c1b = V(C("c1").ap.unsqueeze(1).broadcast_to([128, 2, 128]), CST.bufs)
            m1b = V(C("m1").ap.unsqueeze(1).broadcast_to([128, 4, 128]), CST.bufs)
            m2b = V(C("m2").ap.unsqueeze(1).broadcast_to([128, 4, 128]), CST.bufs)

            def tile(i):
                par = i % 2
                tsl = slice(i * 128, (i + 1) * 128)
                hT = lambda k: HT(ALL, k, tsl, sub=i)
                vbf = Hp[0] if par == 0 else Hp[14]
                ku = Hp[3] if par == 0 else Hp[15]
                QM0 = Hp[10] if par == 0 else Hp[16]
                QM1 = Hp[13] if par == 0 else Hp[17]
                scT = Hp[7] if par == 0 else Hp[18]
                sg = Fp[3] if par == 0 else Fp[6]
                pga = V(P[0].t[0:16, 0:128], P[0].bufs)
                for k in range(8):
                    S.mm(pga, V(WGA.t[:, k, :], WGA.bufs), hT(k), start=(k == 0), stop=(k == 7))
                S.copy("act", GAT(slice(0, 16), ALL), pga)
                pqk, pv = P[2], P[3]
                for k in range(8):
                    S.mm(pv(), hT(k), wv(k, 0, 512), start=(k == 0), stop=(k == 7))
                yield
                pz = V(P[0].t[:, 256:512], P[0].bufs)
                S.mm(pz, GAT(slice(0, 17), ALL), WAL(slice(0, 17), ALL))
                for k in range(8):
                    S.mm(pqk(), hT(k), wqk(k, 0, 512), start=(k == 0), stop=(k == 7))
                e_, sp_ = V(Fp[0].t[:, 0:256], Fp[0].bufs), V(Fp[0].t[:, 256:512], Fp[0].bufs)
                S.act(e_, pz, AF.Exp, scale=-1.0)
                S.act(sp_, e_, AF.Ln, bias=1.0, scale=1.0)
                S.copy("act", vbf(), pv())
                yield
                pb = V(P[1].t[:, 0:256], P[1].bufs)
                pd = V(P[1].t[:, 256:512], P[1].bufs)
                S.mm(pb, C("tri"), sp_)
                S.mm(pd, C("dm"), sp_)
                for p_ in range(2):
                    S.mm(V(P[0].t[:, 128 + 2 * p_:130 + 2 * p_], P[0].bufs),
                         V(Fp[0].t[:, 256 + p_ * 128:256 + (p_ + 1) * 128], Fp[0].bufs), C("cind"))
                pbl = V(P[0].t[:, 128:132], P[0].bufs)
                pr = P[3]
                for k in range(8):
                    S.mm(pr(), hT(k), wr_(k, 0, 512), start=(k == 0), stop=(k == 7))
                eb, enb = V(Fp[1].t[:, 0:256], Fp[1].bufs), V(Fp[1].t[:, 256:512], Fp[1].bufs)
                eD = V(Fp[2].t[:, 0:256], Fp[2].bufs)
                dec = V(SM.t[:, 32 + 4 * par:36 + 4 * par], SM.bufs)
                S.act(eb, pb, AF.Exp)
                S.act(enb, pb, AF.Exp, scale=-1.0)
                S.act(eD, pd, AF.Exp)
                S.act(dec, pbl, AF.Exp)
                yield
                QKa, QKb = Hp[1], Hp[2]
                pq_ = V(pqk.t[:, 0:256], pqk.bufs)
                pk_ = V(pqk.t[:, 256:512], pqk.bufs)
                S.stt(QKa(ALL, slice(0, 256)), pq_, 0.125, eb, ALU.mult, ALU.mult)
                S.tt("dve", QKa(ALL, slice(256, 512)), pk_, enb, ALU.mult)
                S.stt(QKb(ALL, slice(0, 256)), pq_, 0.125, enb, ALU.mult, ALU.mult)
                S.tt("dve", QKb(ALL, slice(256, 512)), pk_, eb, ALU.mult)
                for c in range(2):
                    S.stt(ku(ALL, slice(c * 256, (c + 1) * 256)), pk_, hm(c), eD, ALU.mult, ALU.mult)
                S.act(sg(), pr(), AF.Silu)
                yield
                S.tt("dve", sg(), sg(), BCP(ALL, slice(0, 512)), ALU.mult)
                ptb = pbf(None)
                for src_i, src in enumerate((QKa, QKb)):
                    for j in range(4):
                        jj = src_i * 4 + j
                        S.tr(V(ptb.ap[:, jj * 128:(jj + 1) * 128], ptb.bufs), src(ALL, slice(j * 128, (j + 1) * 128)), IDB(),
                             signal=(j == 3))
                yield
                KT, QF, QB = Hp[4], Hp[5], Hp[6]
                S.copy("act", KT(ALL, slice(0, 256)), V(ptb.ap[:, 256:512], ptb.bufs))
                S.copy("act", KT(ALL, slice(256, 512)), V(ptb.ap[:, 768:1024], ptb.bufs))
                qfT = V(ptb.ap[:, 0:256], ptb.bufs)
                qbT = V(ptb.ap[:, 512:768], ptb.bufs)
                for hl in range(2):
                    hsl = slice(hl * 256, (hl + 1) * 256)
                    S.ts("dve", QF(ALL, hsl), qfT, hm(hl), None, op0=ALU.mult)
                    S.ts("dve", QB(ALL, hsl), qbT, hm(hl), None, op0=ALU.mult)
                    S.stt(r2(QM0(ALL, hsl)), r2(qfT), hm(hl), c0b, ALU.mult, ALU.mult)
                    S.stt(r2(QM1(ALL, hsl)), r2(qfT), hm(hl), c1b, ALU.mult, ALU.mult)
                yield
                psf, psb = P[2], P[3]
                for h in range(4):
                    pr2, hl = h // 2, h % 2
                    S.mm(V(psf.t[:, h * 128:(h + 1) * 128], psf.bufs), KT(ALL, slice(pr2 * 128, (pr2 + 1) * 128)),
                         QF(ALL, slice(hl * 256 + pr2 * 128, hl * 256 + (pr2 + 1) * 128)), signal=(h == 3))
                for h in range(4):
                    pr2, hl = h // 2, h % 2
                    S.mm(V(psb.t[:, h * 128:(h + 1) * 128], psb.bufs), KT(ALL, slice(256 + pr2 * 128, 256 + (pr2 + 1) * 128)),
                         QB(ALL, slice(hl * 256 + pr2 * 128, hl * 256 + (pr2 + 1) * 128)), signal=(h == 3))
                yield
                mm1, mm2 = Fp[4], Fp[5]
                S.tt("dve", r3(mm1()), r3(psf()), m1b, ALU.mult)
                S.tt("dve", r3(mm2()), r3(psb()), m2b, ALU.mult)
                S.tt("dve", scT(), mm1(), mm2(), ALU.add)
                yield 'B'
                po, pu = P[4], P[5]
                for h in range(4):
                    S.mm(V(po.t[:, h * 128:(h + 1) * 128], po.bufs), scT(ALL, slice(h * 128, (h + 1) * 128)),
                         vbf(ALL, slice(h * 128, (h + 1) * 128)), start=(h == 0), stop=False, signal=False)
                for c in range(2):
                    Sb = SbA if c == 0 else SbB
                    QM = QM0 if c == 0 else QM1
                    for h in range(4):
                        pr2, hl = h // 2, h % 2
                        S.mm(V(po.t[:, h * 128:(h + 1) * 128], po.bufs),
                             QM(ALL, slice(hl * 256 + pr2 * 128, hl * 256 + (pr2 + 1) * 128)),
                             Sb(ALL, slice(pr2 * 128, (pr2 + 1) * 128)),
                             start=False, stop=(c == 1 and h == 3), signal=(c == 1 and h == 3))
                    for pr2 in range(2):
                        S.mm(V(pu.t[:, pr2 * 256:(pr2 + 1) * 256], pu.bufs),
                             ku(ALL, slice(c * 256 + pr2 * 128, c * 256 + (pr2 + 1) * 128)),
                             vbf(ALL, slice(pr2 * 256, (pr2 + 1) * 256)), signal=(pr2 == 1))
                    yield
                    for pr2 in range(2):
                        for hl in range(2):
                            rows = slice(hl * 64, hl * 64 + 64)
                            sv = Sst(rows, slice(pr2 * 128, (pr2 + 1) * 128))
                            S.stt(sv, sv, V(SM.t[rows, 32 + 4 * par + 2 * pr2 + c:33 + 4 * par + 2 * pr2 + c], SM.bufs),
                                  V(pu.t[rows, pr2 * 256 + hl * 128:pr2 * 256 + (hl + 1) * 128], pu.bufs),
                                  ALU.mult, ALU.add)
                    Sn = SbB if c == 0 else SbA
                    S.copy("act", Sn(ALL, slice(0, 256)), Sst(ALL, slice(0, 256)))
                    yield
                sq = Fp[4]
                S.act(sq(), po(), AF.Square)
                ss = V(SM.t[:, 40:44], SM.bufs)
                S.reduce(ss, r3(sq()), ALU.add)
                yield
                lv = V(SM.t[:, 44:48], SM.bufs)
                rs = V(SM.t[:, 48:52], SM.bufs)
                S.act(lv, ss, AF.Ln, bias=EPS, scale=1.0 / 128)
                S.act(rs, lv, AF.Exp, scale=-0.5)
                yield
                og = Hp[8]
                for h in range(4):
                    hs = slice(h * 128, (h + 1) * 128)
                    S.stt(og(ALL, hs), V(po.t[:, hs], po.bufs), V(SM.t[:, 48 + h:49 + h], SM.bufs), sg(ALL, hs),
                          ALU.mult, ALU.mult)
                yield
                pt2 = pbf(None, 512)
                for h in range(4):
                    S.tr(V(pt2.ap[:, h * 128:(h + 1) * 128], pt2.bufs), og(ALL, slice(h * 128, (h + 1) * 128)), IDB(),
                         signal=(h == 3))
                ogT = Hp[9]
                S.copy("act", ogT(), pt2)
                yield
                yield from out_proj_update(i, wo, ogT, 16, (P[5], P[6]))

            run_pipelined(tile)
            for n_ in (f"gqk{l}", f"gv{l}", f"gr{l}", f"wog{l}"):
                wrel(n_)

        def ret_pass(l):
            if "gla" not in parts:
                S.dma("sp", BCP.t[:], bc_d[l], writes=BCP.bufs)
            wq = wcols(wget(f"rq{l}"), 512)
            wk = wcols(wget(f"rk{l}"), 512)
            wv = wcols(wget(f"rv{l}"), 512)
            wg = wcols(wget(f"rg{l}"), 512)
            wo = wget(f"wor{l}")
            R = Fp[7]
            RbA, RbB = Hp[11], Hp[12]
            S.memset("dve", R(), 0.0)
            S.memset("dve", RbA(), 0.0)

            def tile(i):
                par = i % 2
                tsl = slice(i * 128, (i + 1) * 128)
                hT = lambda k: HT(ALL, k, tsl, sub=i)
                vbf = Hp[0] if par == 0 else Hp[14]
                kdc = (Hp[3], Hp[13]) if par == 0 else (Hp[15], Hp[16])
                qd0 = Hp[6] if par == 0 else Hp[17]
                qd1 = Hp[7] if par == 0 else Hp[18]
                scT = Hp[8] if par == 0 else Hp[19]
                sg = Fp[3] if par == 0 else Fp[6]
                so = 0 if par == 0 else 24
                pq, pk, pv, pg = P[0], P[1], P[2], P[3]
                for (pp, w) in ((pq, wq), (pk, wk)):
                    for k in range(8):
                        S.mm(pp(), hT(k), w(k, 0, 512), start=(k == 0), stop=(k == 7))
                    yield
                cosb = V(ROT.t[:, i, 0:64].unsqueeze(1).unsqueeze(1).broadcast_to([128, 4, 2, 64]), ROT.bufs)
                sinb = V(ROT.t[:, i, 64:128].unsqueeze(1).broadcast_to([128, 4, 64]), ROT.bufs)
                qr, kr = Hp[1], Hp[2]
                for n_, (pp, dst) in enumerate(((pq, qr), (pk, kr))):
                    a_, b_ = Fp[0], Fp[1]
                    S.tt("dve", r4(a_()), r4(pp()), cosb, ALU.mult)
                    p4 = r4(pp())
                    b4 = r4(b_())
                    S.tt("dve", V(b4.ap[:, :, 0, :], b_.bufs), V(p4.ap[:, :, 1, :], pp.bufs), sinb, ALU.mult)
                    S.tt("dve", V(b4.ap[:, :, 1, :], b_.bufs), V(p4.ap[:, :, 0, :], pp.bufs), sinb, ALU.mult)
                    if n_ == 0:
                        for k in range(8):
                            S.mm(pv(), hT(k), wv(k, 0, 512), start=(k == 0), stop=(k == 7))
                    else:
                        for k in range(8):
                            S.mm(pg(), hT(k), wg(k, 0, 512), start=(k == 0), stop=(k == 7))
                    yield
                    a4 = r4(a_())
                    d4 = r4(dst())
                    S.tt("dve", V(d4.ap[:, :, 0, :], dst.bufs), V(a4.ap[:, :, 0, :], a_.bufs), V(b4.ap[:, :, 0, :], b_.bufs),
                         ALU.subtract)
                    S.tt("dve", V(d4.ap[:, :, 1, :], dst.bufs), V(a4.ap[:, :, 1, :], a_.bufs), V(b4.ap[:, :, 1, :], b_.bufs),
                         ALU.add)
                    if n_ == 0:
                        S.copy("act", vbf(), pv())
                    else:
                        S.act(sg(), pg(), AF.Silu)
                    yield
                ptq = pbf(None, 512)
                ptk = V(PB.t[:, 512:1024], PB.bufs)
                for h in range(4):
                    hs = slice(h * 128, (h + 1) * 128)
                    S.tr(V(ptq.ap[:, hs], ptq.bufs), qr(ALL, hs), IDB(), signal=(h == 3))
                for h in range(4):
                    hs = slice(h * 128, (h + 1) * 128)
                    S.tr(V(ptk.ap[:, hs], ptk.bufs), kr(ALL, hs), IDB(), signal=(h == 3))
                for c in range(2):
                    for h in range(4):
                        hs = slice(h * 128, (h + 1) * 128)
                        S.ts("dve", kdc[c](ALL, hs), kr(ALL, hs), C("kdec", 4 * c + h, 4 * c + h + 1), None, op0=ALU.mult)
                yield
                qT, kT = Hp[4], Hp[5]
                S.copy("act", qT(), ptq)
                S.copy("act", kT(), ptk)
                S.tt("dve", qd0(), ptq, C("qd0"), ALU.mult)
                S.tt("dve", qd1(), ptq, C("qd1"), ALU.mult)
                yield
                ps = P[0]
                for h in range(4):
                    hs = slice(h * 128, (h + 1) * 128)
                    S.mm(V(ps.t[:, hs], ps.bufs), kT(ALL, hs), qT(ALL, hs), signal=(h == 3))
                yield
                S.tt("dve", scT(), ps(), C("dret"), ALU.mult)
                yield 'B'
                po, pu = P[4], P[5]
                for h in range(4):
                    hs = slice(h * 128, (h + 1) * 128)
                    S.mm(V(po.t[:, hs], po.bufs), scT(ALL, hs), vbf(ALL, hs), start=(h == 0), stop=False, signal=False)
                for c in range(2):
                    Rb = RbA if c == 0 else RbB
                    qd = qd0 if c == 0 else qd1
                    for h in range(4):
                        hs = slice(h * 128, (h + 1) * 128)
                        S.mm(V(po.t[:, hs], po.bufs), qd(ALL, hs), Rb(ALL, hs), start=False, stop=(c == 1 and h == 3),
                             signal=(c == 1 and h == 3))
                    for h in range(4):
                        hs = slice(h * 128, (h + 1) * 128)
                        S.mm(V(pu.t[:, hs], pu.bufs), kdc[c](ALL, hs), vbf(ALL, hs), signal=(h == 3))
                    yield
                    for h in range(4):
                        hs = slice(h * 128, (h + 1) * 128)
                        S.stt(R(ALL, hs), R(ALL, hs), float(GAMMA[h] ** 64), V(pu.t[:, hs], pu.bufs), ALU.mult, ALU.add)
                    Rn = RbB if c == 0 else RbA
                    S.copy("act", Rn(), R())
                    yield
                sm = lambda a, b: V(SM.t[:, so + a:so + b], SM.bufs)
                s1, s2, mean, var, rs, nmr = sm(0, 4), sm(4, 8), sm(8, 12), sm(12, 16), sm(16, 20), sm(20, 24)
                S.reduce(s1, r3(po()), ALU.add)
                sq = Fp[4]
                S.act(sq(), po(), AF.Square)
                yield
                S.reduce(s2, r3(sq()), ALU.add)
                S.ts("dve", mean, s1, 1.0 / 128, None, op0=ALU.mult)
                S.tt("dve", var, mean, mean, ALU.mult)
                S.stt(var, s2, 1.0 / 128, var, ALU.mult, ALU.subtract)
                S.ts("dve", var, var, 0.0, None, op0=ALU.max)
                yield
                S.act(var, var, AF.Ln, bias=EPS, scale=1.0)
                S.act(rs, var, AF.Exp, scale=-0.5)
                yield
                S.stt(nmr, mean, -1.0, rs, ALU.mult, ALU.mult)
                on = Fp[5]
                for h in range(4):
                    hs = slice(h * 128, (h + 1) * 128)
                    S.act(on(ALL, hs), V(po.t[:, hs], po.bufs), AF.Identity, bias=V(SM.t[:, so + 20 + h:so + 21 + h], SM.bufs),
                          scale=V(SM.t[:, so + 16 + h:so + 17 + h], SM.bufs))
                yield
                S.tt("dve", on(), on(), BCP(ALL, slice(512, 1024)), ALU.mult)
                S.tt("dve", on(), on(), BCP(ALL, slice(1024, 1536)), ALU.add)
                orr = Hp[9]
                S.tt("dve", orr(), on(), sg(), ALU.mult)
                yield
                pt2 = pbf(None, 512)
                for h in range(4):
                    hs = slice(h * 128, (h + 1) * 128)
                    S.tr(V(pt2.ap[:, hs], pt2.bufs), orr(ALL, hs), IDB(), signal=(h == 3))
                orT = Hp[10]
                S.copy("act", orT(), pt2)
                yield
                yield from out_proj_update(i, wo, orT, 16, (P[5], P[6]))

            run_pipelined(tile)
            for n_ in (f"rq{l}", f"rk{l}", f"rv{l}", f"rg{l}", f"wor{l}"):
                wrel(n_)

        def ffn_group(names, nch, gate):
            n1, n3, n2 = names
            w1 = wcols(wget(n1), nch * 128)
            w3 = wcols(wget(n3), nch * 128)
            w2 = wrows(wget(n2))
            hb = ((P[4], P[5]), (P[6], None))
            py = P[0:4]

            def pbank(j, which):
                p = hb[j % 2][which]
                if p is None:
                    return V(PB.t[:].bitcast(F32), PB.bufs)
                return p()

            def up(s, j):
                ssl = slice(s * 512, (s + 1) * 512)
                subs = list(range(4 * s, 4 * s + 4))
                ph1, ph3 = pbank(j, 0), pbank(j, 1)
                for (ph, w) in ((ph1, w1), (ph3, w3)):
                    for k in range(8):
                        S.mm(ph, w(k, j * 128, (j + 1) * 128), HT(ALL, k, ssl, sub=subs), start=(k == 0), stop=(k == 7))
                s_ = Fp[j % 4]
                a_ = Hp[(s % 2) * 4 + j]
                S.act(s_(), ph1, AF.Silu)
                if gate is not None:
                    S.tt("dve", s_(), s_(), gate(s), ALU.mult)
                S.tt("dve", a_(), ph3, s_(), ALU.mult)

            def down(s, half):
                tsl = slice(s * 512 + half * 256, s * 512 + (half + 1) * 256)
                subs = [4 * s + 2 * half, 4 * s + 2 * half + 1]
                for j in range(nch):
                    a_ = Hp[(s % 2) * 4 + j]
                    for m in range(8):
                        o = V(py[m // 2].t[:, (m % 2) * 256:(m % 2 + 1) * 256], py[m // 2].bufs)
                        S.mm(o, w2(j, m * 128, (m + 1) * 128), a_(ALL, slice(half * 256, (half + 1) * 256)),
                             start=(j == 0 and m % 2 == 0), stop=(j == nch - 1 and m % 2 == 1), signal=(j == nch - 1))
                for m in range(8):
                    o = V(py[m // 2].t[:, (m % 2) * 256:(m % 2 + 1) * 256], py[m // 2].bufs)
                    S.stt(X(ALL, m, tsl, sub=subs), o, MOD(ALL, slice(40 + m, 41 + m)), X(ALL, m, tsl, sub=subs),
                          ALU.mult, ALU.add)

            nfirst = (nch + 1) // 2
            for j in range(nch):
                up(0, j)
            for s in range(4):
                down(s, 0)
                if s + 1 < 4:
                    for j in range(nfirst):
                        up(s + 1, j)
                down(s, 1)
                if s + 1 < 4:
                    for j in range(nfirst, nch):
                        up(s + 1, j)
            for n_ in names:
                wrel(n_)

        def dense_ffn(l):
            for gi, (c0, n) in enumerate(ffn_groups(22)):
                ffn_group((f"w1_{l}_{gi}", f"w3_{l}_{gi}", f"w2_{l}_{gi}"), n, None)

        def moe_ffn(l):
            i_ = l // 2
            S.dma("pool", RW.t[:], rw_d[i_].rearrange("(k p) e -> p k e", p=128), writes=RW.bufs)
            for i in range(NT):
                tsl = slice(i * 128, (i + 1) * 128)
                pl = V(P[4 + (i % 2)].t[:, 0:8], P[4 + (i % 2)].bufs)
                for k in range(8):
                    S.mm(pl, HT(ALL, k, tsl, sub=i), V(RW.t[:, k, :], RW.bufs), start=(k == 0), stop=(k == 7))
                sm = lambda a, b: V(SM.t[:, a:b], SM.bufs)
                lg, m1_, eq, l2, m2_, selm, ex, nm1, ssum = (sm(0, 8), sm(8, 9), sm(16, 24), sm(24, 32), sm(9, 10),
                                                              sm(32, 40), sm(40, 48), sm(10, 11), sm(11, 12))
                S.copy("dve", lg, pl)
                S.reduce(m1_, lg, ALU.max)
                S.ts("dve", eq, lg, m1_, None, op0=ALU.is_equal)
                S.stt(l2, eq, -1e30, lg, ALU.mult, ALU.add)
                S.reduce(m2_, l2, ALU.max)
                S.ts("dve", selm, lg, m2_, None, op0=ALU.is_ge)
                S.ts("dve", nm1, m1_, -1.0, None, op0=ALU.mult)
                S.act(ex, lg, AF.Exp, bias=nm1, scale=1.0)
                S.tt("dve", ex, ex, selm, ALU.mult)
                S.reduce(ssum, ex, ALU.add)
                S.op("dve", lambda h: h.reciprocal(out=ssum.ap, in_=ssum.ap), reads=ssum.bufs, writes=ssum.bufs)
                S.ts("dve", V(GATES.t[:, i, :], GATES.bufs), ex, ssum, None, op0=ALU.mult)
            GB = [Fp[4], Fp[5], Fp[6], Fp[7]]
            if debug:
                S.dma("sp", dbg_d[:, 0:128], GATES.t[:].rearrange("p i e -> p (i e)"), reads=GATES.bufs)
            for e in range(NE):
                for s in range(4):
                    pg_ = P[4 + (s % 2)]
                    for j in range(4):
                        i = s * 4 + j
                        gbt = Hp[4 + (i % 2)]
                        gb = lambda gbt=gbt: V(gbt.t[:].bitcast(F32)[:, 0:128], gbt.bufs)
                        S.ts("dve", gb(), ones, V(GATES.t[:, i, e:e + 1], GATES.bufs), None, op0=ALU.mult)
                        S.mm(V(pg_.t[:, j * 128:(j + 1) * 128], pg_.bufs), gb(), ident, signal=(j == 3))
                    S.copy("act", GB[s](), pg_())
                if debug and e == 3:
                    for s in range(4):
                        S.dma("sp", dbg_d[:, 128 + s * 512:128 + (s + 1) * 512], GB[s].t[:], reads=GB[s].bufs)
                gate = lambda s_i: GB[s_i]()
                for gi, (c0, n) in enumerate(ffn_groups(11)):
                    ffn_group((f"w1_{l}_{e}_{gi}", f"w3_{l}_{e}_{gi}", f"w2_{l}_{e}_{gi}"), n, gate)

        for l in layers:
            ada_phase(l)
            if "gla" in parts or "ret" in parts:
                norm_phase(0, 0)
            if "gla" in parts:
                gla_pass(l)
            if "ret" in parts:
                ret_pass(l)
            if "ffn" in parts:
                norm_phase(8, 24)
                if l % 2 == 0:
                    dense_ffn(l)
                else:
                    moe_ffn(l)

        fg0 = 2 * DEPTH * 8
        for s in range(4):
            subs = list(range(4 * s, 4 * s + 4))
            rstd = Fp[3]
            if do_final:
                norm_stats(s, P[s % 2], Fp[0], Fp[1], Fp[2], rstd)
            for j in range(4):
                ti = s * 4 + j
                tsl = slice(ti * 128, (ti + 1) * 128)
                ob = (Fp[4], Fp[5]) if ti % 2 == 0 else (Fp[6], Fp[7])
                for k in range(8):
                    t = Hp[k % 2]
                    tv = V(t.t[:].bitcast(F32)[:, 0:128], t.bufs)
                    if do_final:
                        S.stt(tv, X(ALL, k, tsl, sub=ti), NG(ALL, slice(fg0 + k, fg0 + k + 1)),
                              rstd(ALL, slice(j * 128, (j + 1) * 128)), ALU.mult, ALU.mult)
                    else:
                        S.copy("dve", tv, X(ALL, k, tsl, sub=ti))
                    pt = P[2 + k // 4]
                    S.tr(V(pt.t[:, (k % 4) * 128:(k % 4 + 1) * 128], pt.bufs), tv, ident, signal=(k % 4 == 3))
                    if k % 4 == 3:
                        S.copy("act", ob[k // 4](), pt())
                for hh in range(2):
                    S.dma("sp" if hh == 0 else "act", y_d[ti * 128:(ti + 1) * 128, hh * 512:(hh + 1) * 512], ob[hh].t[:],
                          reads=ob[hh].bufs)
        S.wait_bufs("sp", Fp[4].bufs + Fp[5].bufs + Fp[6].bufs + Fp[7].bufs)
        assert state["next"] == len(jobs), (state["next"], len(jobs))
        build_program.stats = (S.nins, S.nwaits)
    return nc


def _prep_shared(inp):
    f = lambda a: np.ascontiguousarray(np.asarray(a, dtype=np.float32))
    sh = {}
    sh["cst"] = make_consts()
    sh["ada_w"] = f(inp["ada_w"])
    sh["ada_b_fm"] = f(np.asarray(inp["ada_b"], np.float32).reshape(DEPTH, 48, 128).transpose(0, 2, 1))
    ng = np.concatenate([
        np.asarray(inp["norm_mix_g"], np.float32).reshape(DEPTH, 8, 128).transpose(2, 0, 1).reshape(128, DEPTH * 8),
        np.asarray(inp["norm_ffn_g"], np.float32).reshape(DEPTH, 8, 128).transpose(2, 0, 1).reshape(128, DEPTH * 8),
        np.asarray(inp["final_g"], np.float32).reshape(8, 128).T], axis=1)
    sh["norm_g_fm"] = f(ng)
    sh["w_in"] = f(inp["w_in"])
    sh["w_alpha_aug"] = f(np.concatenate([np.asarray(inp["gla_w_alpha"], np.float32),
                                          np.asarray(inp["gla_b_alpha"], np.float32)[:, None, :]], axis=1))
    bc = np.concatenate([np.asarray(inp["gla_norm_g"], np.float32), np.asarray(inp["ret_gn_g"], np.float32),
                         np.asarray(inp["ret_gn_b"], np.float32)], axis=1)
    sh["bc_params"] = f(np.broadcast_to(bc[:, None, :], (DEPTH, 128, 1536)))
    sh["w_out"] = f(inp["w_out"])
    for k_ in ("ffn_w1", "ffn_w3", "ffn_w2", "router_w", "moe_w1", "moe_w3", "moe_w2"):
        sh[k_] = f(inp[k_])
    return sh


def _prep_core(inp, b):
    d = {}
    d["x"] = np.ascontiguousarray(np.asarray(inp["x"][b], np.float32))
    d["cvec"] = np.ascontiguousarray(np.asarray(inp["c"][b], np.float32).reshape(8, 128).T)
    d["pos"] = np.ascontiguousarray(np.asarray(inp["positions"][b]).astype(np.int32).reshape(NT, 128).T)
    return d


_CACHE = {}


def kernel(**inputs):
    key = "full"
    if key not in _CACHE:
        _CACHE[key] = build_program()
    nc = _CACHE[key]
    sh = _prep_shared(inputs)
    in_maps = []
    for b in range(8):
        d = dict(sh)
        d.update(_prep_core(inputs, b))
        in_maps.append(d)
    res = run_bass_kernel_spmd(nc, in_maps, core_ids=list(range(8)))
    out = np.stack([np.asarray(res.results[b]["y"], np.float32) for b in range(8)], axis=0)
    return out
```

```python
import math
from contextlib import ExitStack

import numpy as np
import concourse.bass as bass
import concourse.mybir as mybir
from concourse.bass_utils import run_bass_kernel_spmd

F32 = mybir.dt.float32
BF16 = mybir.dt.bfloat16
I32 = mybir.dt.int32
AF = mybir.ActivationFunctionType
ALU = mybir.AluOpType
AX = mybir.AxisListType

D = 1024
SEQ = 2048
NT = 16
DEPTH = 4
D_FF = 2816
NE = 8
D_FFE = 1408
EPS = 1e-6
ALL = slice(None)
import os as _os
_DBG_STOP = int(_os.environ.get('DBG_STOP', '0'))


class Buf:
    __slots__ = ("name", "w", "r", "excl")

    def __init__(self, name, excl=False):
        self.name = name
        self.w = None
        self.r = {}
        self.excl = excl


class V:
    __slots__ = ("ap", "bufs")

    def __init__(self, ap, bufs):
        self.ap = ap
        self.bufs = bufs


class T:
    def __init__(self, tensor, name, nsub=1, excl=False):
        self.t = tensor
        self.name = name
        self.bufs = [Buf(f"{name}.{i}", excl) for i in range(nsub)]

    def __call__(self, *idx, sub=None):
        ap = self.t[idx] if idx else self.t[:]
        if sub is None:
            bufs = self.bufs
        elif isinstance(sub, int):
            bufs = [self.bufs[sub]]
        else:
            bufs = [self.bufs[s] for s in sub]
        return V(ap, bufs)


class Eng:
    def __init__(self, key, h, sem):
        self.key = key
        self.h = h
        self.sem = sem
        self.seq = 0
        self.cnt = 0
        self.last = None
        self.last_inc = False
        self.miles = []
        self.seen = {}


class Sync:
    def __init__(self, nc, sems, dma_sems):
        self.nc = nc
        keys = [("pe", nc.tensor), ("act", nc.scalar), ("dve", nc.vector),
                ("pool", nc.gpsimd), ("sp", nc.sync)]
        self.E = {}
        for (k, h), sm in zip(keys, sems):
            self.E[k] = Eng(k, h, sm)
        self.dsem = dma_sems
        self.dval = {q: [0] * len(v) for q, v in dma_sems.items()}
        self.drr = {q: 0 for q in dma_sems}
        self.nwaits = 0
        self.nins = 0

    def _resolve(self, tok):
        if tok[0] == 'd':
            return ('d', tok[1], tok[2]), self.dsem[tok[1]][tok[2]], tok[3]
        e = self.E[tok[1]]
        n = tok[2]
        val = None
        for (sq, v) in reversed(e.miles):
            if sq >= n:
                val = v
            else:
                break
        if val is None:
            assert e.seq >= n and e.last is not None
            if not e.last_inc:
                e.cnt += 1
                e.last.then_inc(e.sem, 1)
                e.last_inc = True
                e.miles.append((e.seq, e.cnt))
            val = e.cnt
        return ('e', e.key), e.sem, val

    def _need(self, eng, toks, raw=()):
        e = self.E[eng]
        need = {}
        for lst in (toks, raw):
            for t in lst:
                if t is None:
                    continue
                if t[0] == 'e' and t[1] == eng and eng == "pe":
                    continue
                sk, sh, v = self._resolve(t)
                if e.seen.get(sk, 0) >= v:
                    continue
                if sk not in need or need[sk][1] < v:
                    need[sk] = (sh, v)
        return [(sk, sh, v) for sk, (sh, v) in need.items()]

    def _wait(self, eng, toks, raw=(), keep_one=False):
        e = self.E[eng]
        need = self._need(eng, toks, raw)
        kept = None
        if keep_one and need:
            kept = need.pop()
        for sk, sh, v in need:
            e.h.wait_ge(sh, v)
            e.seen[sk] = v
            self.nwaits += 1
        if kept is not None:
            e.seen[kept[0]] = kept[2]
        return kept

    @staticmethod
    def _deps(reads, writes):
        toks = []
        raw = []
        for b in reads:
            raw.append(b.w)
            if b.excl:
                toks.extend(b.r.values())
        for b in writes:
            toks.append(b.w)
            toks.extend(b.r.values())
        return toks, raw

    @staticmethod
    def _commit(tok, reads, writes):
        key = tok[:2] if tok[0] == 'e' else tok[:3]
        for b in reads:
            b.r[key] = tok
        for b in writes:
            b.w = tok
            b.r = {}

    def op(self, eng, fn, reads=(), writes=(), signal=None):
        e = self.E[eng]
        if signal is None:
            signal = eng != "pe"
        kept = self._wait(eng, *self._deps(reads, writes), keep_one=(eng != "pe"))
        ins = fn(e.h)
        if kept is not None:
            ins._wait_ge(kept[1], kept[2])
        e.seq += 1
        self.nins += 1
        e.last = ins
        e.last_inc = False
        if signal:
            e.cnt += 1
            ins.then_inc(e.sem, 1)
            e.last_inc = True
            e.miles.append((e.seq, e.cnt))
            if len(e.miles) > 4096:
                e.miles = e.miles[-2048:]
        tok = ('e', eng, e.seq)
        self._commit(tok, reads, writes)
        return tok

    def dma(self, q, out, in_, reads=(), writes=(), **kw):
        e = self.E[q]
        i = self.drr[q]
        self.drr[q] = (i + 1) % len(self.dsem[q])
        toks, raw = self._deps(reads, writes)
        if self.dval[q][i] > 0:
            toks.append(('d', q, i, self.dval[q][i]))
        self._wait(q, toks, raw)
        ins = e.h.dma_start(out=out, in_=in_, **kw)
        ins.then_inc(self.dsem[q][i], 16)
        self.dval[q][i] += 16
        self.nins += 1
        tok = ('d', q, i, self.dval[q][i])
        self._commit(tok, reads, writes)
        return tok

    def wait_bufs(self, eng, bufs):
        toks = []
        for b in bufs:
            toks.append(b.w)
            toks.extend(b.r.values())
        self._wait(eng, toks)

    @staticmethod
    def _rb(*vs):
        out = []
        for v in vs:
            if isinstance(v, V):
                out.extend(v.bufs)
        return out

    @staticmethod
    def _a(v):
        return v.ap if isinstance(v, V) else v

    def mm(self, out, lhsT, rhs, start=True, stop=True, signal=None, **kw):
        if signal is None:
            signal = bool(stop)
        return self.op("pe", lambda h: h.matmul(out.ap, lhsT.ap, rhs.ap, start=start, stop=stop, **kw),
                       reads=self._rb(lhsT, rhs), writes=out.bufs, signal=signal)

    def tr(self, out, in_, ident, signal=True):
        return self.op("pe", lambda h: h.transpose(out.ap, in_.ap, ident.ap),
                       reads=self._rb(in_, ident), writes=out.bufs, signal=signal)

    def act(self, out, in_, func, bias=None, scale=None):
        kw = {}
        if bias is not None:
            kw["bias"] = self._a(bias)
        if scale is not None:
            kw["scale"] = self._a(scale)
        return self.op("act", lambda h: h.activation(out=out.ap, in_=in_.ap, func=func, **kw),
                       reads=self._rb(in_, bias, scale), writes=out.bufs)

    def tt(self, eng, out, in0, in1, op):
        return self.op(eng, lambda h: h.tensor_tensor(out=out.ap, in0=in0.ap, in1=in1.ap, op=op),
                       reads=self._rb(in0, in1), writes=out.bufs)

    def ts(self, eng, out, in0, s1, s2=None, op0=ALU.mult, op1=None):
        kw = {}
        if op1 is not None:
            kw["op1"] = op1
        return self.op(eng, lambda h: h.tensor_scalar(out=out.ap, in0=in0.ap, scalar1=self._a(s1),
                                                      scalar2=self._a(s2), op0=op0, **kw),
                       reads=self._rb(in0, s1, s2), writes=out.bufs)

    def stt(self, out, in0, scalar, in1, op0, op1):
        return self.op("dve", lambda h: h.scalar_tensor_tensor(out=out.ap, in0=in0.ap, scalar=self._a(scalar),
                                                               in1=in1.ap, op0=op0, op1=op1),
                       reads=self._rb(in0, scalar, in1), writes=out.bufs)

    def copy(self, eng, out, in_):
        if eng == "act":
            return self.op(eng, lambda h: h.copy(out=out.ap, in_=in_.ap), reads=in_.bufs, writes=out.bufs)
        return self.op(eng, lambda h: h.tensor_copy(out=out.ap, in_=in_.ap), reads=in_.bufs, writes=out.bufs)

    def reduce(self, out, in_, op, axis=AX.X):
        return self.op("dve", lambda h: h.tensor_reduce(out=out.ap, in_=in_.ap, axis=axis, op=op),
                       reads=in_.bufs, writes=out.bufs)

    def memset(self, eng, out, val):
        return self.op(eng, lambda h: h.memset(out.ap, val), writes=out.bufs)


class CL:
    off = {}
    n = 0

    @classmethod
    def add(cls, name, w):
        cls.off[name] = (cls.n, cls.n + w)
        cls.n += w


for _n, _w in [("ident", 128), ("ones", 128), ("tri", 128), ("dm", 128), ("cind", 2), ("m1", 128), ("m2", 128),
               ("dret", 512), ("qd0", 512), ("qd1", 512), ("kdec", 8), ("invf", 64), ("c0", 128), ("c1", 128), ("hm", 2)]:
    CL.add(_n, _w)

GAMMA = [1.0 - 2.0 ** (-5.0 - h) for h in range(4)]


def make_consts():
    c = np.zeros((128, CL.n), np.float64)

    def put(name, arr):
        a, b = CL.off[name]
        c[:, a:b] = np.asarray(arr, np.float64).reshape(128, b - a)

    s = np.arange(128)[:, None]
    t = np.arange(128)[None, :]
    same = (s // 64) == (t // 64)
    put("ident", np.eye(128))
    put("ones", np.ones((128, 128)))
    put("tri", np.where(same & (s <= t), -1.0 / 16, 0.0))
    put("dm", np.where(same & (s > t), -1.0 / 16, 0.0))
    put("cind", np.where((s // 64) == np.arange(2)[None, :], -1.0 / 16, 0.0))
    put("m1", np.where(same & (t >= s), 1.0, 0.0))
    put("m2", np.where(same & (t < s), 1.0, 0.0))
    dret = np.zeros((128, 4, 128))
    qd0 = np.zeros((128, 4, 128))
    qd1 = np.zeros((128, 4, 128))
    kdec = np.zeros((128, 8))
    tt = np.arange(128)
    for h in range(4):
        lg = math.log(GAMMA[h])
        dret[:, h, :] = np.where(same, np.exp(lg * np.abs(t - s)), 0.0) * 128 ** -0.5
        qd = np.exp(lg * ((tt % 64) + 1)) * 128 ** -0.5
        qd0[:, h, :] = np.where(tt < 64, qd, 0.0)[None, :]
        qd1[:, h, :] = np.where(tt >= 64, qd, 0.0)[None, :]
        kdec[:, h] = np.where(tt < 64, np.exp(lg * (63 - (tt % 64))), 0.0)
        kdec[:, 4 + h] = np.where(tt >= 64, np.exp(lg * (63 - (tt % 64))), 0.0)
    put("dret", dret)
    put("qd0", qd0)
    put("qd1", qd1)
    put("kdec", kdec)
    invf = 10000.0 ** (-np.arange(64) / 64.0)
    put("invf", np.broadcast_to(invf[None, :], (128, 64)))
    put("c0", np.broadcast_to((tt < 64)[None, :], (128, 128)))
    put("c1", np.broadcast_to((tt >= 64)[None, :], (128, 128)))
    put("hm", np.stack([tt < 64, tt >= 64], axis=1))
    return c.astype(np.float32)


def build_program(layers=(0, 1, 2, 3), do_final=True, parts=("gla", "ret", "ffn"), nslots=6, debug=False):
    nc = bass.Bass("TRN2", target_bir_lowering=False)

    def din(name, shape, dt=F32):
        return nc.dram_tensor(name, list(shape), dt, kind="ExternalInput").ap()

    x_d = din("x", [SEQ, D])
    c_d = din("cvec", [128, 8])
    pos_d = din("pos", [128, NT], I32)
    cst_d = din("cst", [128, CL.n])
    adaw_d = din("ada_w", [DEPTH, D, 6 * D])
    adab_d = din("ada_b_fm", [DEPTH, 128, 48])
    ng_d = din("norm_g_fm", [128, 2 * DEPTH * 8 + 8])
    win_d = din("w_in", [DEPTH, D, 3600])
    wal_d = din("w_alpha_aug", [DEPTH, 17, 256])
    bc_d = din("bc_params", [DEPTH, 128, 1536])
    wout_d = din("w_out", [DEPTH, D, D])
    w1_d = din("ffn_w1", [2, D, D_FF])
    w3_d = din("ffn_w3", [2, D, D_FF])
    w2_d = din("ffn_w2", [2, D_FF, D])
    rw_d = din("router_w", [2, D, NE])
    m1_d = din("moe_w1", [2, NE, D, D_FFE])
    m3_d = din("moe_w3", [2, NE, D, D_FFE])
    m2_d = din("moe_w2", [2, NE, D_FFE, D])
    y_d = nc.dram_tensor("y", [SEQ, D], F32, kind="ExternalOutput").ap()
    dbg_d = nc.dram_tensor("dbg", [128, 4096], F32, kind="ExternalOutput").ap() if debug else None

    with ExitStack() as st:
        def sb(name, shape, dt, nsub=1):
            return T(st.enter_context(nc.sbuf_tensor(name, list(shape), dt)), name, nsub)

        X = sb("X", [128, 8, SEQ], F32, NT)
        HT = sb("HT", [128, 8, SEQ], BF16, NT)
        CST = sb("CST", [128, CL.n], F32)
        SLOT = [sb(f"slot{i}", [128, 4096], BF16) for i in range(nslots)]
        Fp = [sb(f"F{i}", [128, 512], F32) for i in range(8)]
        Hp = [sb(f"H{i}", [128, 512], BF16) for i in range(20)]
        NG = sb("NG", [128, 2 * DEPTH * 8 + 8], F32)
        MOD = sb("MOD", [128, 48], F32)
        MODB = sb("MODB", [128, 48], F32)
        AB = sb("AB", [128, 16], F32)
        CV = sb("CV", [128, 8], F32)
        CVB = sb("CVB", [128, 8], BF16)
        IDB = sb("IDB", [128, 128], BF16)
        WGA = sb("WGA", [128, 8, 16], BF16)
        WAL = sb("WAL", [32, 256], BF16)
        GAT = sb("GAT", [32, 128], BF16)
        BCP = sb("BCP", [128, 1536], F32)
        ROT = sb("ROT", [128, NT, 128], F32)
        SM = sb("SM", [128, 64], F32)
        POSI = sb("POSI", [128, NT], I32)
        RW = sb("RW", [128, 8, 8], BF16)
        GATES = sb("GATES", [128, NT, 8], F32)
        P = [T(st.enter_context(nc.psum_tensor(f"P{i}", [128, 512], F32)), f"P{i}", excl=True) for i in range(7)]
        PB = T(st.enter_context(nc.psum_tensor("PB", [128, 1024], BF16)), "PB", excl=True)
        sems = [st.enter_context(nc.semaphore(f"s_{k}")) for k in ["pe", "act", "dve", "pool", "sp"]]
        dsems = {q: [st.enter_context(nc.semaphore(f"d_{q}{i}")) for i in range(12)] for q in ["sp", "act", "pool"]}
        S = Sync(nc, sems, dsems)

        def C(name, lo=None, hi=None, rows=ALL):
            a, b = CL.off[name]
            if lo is not None:
                a, b = a + lo, a + hi
            return CST(rows, slice(a, b))

        ident = C("ident")
        ones = C("ones")

        def pbf(p, w=1024):
            return V(PB.t[:, 0:w], PB.bufs)

        jobs = []
        job_slot = {}
        state = {"next": 0}
        free = list(range(nslots))

        def issue_pending():
            while free and state["next"] < len(jobs):
                name, fn = jobs[state["next"]]
                state["next"] += 1
                si = free.pop(0)
                sl = SLOT[si]
                for (o, i) in fn(sl):
                    S.dma("pool", o, i, writes=sl.bufs)
                job_slot[name] = si

        def wget(name):
            assert name in job_slot, f"weight job {name} not issued (ring too small)"
            return SLOT[job_slot[name]]

        def wrel(name):
            si = job_slot.pop(name)
            free.append(si)
            issue_pending()

        def job_cols(name, src2d, c0, w):
            def fn(sl):
                dst = sl.t[:, 0:8 * w].rearrange("p (k c) -> p k c", k=8)
                return [(dst, src2d.rearrange("(k p) c -> p k c", p=128)[:, :, c0:c0 + w])]
            jobs.append((name, fn))

        def job_rows(name, src2d, r0, nch):
            def fn(sl):
                dst = sl.t[:, 0:nch * 1024].rearrange("p (j c) -> p j c", j=nch)
                return [(dst, src2d[r0:r0 + nch * 128, :].rearrange("(j p) c -> p j c", p=128))]
            jobs.append((name, fn))

        def wcols(sl, w):
            return lambda k, a, b: V(sl.t[:, k * w + a:k * w + b], sl.bufs)

        def wrows(sl):
            return lambda j, a, b: V(sl.t[:, j * 1024 + a:j * 1024 + b], sl.bufs)

        def ffn_groups(nchunks):
            g = []
            c = 0
            while c < nchunks:
                n = min(4, nchunks - c)
                g.append((c, n))
                c += n
            return g

        for l in layers:
            for g in range(12):
                job_cols(f"ada{l}_{g}", adaw_d[l], g * 512, 512)
            if "gla" in parts:
                job_cols(f"gqk{l}", win_d[l], 0, 512)
                job_cols(f"gv{l}", win_d[l], 512, 512)
                job_cols(f"gr{l}", win_d[l], 1024, 512)
                job_rows(f"wog{l}", wout_d[l], 0, 4)
            if "ret" in parts:
                job_cols(f"rq{l}", win_d[l], 1552, 512)
                job_cols(f"rk{l}", win_d[l], 2064, 512)
                job_cols(f"rv{l}", win_d[l], 2576, 512)
                job_cols(f"rg{l}", win_d[l], 3088, 512)
                job_rows(f"wor{l}", wout_d[l], 512, 4)
            if "ffn" in parts:
                i = l // 2
                if l % 2 == 0:
                    for gi, (c0, n) in enumerate(ffn_groups(22)):
                        job_cols(f"w1_{l}_{gi}", w1_d[i], c0 * 128, n * 128)
                        job_cols(f"w3_{l}_{gi}", w3_d[i], c0 * 128, n * 128)
                        job_rows(f"w2_{l}_{gi}", w2_d[i], c0 * 128, n)
                else:
                    for e in range(NE):
                        for gi, (c0, n) in enumerate(ffn_groups(11)):
                            job_cols(f"w1_{l}_{e}_{gi}", m1_d[i, e], c0 * 128, n * 128)
                            job_cols(f"w3_{l}_{e}_{gi}", m3_d[i, e], c0 * 128, n * 128)
                            job_rows(f"w2_{l}_{e}_{gi}", m2_d[i, e], c0 * 128, n)

        S.dma("sp", CST.t[:], cst_d, writes=CST.bufs)
        S.dma("sp", NG.t[:], ng_d, writes=NG.bufs)
        S.dma("sp", CV.t[:], c_d, writes=CV.bufs)
        S.dma("sp", POSI.t[:], pos_d, writes=POSI.bufs)
        issue_pending()
        S.copy("dve", IDB(), ident)
        S.act(CVB(), CV(), AF.Silu)
        S.memset("dve", GAT(), 1.0)

        for i in range(NT):
            xb = Fp[(i % 2) * 2], Fp[(i % 2) * 2 + 1]
            for hh in range(2):
                S.dma("sp" if hh == 0 else "act", xb[hh].t[:], x_d[i * 128:(i + 1) * 128, hh * 512:(hh + 1) * 512],
                      writes=xb[hh].bufs)
                pt = P[hh + 2 * (i % 2)]
                for j in range(4):
                    S.tr(V(pt.t[:, j * 128:(j + 1) * 128], pt.bufs), xb[hh](ALL, slice(j * 128, (j + 1) * 128)), ident,
                         signal=(j == 3))
                dst = V(X.t[:, hh * 4:(hh + 1) * 4, i * 128:(i + 1) * 128], [X.bufs[i]])
                src = V(pt.t[:].rearrange("p (j t) -> p j t", j=4), pt.bufs)
                S.copy("act" if hh == 0 else "dve", dst, src)

        if "ret" in parts:
            posf = V(SM.t[:, 0:NT], SM.bufs)
            S.copy("dve", posf, POSI())
            ang = Fp[4]
            angv = lambda i: V(ang.t[:, 0:1024].rearrange("p (i j) -> p i j", i=NT)[:, i, :], ang.bufs)
            two_pi = 2.0 * math.pi
            for which, shift in (("sin", 0.0), ("cos", math.pi / 2)):
                for half in range(2):
                    a = Fp[4 + half]
                    for ii in range(8):
                        i = half * 8 + ii
                        S.ts("dve", a(ALL, slice(ii * 64, (ii + 1) * 64)), C("invf"), V(SM.t[:, i:i + 1], SM.bufs),
                             shift, op0=ALU.mult, op1=ALU.add)
                    u = Fp[6]
                    ki = V(Hp[0].t[:].bitcast(I32)[:, 0:256], Hp[0].bufs)
                    ki2 = V(Hp[1].t[:].bitcast(I32)[:, 0:256], Hp[1].bufs)
                    S.ts("dve", u(), a(), 1.0 / two_pi, None, op0=ALU.mult)
                    for q4, kk in ((0, ki), (1, ki2)):
                        S.copy("dve", kk, u(ALL, slice(q4 * 256, (q4 + 1) * 256)))
                        S.copy("dve", u(ALL, slice(q4 * 256, (q4 + 1) * 256)), kk)
                    C1 = 6.28125
                    C2 = two_pi - C1
                    S.stt(a(), u(), -C1, a(), ALU.mult, ALU.add)
                    S.stt(a(), u(), -C2, a(), ALU.mult, ALU.add)
                    m = Fp[7]
                    S.ts("dve", m(), a(), math.pi, -two_pi, op0=ALU.is_gt, op1=ALU.mult)
                    S.tt("dve", a(), a(), m(), ALU.add)
                    S.ts("dve", m(), a(), -math.pi, two_pi, op0=ALU.is_lt, op1=ALU.mult)
                    S.tt("dve", a(), a(), m(), ALU.add)
                    S.ts("dve", a(), a(), math.pi, -math.pi, op0=ALU.min, op1=ALU.max)
                    col = 64 if which == "sin" else 0
                    dst = V(ROT.t[:, half * 8:(half + 1) * 8, col:col + 64], ROT.bufs)
                    S.act(dst, V(a.t[:].rearrange("p (i j) -> p i j", i=8), a.bufs), AF.Sin)

        def ada_phase(l):
            S.dma("sp", MODB.t[:], adab_d[l], writes=MODB.bufs)
            prow = P[5]
            pmod = P[6]
            for g in range(12):
                sl = wget(f"ada{l}_{g}")
                wc = wcols(sl, 512)
                for k in range(8):
                    S.mm(V(prow.t[0:1, :], prow.bufs), CVB(ALL, slice(k, k + 1)), wc(k, 0, 512),
                         start=(k == 0), stop=(k == 7))
                wrel(f"ada{l}_{g}")
                rb = Fp[g % 2]
                S.copy("act", rb(slice(0, 1), ALL), V(prow.t[0:1, :], prow.bufs))
                for j in range(4):
                    cc = g * 4 + j
                    S.mm(V(pmod.t[:, cc:cc + 1], pmod.bufs), rb(slice(0, 1), slice(j * 128, (j + 1) * 128)),
                         C("ones", 0, 1, rows=slice(0, 1)), start=True, stop=True, signal=(j == 3))
            S.tt("dve", MOD(), V(pmod.t[:, 0:48], pmod.bufs), MODB(), ALU.add)
            S.stt(AB(ALL, slice(0, 8)), MOD(ALL, slice(8, 16)), 1.0, NG(ALL, slice(l * 8, l * 8 + 8)), ALU.add, ALU.mult)
            S.stt(AB(ALL, slice(8, 16)), MOD(ALL, slice(32, 40)), 1.0,
                  NG(ALL, slice(DEPTH * 8 + l * 8, DEPTH * 8 + l * 8 + 8)), ALU.add, ALU.mult)

        def norm_stats(s, pss, sqa, sqb, lnv, rstd):
            sl = slice(s * 512, (s + 1) * 512)
            subs = list(range(4 * s, 4 * s + 4))
            for k in range(8):
                q = sqa if k % 2 == 0 else sqb
                S.act(q(), X(ALL, k, sl, sub=subs), AF.Square)
                S.mm(pss(), ones, q(), start=(k == 0), stop=(k == 7))
            S.act(lnv(), pss(), AF.Ln, bias=EPS, scale=1.0 / D)
            S.act(rstd(), lnv(), AF.Exp, scale=-0.5)

        def norm_phase(a_off, b_off):
            for s in range(4):
                sl = slice(s * 512, (s + 1) * 512)
                subs = list(range(4 * s, 4 * s + 4))
                pss = P[s % 2]
                rstd = Fp[3]
                norm_stats(s, pss, Fp[0], Fp[1], Fp[2], rstd)
                for k in range(8):
                    t = Fp[4 + (k % 2)]
                    S.stt(t(), X(ALL, k, sl, sub=subs), AB(ALL, slice(a_off + k, a_off + k + 1)), rstd(), ALU.mult, ALU.mult)
                    S.act(HT(ALL, k, sl, sub=subs), t(), AF.Identity, bias=MOD(ALL, slice(b_off + k, b_off + k + 1)), scale=1.0)

        def out_proj_update(i, wo, oT, g_off, pys):
            wr = wrows(wo)
            tsl = slice(i * 128, (i + 1) * 128)
            for half in range(2):
                py = pys[half]
                for mm_ in range(4):
                    m = half * 4 + mm_
                    o = V(py.t[:, mm_ * 128:(mm_ + 1) * 128], py.bufs)
                    for j in range(4):
                        S.mm(o, wr(j, m * 128, (m + 1) * 128), oT(ALL, slice(j * 128, (j + 1) * 128)),
                             start=(j == 0), stop=(j == 3), signal=(j == 3 and mm_ == 3))
                yield
                for mm_ in range(4):
                    m = half * 4 + mm_
                    o = V(py.t[:, mm_ * 128:(mm_ + 1) * 128], py.bufs)
                    S.stt(X(ALL, m, tsl, sub=i), o, MOD(ALL, slice(g_off + m, g_off + m + 1)), X(ALL, m, tsl, sub=i),
                          ALU.mult, ALU.add)
                yield

        def run_pipelined(tile_gen, depth=2):
            active = []
            nxt = 0
            while nxt < NT or active:
                if nxt < NT and len(active) < depth and all(a_[1] for a_ in active):
                    active.append([tile_gen(nxt), False])
                    nxt += 1
                for idx, a_ in enumerate(list(active)):
                    if a_[1] and idx > 0:
                        continue
                    try:
                        r = next(a_[0])
                        if r == 'B':
                            a_[1] = True
                    except StopIteration:
                        active.remove(a_)

        r3 = lambda v: V(v.ap.rearrange("p (h t) -> p h t", h=4), v.bufs)
        r2 = lambda v: V(v.ap.rearrange("p (a t) -> p a t", a=2), v.bufs)
        r4 = lambda v: V(v.ap.rearrange("p (h a j) -> p h a j", h=4, a=2), v.bufs)

        def gla_pass(l):
            S.dma("pool", WGA.t[:], win_d[l].rearrange("(k p) c -> p k c", p=128)[:, :, 1536:1552], writes=WGA.bufs)
            S.dma("pool", WAL.t[0:17, :], wal_d[l], writes=WAL.bufs)
            S.dma("sp", BCP.t[:], bc_d[l], writes=BCP.bufs)
            wqk = wcols(wget(f"gqk{l}"), 512)
            wv = wcols(wget(f"gv{l}"), 512)
            wr_ = wcols(wget(f"gr{l}"), 512)
            wo = wget(f"wog{l}")
            Sst = Fp[7]
            SbA, SbB = Hp[11], Hp[12]
            S.memset("dve", Sst(ALL, slice(0, 256)), 0.0)
            S.memset("dve", SbA(ALL, slice(0, 256)), 0.0)
            hm = lambda j: C("hm", j, j + 1)
            c0b = V(C("c0").ap.unsqueeze(1).broadcast_to([128, 2, 128]), CST.bufs)
            c1b = V(C("c1").ap.unsqueeze(1).broadcast_to([128, 2, 128]), CST.bufs)
            m1b = V(C("m1").ap.unsqueeze(1).broadcast_to([128, 4, 128]), CST.bufs)
            m2b = V(C("m2").ap.unsqueeze(1).broadcast_to([128, 4, 128]), CST.bufs)

            def tile(i):
                par = i % 2
                tsl = slice(i * 128, (i + 1) * 128)
                hT = lambda k: HT(ALL, k, tsl, sub=i)
                vbf = Hp[0] if par == 0 else Hp[14]
                ku = Hp[3] if par == 0 else Hp[15]
                QM0 = Hp[10] if par == 0 else Hp[16]
                QM1 = Hp[13] if par == 0 else Hp[17]
                scT = Hp[7] if par == 0 else Hp[18]
                sg = Fp[3] if par == 0 else Fp[6]
                pga = V(P[0].t[0:16, 0:128], P[0].bufs)
                for k in range(8):
                    S.mm(pga, V(WGA.t[:, k, :], WGA.bufs), hT(k), start=(k == 0), stop=(k == 7))
                S.copy("act", GAT(slice(0, 16), ALL), pga)
                pqk, pv = P[2], P[3]
                for k in range(8):
                    S.mm(pv(), hT(k), wv(k, 0, 512), start=(k == 0), stop=(k == 7))
                yield
                pz = V(P[0].t[:, 256:512], P[0].bufs)
                S.mm(pz, GAT(slice(0, 17), ALL), WAL(slice(0, 17), ALL))
                for k in range(8):
                    S.mm(pqk(), hT(k), wqk(k, 0, 512), start=(k == 0), stop=(k == 7))
                e_, sp_ = V(Fp[0].t[:, 0:256], Fp[0].bufs), V(Fp[0].t[:, 256:512], Fp[0].bufs)
                S.act(e_, pz, AF.Exp, scale=-1.0)
                S.act(sp_, e_, AF.Ln, bias=1.0, scale=1.0)
                S.copy("act", vbf(), pv())
                yield
                pb = V(P[1].t[:, 0:256], P[1].bufs)
                pd = V(P[1].t[:, 256:512], P[1].bufs)
                S.mm(pb, C("tri"), sp_)
                S.mm(pd, C("dm"), sp_)
                for p_ in range(2):
                    S.mm(V(P[0].t[:, 128 + 2 * p_:130 + 2 * p_], P[0].bufs),
                         V(Fp[0].t[:, 256 + p_ * 128:256 + (p_ + 1) * 128], Fp[0].bufs), C("cind"))
                pbl = V(P[0].t[:, 128:132], P[0].bufs)
                pr = P[3]
                for k in range(8):
                    S.mm(pr(), hT(k), wr_(k, 0, 512), start=(k == 0), stop=(k == 7))
                eb, enb = V(Fp[1].t[:, 0:256], Fp[1].bufs), V(Fp[1].t[:, 256:512], Fp[1].bufs)
                eD = V(Fp[2].t[:, 0:256], Fp[2].bufs)
                dec = V(SM.t[:, 32 + 4 * par:36 + 4 * par], SM.bufs)
                S.act(eb, pb, AF.Exp)
                S.act(enb, pb, AF.Exp, scale=-1.0)
                S.act(eD, pd, AF.Exp)
                S.act(dec, pbl, AF.Exp)
                yield
                QKa, QKb = Hp[1], Hp[2]
                pq_ = V(pqk.t[:, 0:256], pqk.bufs)
                pk_ = V(pqk.t[:, 256:512], pqk.bufs)
                S.stt(QKa(ALL, slice(0, 256)), pq_, 0.125, eb, ALU.mult, ALU.mult)
                S.tt("dve", QKa(ALL, slice(256, 512)), pk_, enb, ALU.mult)
                S.stt(QKb(ALL, slice(0, 256)), pq_, 0.125, enb, ALU.mult, ALU.mult)
                S.tt("dve", QKb(ALL, slice(256, 512)), pk_, eb, ALU.mult)
                for c in range(2):
                    S.stt(ku(ALL, slice(c * 256, (c + 1) * 256)), pk_, hm(c), eD, ALU.mult, ALU.mult)
                S.act(sg(), pr(), AF.Silu)
                yield
                S.tt("dve", sg(), sg(), BCP(ALL, slice(0, 512)), ALU.mult)
                ptb = pbf(None)
                for src_i, src in enumerate((QKa, QKb)):
                    for j in range(4):
                        jj = src_i * 4 + j
                        S.tr(V(ptb.ap[:, jj * 128:(jj + 1) * 128], ptb.bufs), src(ALL, slice(j * 128, (j + 1) * 128)), IDB(),
                             signal=(j == 3))
                yield
                KT, QF, QB = Hp[4], Hp[5], Hp[6]
                S.copy("act", KT(ALL, slice(0, 256)), V(ptb.ap[:, 256:512], ptb.bufs))
                S.copy("act", KT(ALL, slice(256, 512)), V(ptb.ap[:, 768:1024], ptb.bufs))
                qfT = V(ptb.ap[:, 0:256], ptb.bufs)
                qbT = V(ptb.ap[:, 512:768], ptb.bufs)
                for hl in range(2):
                    hsl = slice(hl * 256, (hl + 1) * 256)
                    S.act(QF(ALL, hsl), qfT, AF.Copy, scale=hm(hl))
                    S.act(QB(ALL, hsl), qbT, AF.Copy, scale=hm(hl))
                    S.stt(r2(QM0(ALL, hsl)), r2(qfT), hm(hl), c0b, ALU.mult, ALU.mult)
                    S.stt(r2(QM1(ALL, hsl)), r2(qfT), hm(hl), c1b, ALU.mult, ALU.mult)
                yield
                psf, psb = P[2], P[3]
                for h in range(4):
                    pr2, hl = h // 2, h % 2
                    S.mm(V(psf.t[:, h * 128:(h + 1) * 128], psf.bufs), KT(ALL, slice(pr2 * 128, (pr2 + 1) * 128)),
                         QF(ALL, slice(hl * 256 + pr2 * 128, hl * 256 + (pr2 + 1) * 128)), signal=(h == 3))
                for h in range(4):
                    pr2, hl = h // 2, h % 2
                    S.mm(V(psb.t[:, h * 128:(h + 1) * 128], psb.bufs), KT(ALL, slice(256 + pr2 * 128, 256 + (pr2 + 1) * 128)),
                         QB(ALL, slice(hl * 256 + pr2 * 128, hl * 256 + (pr2 + 1) * 128)), signal=(h == 3))
                yield
                mm1, mm2 = Fp[4], Fp[5]
                S.tt("dve", r3(mm1()), r3(psf()), m1b, ALU.mult)
                S.tt("dve", r3(mm2()), r3(psb()), m2b, ALU.mult)
                S.tt("dve", scT(), mm1(), mm2(), ALU.add)
                yield 'B'
                po, pu = P[4], P[5]
                for h in range(4):
                    S.mm(V(po.t[:, h * 128:(h + 1) * 128], po.bufs), scT(ALL, slice(h * 128, (h + 1) * 128)),
                         vbf(ALL, slice(h * 128, (h + 1) * 128)), start=(h == 0), stop=False, signal=False)
                for c in range(2):
                    Sb = SbA if c == 0 else SbB
                    QM = QM0 if c == 0 else QM1
                    for h in range(4):
                        pr2, hl = h // 2, h % 2
                        S.mm(V(po.t[:, h * 128:(h + 1) * 128], po.bufs),
                             QM(ALL, slice(hl * 256 + pr2 * 128, hl * 256 + (pr2 + 1) * 128)),
                             Sb(ALL, slice(pr2 * 128, (pr2 + 1) * 128)),
                             start=False, stop=(c == 1 and h == 3), signal=(c == 1 and h == 3))
                    for pr2 in range(2):
                        S.mm(V(pu.t[:, pr2 * 256:(pr2 + 1) * 256], pu.bufs),
                             ku(ALL, slice(c * 256 + pr2 * 128, c * 256 + (pr2 + 1) * 128)),
                             vbf(ALL, slice(pr2 * 256, (pr2 + 1) * 256)), signal=(pr2 == 1))
                    yield
                    for pr2 in range(2):
                        for hl in range(2):
                            rows = slice(hl * 64, hl * 64 + 64)
                            sv = Sst(rows, slice(pr2 * 128, (pr2 + 1) * 128))
                            S.stt(sv, sv, V(SM.t[rows, 32 + 4 * par + 2 * pr2 + c:33 + 4 * par + 2 * pr2 + c], SM.bufs),
                                  V(pu.t[rows, pr2 * 256 + hl * 128:pr2 * 256 + (hl + 1) * 128], pu.bufs),
                                  ALU.mult, ALU.add)
                    Sn = SbB if c == 0 else SbA
                    S.copy("act", Sn(ALL, slice(0, 256)), Sst(ALL, slice(0, 256)))
                    yield
                sq = Fp[4]
                S.act(sq(), po(), AF.Square)
                ss = V(SM.t[:, 40:44], SM.bufs)
                S.reduce(ss, r3(sq()), ALU.add)
                yield
                lv = V(SM.t[:, 44:48], SM.bufs)
                rs = V(SM.t[:, 48:52], SM.bufs)
                S.act(lv, ss, AF.Ln, bias=EPS, scale=1.0 / 128)
                S.act(rs, lv, AF.Exp, scale=-0.5)
                yield
                og = Hp[8]
                for h in range(4):
                    hs = slice(h * 128, (h + 1) * 128)
                    S.stt(og(ALL, hs), V(po.t[:, hs], po.bufs), V(SM.t[:, 48 + h:49 + h], SM.bufs), sg(ALL, hs),
                          ALU.mult, ALU.mult)
                yield
                pt2 = pbf(None, 512)
                for h in range(4):
                    S.tr(V(pt2.ap[:, h * 128:(h + 1) * 128], pt2.bufs), og(ALL, slice(h * 128, (h + 1) * 128)), IDB(),
                         signal=(h == 3))
                ogT = Hp[9]
                S.copy("act", ogT(), pt2)
                yield
                yield from out_proj_update(i, wo, ogT, 16, (P[5], P[6]))

            run_pipelined(tile)
            for n_ in (f"gqk{l}", f"gv{l}", f"gr{l}", f"wog{l}"):
                wrel(n_)

        def ret_pass(l):
            if "gla" not in parts:
                S.dma("sp", BCP.t[:], bc_d[l], writes=BCP.bufs)
            wq = wcols(wget(f"rq{l}"), 512)
            wk = wcols(wget(f"rk{l}"), 512)
            wv = wcols(wget(f"rv{l}"), 512)
            wg = wcols(wget(f"rg{l}"), 512)
            wo = wget(f"wor{l}")
            R = Fp[7]
            RbA, RbB = Hp[11], Hp[12]
            S.memset("dve", R(), 0.0)
            S.memset("dve", RbA(), 0.0)

            def tile(i):
                par = i % 2
                tsl = slice(i * 128, (i + 1) * 128)
                hT = lambda k: HT(ALL, k, tsl, sub=i)
                vbf = Hp[0] if par == 0 else Hp[14]
                kdc = (Hp[3], Hp[13]) if par == 0 else (Hp[15], Hp[16])
                qd0 = Hp[6] if par == 0 else Hp[17]
                qd1 = Hp[7] if par == 0 else Hp[18]
                scT = Hp[8] if par == 0 else Hp[19]
                sg = Fp[3] if par == 0 else Fp[6]
                so = 0 if par == 0 else 24
                pq, pk, pv, pg = P[0], P[1], P[2], P[3]
                for (pp, w) in ((pq, wq), (pk, wk)):
                    for k in range(8):
                        S.mm(pp(), hT(k), w(k, 0, 512), start=(k == 0), stop=(k == 7))
                    yield
                cosb = V(ROT.t[:, i, 0:64].unsqueeze(1).unsqueeze(1).broadcast_to([128, 4, 2, 64]), ROT.bufs)
                sinb = V(ROT.t[:, i, 64:128].unsqueeze(1).broadcast_to([128, 4, 64]), ROT.bufs)
                qr, kr = Hp[1], Hp[2]
                for n_, (pp, dst) in enumerate(((pq, qr), (pk, kr))):
                    a_, b_ = Fp[0], Fp[1]
                    S.tt("dve", r4(a_()), r4(pp()), cosb, ALU.mult)
                    p4 = r4(pp())
                    b4 = r4(b_())
                    S.stt(V(b4.ap[:, :, 0, :], b_.bufs), V(p4.ap[:, :, 1, :], pp.bufs), -1.0, sinb, ALU.mult, ALU.mult)
                    S.tt("dve", V(b4.ap[:, :, 1, :], b_.bufs), V(p4.ap[:, :, 0, :], pp.bufs), sinb, ALU.mult)
                    if n_ == 0:
                        for k in range(8):
                            S.mm(pv(), hT(k), wv(k, 0, 512), start=(k == 0), stop=(k == 7))
                    else:
                        for k in range(8):
                            S.mm(pg(), hT(k), wg(k, 0, 512), start=(k == 0), stop=(k == 7))
                    yield
                    S.tt("dve", dst(), a_(), b_(), ALU.add)
                    if n_ == 0:
                        S.copy("act", vbf(), pv())
                    else:
                        S.act(sg(), pg(), AF.Silu)
                    yield
                ptq = pbf(None, 512)
                ptk = V(PB.t[:, 512:1024], PB.bufs)
                for h in range(4):
                    hs = slice(h * 128, (h + 1) * 128)
                    S.tr(V(ptq.ap[:, hs], ptq.bufs), qr(ALL, hs), IDB(), signal=(h == 3))
                for h in range(4):
                    hs = slice(h * 128, (h + 1) * 128)
                    S.tr(V(ptk.ap[:, hs], ptk.bufs), kr(ALL, hs), IDB(), signal=(h == 3))
                for c in range(2):
                    for h in range(4):
                        hs = slice(h * 128, (h + 1) * 128)
                        S.act(kdc[c](ALL, hs), kr(ALL, hs), AF.Copy, scale=C("kdec", 4 * c + h, 4 * c + h + 1))
                yield
                qT, kT = Hp[4], Hp[5]
                S.copy("act", qT(), ptq)
                S.copy("act", kT(), ptk)
                S.tt("dve", qd0(), ptq, C("qd0"), ALU.mult)
                S.tt("dve", qd1(), ptq, C("qd1"), ALU.mult)
                yield
                ps = P[0]
                for h in range(4):
                    hs = slice(h * 128, (h + 1) * 128)
                    S.mm(V(ps.t[:, hs], ps.bufs), kT(ALL, hs), qT(ALL, hs), signal=(h == 3))
                yield
                S.tt("dve", scT(), ps(), C("dret"), ALU.mult)
                yield 'B'
                po, pu = P[4], P[5]
                for h in range(4):
                    hs = slice(h * 128, (h + 1) * 128)
                    S.mm(V(po.t[:, hs], po.bufs), scT(ALL, hs), vbf(ALL, hs), start=(h == 0), stop=False, signal=False)
                for c in range(2):
                    Rb = RbA if c == 0 else RbB
                    qd = qd0 if c == 0 else qd1
                    for h in range(4):
                        hs = slice(h * 128, (h + 1) * 128)
                        S.mm(V(po.t[:, hs], po.bufs), qd(ALL, hs), Rb(ALL, hs), start=False, stop=(c == 1 and h == 3),
                             signal=(c == 1 and h == 3))
                    for h in range(4):
                        hs = slice(h * 128, (h + 1) * 128)
                        S.mm(V(pu.t[:, hs], pu.bufs), kdc[c](ALL, hs), vbf(ALL, hs), signal=(h == 3))
                    yield
                    for h in range(4):
                        hs = slice(h * 128, (h + 1) * 128)
                        S.stt(R(ALL, hs), R(ALL, hs), float(GAMMA[h] ** 64), V(pu.t[:, hs], pu.bufs), ALU.mult, ALU.add)
                    Rn = RbB if c == 0 else RbA
                    S.copy("act", Rn(), R())
                    yield
                sm = lambda a, b: V(SM.t[:, so + a:so + b], SM.bufs)
                s1, s2, mean, var, rs, nmr = sm(0, 4), sm(4, 8), sm(8, 12), sm(12, 16), sm(16, 20), sm(20, 24)
                S.reduce(s1, r3(po()), ALU.add)
                sq = Fp[4]
                S.act(sq(), po(), AF.Square)
                yield
                S.reduce(s2, r3(sq()), ALU.add)
                S.ts("dve", mean, s1, 1.0 / 128, None, op0=ALU.mult)
                S.tt("dve", var, mean, mean, ALU.mult)
                S.stt(var, s2, 1.0 / 128, var, ALU.mult, ALU.subtract)
                S.ts("dve", var, var, 0.0, None, op0=ALU.max)
                yield
                S.act(var, var, AF.Ln, bias=EPS, scale=1.0)
                S.act(rs, var, AF.Exp, scale=-0.5)
                yield
                S.stt(nmr, mean, -1.0, rs, ALU.mult, ALU.mult)
                on = Fp[5]
                for h in range(4):
                    hs = slice(h * 128, (h + 1) * 128)
                    S.act(on(ALL, hs), V(po.t[:, hs], po.bufs), AF.Identity, bias=V(SM.t[:, so + 20 + h:so + 21 + h], SM.bufs),
                          scale=V(SM.t[:, so + 16 + h:so + 17 + h], SM.bufs))
                yield
                S.tt("dve", on(), on(), BCP(ALL, slice(512, 1024)), ALU.mult)
                S.tt("dve", on(), on(), BCP(ALL, slice(1024, 1536)), ALU.add)
                orr = Hp[9]
                S.tt("dve", orr(), on(), sg(), ALU.mult)
                yield
                pt2 = pbf(None, 512)
                for h in range(4):
                    hs = slice(h * 128, (h + 1) * 128)
                    S.tr(V(pt2.ap[:, hs], pt2.bufs), orr(ALL, hs), IDB(), signal=(h == 3))
                orT = Hp[10]
                S.copy("act", orT(), pt2)
                yield
                yield from out_proj_update(i, wo, orT, 16, (P[5], P[6]))

            run_pipelined(tile)
            for n_ in (f"rq{l}", f"rk{l}", f"rv{l}", f"rg{l}", f"wor{l}"):
                wrel(n_)

        def ffn_group(names, nch, gate):
            n1, n3, n2 = names
            w1 = wcols(wget(n1), nch * 128)
            w3 = wcols(wget(n3), nch * 128)
            w2 = wrows(wget(n2))
            hb = ((P[4], P[5]), (P[6], None))
            py = P[0:4]

            def pbank(j, which):
                p = hb[j % 2][which]
                if p is None:
                    return V(PB.t[:].bitcast(F32), PB.bufs)
                return p()

            def up(s, j):
                ssl = slice(s * 512, (s + 1) * 512)
                subs = list(range(4 * s, 4 * s + 4))
                ph1, ph3 = pbank(j, 0), pbank(j, 1)
                for (ph, w) in ((ph1, w1), (ph3, w3)):
                    for k in range(8):
                        S.mm(ph, w(k, j * 128, (j + 1) * 128), HT(ALL, k, ssl, sub=subs), start=(k == 0), stop=(k == 7))
                s_ = Fp[j % 4]
                a_ = Hp[(s % 2) * 4 + j]
                S.act(s_(), ph1, AF.Silu)
                if gate is not None:
                    S.tt("dve", s_(), s_(), gate(s), ALU.mult)
                S.tt("dve", a_(), ph3, s_(), ALU.mult)

            def down(s, mrange):
                tsl = slice(s * 512, (s + 1) * 512)
                subs = list(range(4 * s, 4 * s + 4))
                for m in mrange:
                    o = py[m % 4]()
                    for j in range(nch):
                        a_ = Hp[(s % 2) * 4 + j]
                        S.mm(o, w2(j, m * 128, (m + 1) * 128), a_(), start=(j == 0), stop=(j == nch - 1))
                    S.stt(X(ALL, m, tsl, sub=subs), o, MOD(ALL, slice(40 + m, 41 + m)), X(ALL, m, tsl, sub=subs),
                          ALU.mult, ALU.add)

            nfirst = (nch + 1) // 2
            for j in range(nch):
                up(0, j)
            for s in range(4):
                down(s, range(0, 4))
                if s + 1 < 4:
                    for j in range(nfirst):
                        up(s + 1, j)
                down(s, range(4, 8))
                if s + 1 < 4:
                    for j in range(nfirst, nch):
                        up(s + 1, j)
            for n_ in names:
                wrel(n_)

        def dense_ffn(l):
            for gi, (c0, n) in enumerate(ffn_groups(22)):
                ffn_group((f"w1_{l}_{gi}", f"w3_{l}_{gi}", f"w2_{l}_{gi}"), n, None)

        def moe_ffn(l):
            i_ = l // 2
            S.dma("pool", RW.t[:], rw_d[i_].rearrange("(k p) e -> p k e", p=128), writes=RW.bufs)
            plb = P[4]
            for i in range(NT):
                tsl = slice(i * 128, (i + 1) * 128)
                pl = V(plb.t[:, i * 8:(i + 1) * 8], plb.bufs)
                for k in range(8):
                    S.mm(pl, HT(ALL, k, tsl, sub=i), V(RW.t[:, k, :], RW.bufs), start=(k == 0), stop=(k == 7),
                         signal=(k == 7 and i == NT - 1))
            fa = lambda t_, q: V(t_.t[:, q * 128:(q + 1) * 128], t_.bufs)
            f3 = lambda v: V(v.ap.rearrange("p (i e) -> p i e", i=NT), v.bufs)
            lg, eq, l2, selm = fa(Fp[0], 0), fa(Fp[0], 1), fa(Fp[0], 2), fa(Fp[0], 3)
            dd, ex = fa(Fp[1], 0), fa(Fp[1], 1)
            m1_, m2_, ssum = V(SM.t[:, 0:16], SM.bufs), V(SM.t[:, 16:32], SM.bufs), V(SM.t[:, 32:48], SM.bufs)
            bc3 = lambda v: V(v.ap.unsqueeze(2).broadcast_to([128, NT, 8]), v.bufs)
            S.copy("dve", lg, V(plb.t[:, 0:128], plb.bufs))
            S.reduce(m1_, f3(lg), ALU.max)
            S.tt("dve", f3(eq), f3(lg), bc3(m1_), ALU.is_equal)
            S.stt(l2, eq, -1e30, lg, ALU.mult, ALU.add)
            S.reduce(m2_, f3(l2), ALU.max)
            S.tt("dve", f3(selm), f3(lg), bc3(m2_), ALU.is_ge)
            S.tt("dve", f3(dd), f3(lg), bc3(m1_), ALU.subtract)
            S.act(ex, dd, AF.Exp)
            S.tt("dve", ex, ex, selm, ALU.mult)
            S.reduce(ssum, f3(ex), ALU.add)
            S.op("dve", lambda h: h.reciprocal(out=ssum.ap, in_=ssum.ap), reads=ssum.bufs, writes=ssum.bufs)
            S.tt("dve", V(GATES.t[:], GATES.bufs), f3(ex), bc3(ssum), ALU.mult)
            GB = [Fp[4], Fp[5], Fp[6], Fp[7]]
            if debug:
                S.dma("sp", dbg_d[:, 0:128], GATES.t[:].rearrange("p i e -> p (i e)"), reads=GATES.bufs)
            for e in range(NE):
                for s in range(4):
                    pg_ = P[4 + (s % 2)]
                    for j in range(4):
                        i = s * 4 + j
                        gbt = Hp[8 + (i % 2)]
                        gb = lambda gbt=gbt: V(gbt.t[:].bitcast(F32)[:, 0:128], gbt.bufs)
                        S.ts("dve", gb(), ones, V(GATES.t[:, i, e:e + 1], GATES.bufs), None, op0=ALU.mult)
                        S.mm(V(pg_.t[:, j * 128:(j + 1) * 128], pg_.bufs), gb(), ident, signal=(j == 3))
                    S.copy("act", GB[s](), pg_())
                if debug and e == 3:
                    for s in range(4):
                        S.dma("sp", dbg_d[:, 128 + s * 512:128 + (s + 1) * 512], GB[s].t[:], reads=GB[s].bufs)
                gate = lambda s_i: GB[s_i]()
                for gi, (c0, n) in enumerate(ffn_groups(11)):
                    ffn_group((f"w1_{l}_{e}_{gi}", f"w3_{l}_{e}_{gi}", f"w2_{l}_{e}_{gi}"), n, gate)

        for l in layers:
            ada_phase(l)
            if "gla" in parts or "ret" in parts:
                norm_phase(0, 0)
            if "gla" in parts:
                gla_pass(l)
            if "ret" in parts:
                ret_pass(l)
            if "ffn" in parts:
                norm_phase(8, 24)
                if l % 2 == 0:
                    dense_ffn(l)
                else:
                    moe_ffn(l)

        fg0 = 2 * DEPTH * 8
        for s in range(4):
            subs = list(range(4 * s, 4 * s + 4))
            rstd = Fp[3]
            if do_final:
                norm_stats(s, P[s % 2], Fp[0], Fp[1], Fp[2], rstd)
            for j in range(4):
                ti = s * 4 + j
                tsl = slice(ti * 128, (ti + 1) * 128)
                ob = (Fp[4], Fp[5]) if ti % 2 == 0 else (Fp[6], Fp[7])
                for k in range(8):
                    t = Hp[k]
                    tv = V(t.t[:].bitcast(F32)[:, 0:128], t.bufs)
                    if do_final:
                        S.stt(tv, X(ALL, k, tsl, sub=ti), NG(ALL, slice(fg0 + k, fg0 + k + 1)),
                              rstd(ALL, slice(j * 128, (j + 1) * 128)), ALU.mult, ALU.mult)
                    else:
                        S.copy("dve", tv, X(ALL, k, tsl, sub=ti))
                    pt = P[2 + k // 4]
                    S.tr(V(pt.t[:, (k % 4) * 128:(k % 4 + 1) * 128], pt.bufs), tv, ident, signal=(k % 4 == 3))
                    if k % 4 == 3:
                        S.copy("act", ob[k // 4](), pt())
                for hh in range(2):
                    S.dma("sp" if hh == 0 else "act", y_d[ti * 128:(ti + 1) * 128, hh * 512:(hh + 1) * 512], ob[hh].t[:],
                          reads=ob[hh].bufs)
        S.wait_bufs("sp", Fp[4].bufs + Fp[5].bufs + Fp[6].bufs + Fp[7].bufs)
        assert state["next"] == len(jobs), (state["next"], len(jobs))
        build_program.stats = (S.nins, S.nwaits)
    return nc


def _prep_shared(inp):
    f = lambda a: np.ascontiguousarray(np.asarray(a, dtype=np.float32))
    sh = {}
    sh["cst"] = make_consts()
    sh["ada_w"] = f(inp["ada_w"])
    sh["ada_b_fm"] = f(np.asarray(inp["ada_b"], np.float32).reshape(DEPTH, 48, 128).transpose(0, 2, 1))
    ng = np.concatenate([
        np.asarray(inp["norm_mix_g"], np.float32).reshape(DEPTH, 8, 128).transpose(2, 0, 1).reshape(128, DEPTH * 8),
        np.asarray(inp["norm_ffn_g"], np.float32).reshape(DEPTH, 8, 128).transpose(2, 0, 1).reshape(128, DEPTH * 8),
        np.asarray(inp["final_g"], np.float32).reshape(8, 128).T], axis=1)
    sh["norm_g_fm"] = f(ng)
    sh["w_in"] = f(inp["w_in"])
    sh["w_alpha_aug"] = f(np.concatenate([np.asarray(inp["gla_w_alpha"], np.float32),
                                          np.asarray(inp["gla_b_alpha"], np.float32)[:, None, :]], axis=1))
    bc = np.concatenate([np.asarray(inp["gla_norm_g"], np.float32), np.asarray(inp["ret_gn_g"], np.float32),
                         np.asarray(inp["ret_gn_b"], np.float32)], axis=1)
    sh["bc_params"] = f(np.broadcast_to(bc[:, None, :], (DEPTH, 128, 1536)))
    sh["w_out"] = f(inp["w_out"])
    for k_ in ("ffn_w1", "ffn_w3", "ffn_w2", "router_w", "moe_w1", "moe_w3", "moe_w2"):
        sh[k_] = f(inp[k_])
    return sh


def _prep_core(inp, b):
    d = {}
    d["x"] = np.ascontiguousarray(np.asarray(inp["x"][b], np.float32))
    d["cvec"] = np.ascontiguousarray(np.asarray(inp["c"][b], np.float32).reshape(8, 128).T)
    d["pos"] = np.ascontiguousarray(np.asarray(inp["positions"][b]).astype(np.int32).reshape(NT, 128).T)
    return d


_CACHE = {}


def kernel(**inputs):
    key = "full"
    if key not in _CACHE:
        _CACHE[key] = build_program()
    nc = _CACHE[key]
    sh = _prep_shared(inputs)
    in_maps = []
    for b in range(8):
        d = dict(sh)
        d.update(_prep_core(inputs, b))
        in_maps.append(d)
    res = run_bass_kernel_spmd(nc, in_maps, core_ids=list(range(8)))
    out = np.stack([np.asarray(res.results[b]["y"], np.float32) for b in range(8)], axis=0)
    return out
```
